# Optimizing a Trainium2 kernel written in Bass

```python
import jax, jax.numpy as jnp
from jax import lax
import numpy as np

D_MODEL = 1024
BATCH = 1
SEQ = 16384
DEPTH = 2

CHUNK = 64
N_META = 16
CONV_WIDTH = 31
POOL_WINDOWS = (2, 4, 8, 16)
N_POOL_GROUPS = len(POOL_WINDOWS)
POOL_GROUP = D_MODEL // N_POOL_GROUPS
D_FF = 2816
N_EXPERTS = 8
TOP_K = 2
D_FF_EXPERT = 3584
N_MIX_A = (DEPTH + 1) // 2
N_MIX_B = DEPTH // 2
N_DENSE = (DEPTH + 1) // 2
N_MOE = DEPTH // 2
RMS_EPS = 1e-6
LN_EPS = 1e-5

kernel_name = "hybrid_conv_pool_moe_encoder"


def rms_norm(x, g):
    xf = x.astype(jnp.float32)
    y = xf * lax.rsqrt(jnp.mean(xf * xf, axis=-1, keepdims=True) + RMS_EPS)
    return y.astype(x.dtype) * g


def layer_norm(x, g, b):
    xf = x.astype(jnp.float32)
    mu = jnp.mean(xf, axis=-1, keepdims=True)
    var = jnp.mean(jnp.square(xf - mu), axis=-1, keepdims=True)
    y = (xf - mu) * lax.rsqrt(var + LN_EPS)
    return y.astype(x.dtype) * g + b


def conformer_conv(h, w_pw1, b_pw1, w_dw, b_dw, ln_g, ln_b, w_pw2, b_pw2):
    u = h @ w_pw1 + b_pw1
    a, gate = jnp.split(u, 2, axis=-1)
    u = a * jax.nn.sigmoid(gate)
    u = lax.conv_general_dilated(
        u, w_dw[:, None, :].astype(u.dtype),
        window_strides=(1,), padding=((CONV_WIDTH - 1, 0),),
        dimension_numbers=("NWC", "WIO", "NWC"),
        feature_group_count=D_MODEL) + b_dw
    u = jax.nn.silu(layer_norm(u, ln_g, ln_b))
    return u @ w_pw2 + b_pw2


def multiscale_pool(h, w_group, scale):
    B, L, _ = h.shape
    hg = h.reshape(B, L, N_POOL_GROUPS, POOL_GROUP).astype(jnp.float32)
    pos = jnp.arange(L)
    outs = []
    for gi, w in enumerate(POOL_WINDOWS):
        xg = hg[:, :, gi, :]
        cs = jnp.cumsum(xg, axis=1)
        cs_shift = jnp.pad(cs, ((0, 0), (w, 0), (0, 0)))[:, :L]
        count = jnp.minimum(pos + 1, w).astype(jnp.float32)[None, :, None]
        outs.append((cs - cs_shift) / count - xg)
    pooled = jnp.stack(outs, axis=2).astype(h.dtype)
    mixed = jnp.einsum("blgc,gcd->blgd", pooled, w_group)
    return mixed.reshape(B, L, D_MODEL) * scale


def swiglu(h, w_gate, w_up, w_down):
    return (jax.nn.silu(h @ w_gate) * (h @ w_up)) @ w_down


def moe_swiglu(h, w_router, e_gate, e_up, e_down):
    logits = (h @ w_router).astype(jnp.float32)
    top_vals, top_idx = lax.top_k(logits, TOP_K)
    top_w = jax.nn.softmax(top_vals, axis=-1)
    combine = jnp.sum(jax.nn.one_hot(top_idx, N_EXPERTS, dtype=jnp.float32)
                      * top_w[..., None], axis=-2).astype(h.dtype)
    y = jnp.zeros_like(h)
    for e in range(N_EXPERTS):
        y = y + combine[..., e:e + 1] * swiglu(h, e_gate[e], e_up[e], e_down[e])
    return y


def setup_inputs(seed: int = 0) -> dict:
    key = jax.random.key(seed)
    ks = iter(jax.random.split(key, 32))
    D = D_MODEL

    def nrm(shape, fan_in):
        return jax.random.normal(next(ks), shape, jnp.float32) * (fan_in ** -0.5)

    def gain(shape):
        return 1.0 + 0.05 * jax.random.normal(next(ks), shape, jnp.float32)

    def bias(shape):
        return 0.02 * jax.random.normal(next(ks), shape, jnp.float32)

    return {
        "x": jax.random.normal(next(ks), (BATCH, SEQ, D), jnp.float32),
        "meta_tokens": jax.random.normal(next(ks), (N_META, D), jnp.float32),
        "conv_w_pw1": nrm((N_MIX_A, D, 2 * D), D),
        "conv_b_pw1": bias((N_MIX_A, 2 * D)),
        "conv_w_dw": nrm((N_MIX_A, CONV_WIDTH, D), CONV_WIDTH),
        "conv_b_dw": bias((N_MIX_A, D)),
        "conv_ln_g": gain((N_MIX_A, D)),
        "conv_ln_b": bias((N_MIX_A, D)),
        "conv_w_pw2": nrm((N_MIX_A, D, D), D),
        "conv_b_pw2": bias((N_MIX_A, D)),
        "pool_w_group": nrm((N_MIX_B, N_POOL_GROUPS, POOL_GROUP, POOL_GROUP), POOL_GROUP),
        "pool_scale": gain((N_MIX_B, D)),
        "ffn_w_gate": nrm((N_DENSE, D, D_FF), D),
        "ffn_w_up": nrm((N_DENSE, D, D_FF), D),
        "ffn_w_down": nrm((N_DENSE, D_FF, D), D_FF),
        "moe_w_router": nrm((N_MOE, D, N_EXPERTS), D),
        "moe_w_gate": nrm((N_MOE, N_EXPERTS, D, D_FF_EXPERT), D),
        "moe_w_up": nrm((N_MOE, N_EXPERTS, D, D_FF_EXPERT), D),
        "moe_w_down": nrm((N_MOE, N_EXPERTS, D_FF_EXPERT, D), D_FF_EXPERT),
        "mix_norm_g": gain((DEPTH, D)),
        "ffn_norm_g": gain((DEPTH, D)),
        "final_norm_g": gain((D,)),
    }


def reference(x, meta_tokens,
              conv_w_pw1, conv_b_pw1, conv_w_dw, conv_b_dw, conv_ln_g, conv_ln_b,
              conv_w_pw2, conv_b_pw2,
              pool_w_group, pool_scale,
              ffn_w_gate, ffn_w_up, ffn_w_down,
              moe_w_router, moe_w_gate, moe_w_up, moe_w_down,
              mix_norm_g, ffn_norm_g, final_norm_g):
    B = x.shape[0]
    meta = jnp.broadcast_to(meta_tokens[None].astype(x.dtype), (B, N_META, D_MODEL))
    h = jnp.concatenate([meta, x], axis=1)

    for i in range(DEPTH):
        j = i // 2
        hn = rms_norm(h, mix_norm_g[i])
        if i % 2 == 0:
            h = h + conformer_conv(hn, conv_w_pw1[j], conv_b_pw1[j], conv_w_dw[j],
                                   conv_b_dw[j], conv_ln_g[j], conv_ln_b[j],
                                   conv_w_pw2[j], conv_b_pw2[j])
        else:
            h = h + multiscale_pool(hn, pool_w_group[j], pool_scale[j])
        hn = rms_norm(h, ffn_norm_g[i])
        if i % 2 == 0:
            h = h + swiglu(hn, ffn_w_gate[j], ffn_w_up[j], ffn_w_down[j])
        else:
            h = h + moe_swiglu(hn, moe_w_router[j], moe_w_gate[j], moe_w_up[j], moe_w_down[j])

    h = h[:, N_META:]
    return rms_norm(h, final_norm_g)
```

```python
import numpy as np
import concourse.bass as bass
import concourse.mybir as mybir
from concourse.bass_utils import run_bass_kernel_spmd
from contextlib import ExitStack

F32 = mybir.dt.float32
BF16 = mybir.dt.bfloat16
ALU = mybir.AluOpType
AF = mybir.ActivationFunctionType

NCORES = 8
D = 1024
KC = 8
SEQ = 16384
NMETA = 16
TOK = SEQ // NCORES
HALO = 48
TW = TOK + HALO
DFF = 2816
DFE = 3584
NE = 8
CONVW = 31
RMS_EPS = 1e-6
LN_EPS = 1e-5
POOLW = (2, 4, 8, 16)
CAP = 768
NST = CAP // 128
NSLOT = NE * CAP
U32 = mybir.dt.uint32

TT0 = [(0, 432), (432, 416), (848, 416), (1264, 416), (1680, 416)]
TT1 = [(32, 400)] + TT0[1:]
TT2 = [(48, 384)] + TT0[1:]
TT3 = [(48 + 512 * i, 512) for i in range(4)]

CV = {}
_o = 0
for _n, _w in (("mix_g0", 8), ("mix_g1", 8), ("ffn_g0", 8), ("ffn_g1", 8), ("fin_g", 8),
               ("b_pw1", 16), ("b_dw", 8), ("ln_g", 8), ("ln_b", 8), ("b_pw2", 8),
               ("pool_s", 8), ("w_dw", 8 * CONVW)):
    CV[_n] = _o
    _o += _w
NCV = _o


class Buf:
    __slots__ = ("name", "w", "r")

    def __init__(self, name):
        self.name = name
        self.w = {}
        self.r = {}


class Sched:
    ENGS = ("pe", "act", "dve", "pool", "sp")

    def __init__(self, nc, es):
        self.nc = nc
        self.es = es
        self.eng = {"pe": nc.tensor, "act": nc.scalar, "dve": nc.vector,
                    "pool": nc.gpsimd, "sp": nc.sync}
        self.stream = {e: [] for e in self.ENGS}
        self.sems = {}
        self.cnt = {}
        self.seen = {e: {} for e in self.ENGS}
        for e in self.ENGS:
            self.new_sem(e)

    def new_sem(self, name):
        self.sems[name] = self.es.enter_context(self.nc.semaphore("s_" + name))
        self.cnt[name] = 0
        return name

    def op(self, e, fn, reads=(), writes=(), dma_sem=None, inc=True, after=(), waw=True):
        d = {}

        def add(tok):
            if tok is None:
                return
            s, v = tok
            if d.get(s, 0) < v:
                d[s] = v
        for b in reads:
            for s, v in b.w.items():
                add((s, v))
        for b in writes:
            if waw:
                for s, v in b.w.items():
                    if not (dma_sem is not None and s == dma_sem):
                        add((s, v))
            for s, v in b.r.items():
                add((s, v))
        for tok in after:
            add(tok)
        waits = []
        seen = self.seen[e]
        for s, v in d.items():
            if e == "pe" and s == "pe":
                continue
            if seen.get(s, 0) < v:
                waits.append((s, v))
                seen[s] = v
        if not inc:
            self.stream[e].append((waits, fn, None, 0))
            return None
        if dma_sem is None:
            s, n = e, 1
        else:
            s, n = dma_sem, 16
        self.cnt[s] += n
        tok = (s, self.cnt[s])
        self.stream[e].append((waits, fn, s, n))
        for b in writes:
            if waw:
                b.w = {s: tok[1]}
                b.r = {}
            else:
                b.w[s] = tok[1]
        for b in reads:
            if b.r.get(s, 0) < tok[1]:
                b.r[s] = tok[1]
        return tok

    def barrier(self):
        snap = dict(self.cnt)
        for e in self.ENGS:
            waits = []
            for s, v in snap.items():
                if v == 0 or (e == "pe" and s == "pe"):
                    continue
                if self.seen[e].get(s, 0) < v:
                    waits.append((s, v))
                    self.seen[e][s] = v
            if waits:
                self.stream[e].append((waits, None, None, 0))

    def replay(self, e):
        eng = self.eng[e]
        for waits, fn, s, n in self.stream[e]:
            for ws, wv in waits:
                eng.wait_ge(self.sems[ws], wv)
            if fn is None:
                continue
            ins = fn()
            if s is not None:
                ins.then_inc(self.sems[s], n)

    def run_block(self):
        nc = self.nc
        with nc.Block() as block:
            @block.tensor
            def _(t):
                self.replay("pe")

            @block.scalar
            def _(t):
                self.replay("act")

            @block.vector
            def _(t):
                self.replay("dve")

            @block.gpsimd
            def _(t):
                self.replay("pool")

            @block.sync
            def _(t):
                self.replay("sp")


def build_program(debug=False):
    nc = bass.Bass("TRN2", target_bir_lowering=False)
    dbg_t = [nc.dram_tensor(f"dbg{i}", [D, TW], F32, kind="ExternalOutput").ap() for i in range(4)] if debug else []

    def din(name, shape):
        return nc.dram_tensor(name, list(shape), F32, kind="ExternalInput").ap()

    xT = din("xT", (D, TW))
    umask_d = din("umask", (128, HALO))
    cvec_d = din("cvec", (128, NCV))
    wr_d = din("wr", (128, KC * NE))
    ident_d = din("ident", (128, 128))
    w_pw1 = din("w_pw1", (D, 2 * D))
    w_pw2 = din("w_pw2", (D, D))
    pool_w = din("pool_w", (4, 256, 256))
    ffn_wg = din("ffn_wg", (D, DFF))
    ffn_wu = din("ffn_wu", (D, DFF))
    ffn_wd = din("ffn_wd", (DFF, D))
    moe_wg = din("moe_wg", (NE, D, DFE))
    moe_wu = din("moe_wu", (NE, D, DFE))
    moe_wd = din("moe_wd", (NE, DFE, D))
    tri_d = din("tri", (128, 128))
    gbc_d = din("gbc", (128, D))
    out_d = nc.dram_tensor("out", [TOK, D], F32, kind="ExternalOutput").ap()
    xs_d = nc.dram_tensor("xs_scratch", [NSLOT, D], BF16).ap()
    ys_d = nc.dram_tensor("ys_scratch", [NSLOT, D], F32).ap()

    with ExitStack() as es:
        S = Sched(nc, es)

        def sb(name, shape, dt):
            return es.enter_context(nc.sbuf_tensor("sb_" + name, list(shape), dt))

        H = sb("H", (128, KC, TW), F32)
        B1 = sb("B1", (128, KC, TW), BF16)
        B2 = sb("B2", (128, KC * TW), BF16)
        WP1 = [sb(f"wp1_{b}", (128, KC, 512), BF16) for b in range(2)]
        WP2 = [sb(f"wp2_{b}", (128, KC, 512), BF16) for b in range(2)]
        WP3 = [sb(f"wp3_{b}", (128, 4, 1024), BF16) for b in range(2)]
        R1 = sb("R1", (128, 4096), BF16)
        Z = sb("Z", (128, KC, 416), BF16)
        TMP = [sb(f"tmp{i}", (128, 512), F32) for i in range(5)]
        cvec = sb("cvec", (128, NCV), F32)
        wr = sb("wr", (128, KC, NE), F32)
        wrg = sb("wrg", (128, KC, NE), F32)
        identf = sb("identf", (128, 128), F32)
        identb = sb("identb", (128, 128), BF16)
        onesb = sb("onesb", (128, 128), BF16)
        onesf = sb("onesf", (128, 128), F32)
        umask = sb("umask", (128, HALO), F32)
        ps = [es.enter_context(nc.psum_tensor(f"ps{i}", [128, 512], F32)) for i in range(8)]

        Y = B1[:].bitcast(F32)
        U = B2[:].rearrange("p (k n) -> p k n", k=KC)
        B2f = B2[:].bitcast(F32)
        SQ = R1[:, 0:KC * 432].rearrange("p (k n) -> p k n", k=KC)
        Abuf = [R1[:, i * 2048:(i + 1) * 2048].rearrange("p (m n) -> p m n", m=4) for i in range(2)]
        CWe = [B2f[:, i * 2048:(i + 1) * 2048] for i in range(2)]
        RSall = B2f[:, 6144:8192]
        Zf = Z[:].rearrange("p k n -> p (k n)").bitcast(F32)
        RT = Zf[:, 0:640].rearrange("p (j c) -> p j c", j=16)
        Mf = Zf[:, 768:896].rearrange("p (j e) -> p j e", j=16)
        Mb = Zf[:, 896:960].bitcast(BF16)
        WITHIN = Zf[:, 960:1088]
        OFF = Zf[:, 1088:1216].rearrange("p (j e) -> p j e", j=16)
        SLV = Zf[:, 1216:1344].rearrange("p (j e) -> p j e", j=16)
        S0F = Zf[:, 1344:1360]
        S1F = Zf[:, 1360:1376]
        S0U = Zf[:, 1376:1392].bitcast(U32)
        S1U = Zf[:, 1392:1408].bitcast(U32)
        EC = Zf[:, 1440:1568].rearrange("p (j e) -> p j e", j=16)
        SCR = Zf[:, 1568:1664]
        trib = sb("trib", (128, 128), BF16)
        OUTb = [B2f[:, i * 3456:(i + 1) * 3456].rearrange("p (k n) -> p k n", k=KC) for i in range(2)]
        assert tuple(Y.shape) == (128, KC, TW // 2), Y.shape

        P = [Buf(f"ps{i}") for i in range(8)]
        state = {"bank": 0, "dq": 0, "r1": "sq"}

        def cv(name, j=0):
            c = CV[name] + j
            return cvec[:, c:c + 1]

        def mm_group(pairs, n, reads, rows=128):
            i = state["bank"]
            state["bank"] = (i + 1) % 8
            L = len(pairs)
            for j, (l, r) in enumerate(pairs):
                edge = (j == 0 or j == L - 1)
                S.op("pe",
                     (lambda l=l, r=r, j=j, i=i: nc.tensor.matmul(
                         ps[i][:rows, :n], lhsT=l, rhs=r, start=(j == 0), stop=(j == L - 1))),
                     reads=reads if edge else (), writes=[P[i]] if edge else (),
                     inc=(j == L - 1))
            return i

        Bc = Buf("consts")
        sc = S.new_sem("dconst")
        trif = TMP[3][:, 0:128]
        for dst, src in ((cvec[:], cvec_d), (wr[:].rearrange("p k e -> p (k e)"), wr_d),
                         (identf[:], ident_d), (umask[:], umask_d), (trif, tri_d)):
            S.op("sp", (lambda dst=dst, src=src: nc.sync.dma_start(out=dst, in_=src)),
                 writes=[Bc], dma_sem=sc)
        S.op("dve", lambda: nc.vector.memset(onesb[:], 1.0), writes=[Bc])
        S.op("dve", lambda: nc.vector.memset(onesf[:], 1.0), writes=[Bc])
        S.op("dve", lambda: nc.vector.tensor_copy(out=identb[:], in_=identf[:]), reads=[Bc], writes=[Bc])
        for k in range(KC):
            S.op("dve", (lambda k=k: nc.vector.tensor_scalar(
                out=wrg[:, k, :], in0=wr[:, k, :], scalar1=cv("ffn_g1", k), scalar2=None, op0=ALU.mult)),
                reads=[Bc], writes=[Bc])

        Hb = [Buf(f"H{t}") for t in range(5)]
        sx = S.new_sem("dx")
        dbg_sem = S.new_sem("ddbg") if debug else None

        def dump(i):
            if not debug:
                return
            S.barrier()
            for k in range(KC):
                S.op("sp", (lambda k=k, i=i: nc.sync.dma_start(out=dbg_t[i][k * 128:(k + 1) * 128, :], in_=H[:, k, :])),
                     reads=Hb, dma_sem=dbg_sem)
            S.barrier()
        xTv = xT.rearrange("(k p) n -> p k n", p=128)
        sxs = [sx] + [S.new_sem(f"dx{t}") for t in range(1, 5)]
        for t, (o, n) in enumerate(TT0):
            S.op("sp", (lambda o=o, n=n: nc.sync.dma_start(out=H[:, :, o:o + n], in_=xTv[:, :, o:o + n])),
                 writes=[Hb[t]], dma_sem=sxs[t])

        bnd = {}

        def _mk_bound():
            reg = nc.gpsimd.alloc_register("slot_bound")
            ins = nc.gpsimd.reg_mov(reg, NSLOT - 1)
            bnd["v"] = nc.gpsimd.snap(reg)
            return ins
        S.op("pool", _mk_bound, inc=False)

        XS0b = Buf("xs0")
        ZTb = Buf("zt")
        ZT = TMP[4][:, :].bitcast(BF16)
        szf = S.new_sem("dzf")
        S.op("dve", lambda: nc.vector.memset(TMP[4][:, :], 0.0), writes=[ZTb])
        for q in range(NSLOT // 128):
            S.op("sp", (lambda q=q: nc.sync.dma_start(out=xs_d[q * 128:(q + 1) * 128, :], in_=ZT)),
                 reads=[ZTb], writes=[XS0b], dma_sem=szf)
        S.op("dve", lambda: nc.vector.tensor_copy(out=trib[:], in_=trif[:]), reads=[Bc], writes=[Bc])

        WBb = [Buf("wb0"), Buf("wb1")]
        wsem = [S.new_sem("dw0"), S.new_sem("dw1")]

        def wload(b, parts):
            for dst, src in parts:
                S.op("pool", (lambda dst=dst, src=src: nc.gpsimd.dma_start(out=dst, in_=src)),
                     writes=[WBb[b]], dma_sem=wsem[b])

        def kview(ap):
            return ap.rearrange("(k p) n -> p k n", p=128)

        SQb, RSb = Buf("sq"), Buf("rs")

        def rmsnorm(tiles, tbufs_in, gname, out_fn, out_bufs, eps=RMS_EPS, rs_fn=None, rs_buf=None):
            if state["r1"] != "sq":
                S.barrier()
                state["r1"] = "sq"
            for ti, (o, n) in enumerate(tiles):
                hb = tbufs_in[ti]
                rsap = (lambda o=o, n=n: TMP[0][:, :n]) if rs_fn is None else (lambda o=o, n=n: rs_fn(o, n))
                rsb = RSb if rs_buf is None else rs_buf
                S.op("act", (lambda o=o, n=n: nc.scalar.activation(
                    out=SQ[:, :, :n], in_=H[:, :, o:o + n], func=AF.Square)),
                    reads=[hb], writes=[SQb])
                i = mm_group([(onesb[:], SQ[:, k, :n]) for k in range(KC)], n, [Bc, SQb])
                S.op("act", (lambda i=i, n=n, rsap=rsap: nc.scalar.activation(
                    out=rsap(), in_=ps[i][:, :n], func=AF.Sqrt, bias=eps_ap(eps), scale=1.0 / D)),
                    reads=[P[i], Bc], writes=[rsb])
                S.op("dve", (lambda rsap=rsap: nc.vector.reciprocal(out=rsap(), in_=rsap())),
                     reads=[rsb], writes=[rsb])
                for k in range(KC):
                    S.op("dve", (lambda k=k, o=o, n=n, rsap=rsap: nc.vector.scalar_tensor_tensor(
                        out=out_fn(k, o, n), in0=H[:, k, o:o + n], scalar=cv(gname, k),
                        in1=rsap(), op0=ALU.mult, op1=ALU.mult)),
                        reads=[hb, rsb, Bc], writes=[out_bufs[ti]])

        epsc = sb("epsc", (128, 2), F32)
        S.op("dve", lambda: nc.vector.memset(epsc[:, 0:1], RMS_EPS), writes=[Bc])
        S.op("dve", lambda: nc.vector.memset(epsc[:, 1:2], LN_EPS), writes=[Bc])

        def eps_ap(eps):
            return epsc[:, 0:1] if eps == RMS_EPS else epsc[:, 1:2]

        HNb = [Buf(f"HN{t}") for t in range(5)]
        for s in range(2):
            wload(s, [(WP1[s][:], kview(w_pw1[:, 512 * s:512 * s + 512])),
                      (WP2[s][:], kview(w_pw1[:, D + 512 * s:D + 512 * s + 512]))])
        rmsnorm(TT0, Hb, "mix_g0", lambda k, o, n: B1[:, k, o:o + n], HNb)

        Ub = Buf("U")
        SIGb = [Buf("sig0"), Buf("sig1")]
        sgi = 0
        for s in range(2):
            for ti, (o, n) in enumerate(TT0):
                for mc in range(4):
                    ch = 4 * s + mc
                    ia = mm_group([(WP1[s][:, k, mc * 128:(mc + 1) * 128], B1[:, k, o:o + n]) for k in range(KC)],
                                  n, [WBb[s], HNb[ti]])
                    ig = mm_group([(WP2[s][:, k, mc * 128:(mc + 1) * 128], B1[:, k, o:o + n]) for k in range(KC)],
                                  n, [WBb[s], HNb[ti]])
                    sg = sgi % 2
                    sgi += 1
                    S.op("act", (lambda ig=ig, n=n, ch=ch, sg=sg: nc.scalar.activation(
                        out=TMP[1 + sg][:, :n], in_=ps[ig][:, :n], func=AF.Sigmoid, bias=cv("b_pw1", 8 + ch))),
                        reads=[P[ig], Bc], writes=[SIGb[sg]])
                    S.op("dve", (lambda ia=ia, n=n, ch=ch, sg=sg, o=o: nc.vector.scalar_tensor_tensor(
                        out=U[:, ch, o:o + n], in0=ps[ia][:, :n], scalar=cv("b_pw1", ch),
                        in1=TMP[1 + sg][:, :n], op0=ALU.add, op1=ALU.mult)),
                        reads=[P[ia], SIGb[sg], Bc], writes=[Ub])
        for k in range(KC):
            S.op("dve", (lambda k=k: nc.vector.tensor_tensor(
                out=U[:, k, 0:HALO], in0=U[:, k, 0:HALO], in1=umask[:], op=ALU.mult)),
                reads=[Ub, Bc], writes=[Ub])

        S.barrier()

        W2b = [Buf("w2_0"), Buf("w2_1")]
        w2sem = [S.new_sem("dw2_0"), S.new_sem("dw2_1")]
        for s in range(2):
            S.op("pool", (lambda s=s: nc.gpsimd.dma_start(
                out=WP1[s][:], in_=kview(w_pw2[:, 512 * s:512 * s + 512]))),
                writes=[W2b[s]], dma_sem=w2sem[s])

        DGb = [Buf("dg0"), Buf("dg1")]
        Yb = Buf("Y")
        Zb = Buf("Z")
        MUb, M2b = Buf("mu"), ZTb
        groups = [[0, 1], [2, 3], [4]]
        dgi = 0
        for grp in groups:
            gbase = TT1[grp[0]][0]
            for c in range(KC):
                db = dgi % 2
                dgi += 1
                dg = WP3[db][:].rearrange("p a b -> p (a b)")
                for k in range(CONVW):
                    wcol = cvec[:, CV["w_dw"] + c * CONVW + k: CV["w_dw"] + c * CONVW + k + 1]
                    if k % 3 == 0:
                        S.op("pool", (lambda k=k, dg=dg, wcol=wcol: nc.gpsimd.tensor_scalar(
                            out=dg[:, k * 128:(k + 1) * 128], in0=identf[:], scalar1=wcol,
                            scalar2=0.0, op0=ALU.mult, op1=ALU.add)),
                            reads=[Bc], writes=[DGb[db]], waw=False)
                    else:
                        S.op("dve", (lambda k=k, dg=dg, wcol=wcol: nc.vector.tensor_scalar(
                            out=dg[:, k * 128:(k + 1) * 128], in0=identf[:], scalar1=wcol,
                            scalar2=None, op0=ALU.mult)),
                            reads=[Bc], writes=[DGb[db]], waw=False)
                for ti in grp:
                    o, n = TT1[ti]
                    i = mm_group([(dg[:, k * 128:(k + 1) * 128], U[:, c, o - 30 + k: o - 30 + k + n])
                                  for k in range(CONVW)], n, [DGb[db], Ub])
                    S.op("act", (lambda i=i, n=n, c=c, yo=o - gbase: nc.scalar.activation(
                        out=Y[:, c, yo:yo + n], in_=ps[i][:, :n], func=AF.Identity, bias=cv("b_dw", c))),
                        reads=[P[i], Bc], writes=[Yb])
            for ti in grp:
                o, n = TT1[ti]
                yo = o - gbase
                imu = mm_group([(onesf[:], Y[:, k, yo:yo + n]) for k in range(KC)], n, [Bc, Yb])
                S.op("act", (lambda yo=yo, n=n: nc.scalar.activation(
                    out=SQ[:, :, :n], in_=Y[:, :, yo:yo + n], func=AF.Square)),
                    reads=[Yb], writes=[SQb])
                isq = mm_group([(onesb[:], SQ[:, k, :n]) for k in range(KC)], n, [Bc, SQb])
                S.op("dve", (lambda imu=imu, n=n: nc.vector.tensor_scalar(
                    out=TMP[3][:, :n], in0=ps[imu][:, :n], scalar1=1.0 / D, scalar2=None, op0=ALU.mult)),
                    reads=[P[imu]], writes=[MUb])
                S.op("dve", (lambda n=n: nc.vector.tensor_tensor(
                    out=TMP[4][:, :n], in0=TMP[3][:, :n], in1=TMP[3][:, :n], op=ALU.mult)),
                    reads=[MUb], writes=[M2b])
                S.op("dve", (lambda isq=isq, n=n: nc.vector.scalar_tensor_tensor(
                    out=TMP[4][:, :n], in0=ps[isq][:, :n], scalar=1.0 / D, in1=TMP[4][:, :n],
                    op0=ALU.mult, op1=ALU.subtract)),
                    reads=[P[isq], M2b], writes=[M2b])
                S.op("act", (lambda n=n: nc.scalar.activation(
                    out=TMP[0][:, :n], in_=TMP[4][:, :n], func=AF.Sqrt, bias=eps_ap(LN_EPS), scale=1.0)),
                    reads=[M2b, Bc], writes=[RSb])
                S.op("dve", (lambda n=n: nc.vector.reciprocal(out=TMP[0][:, :n], in_=TMP[0][:, :n])),
                     reads=[RSb], writes=[RSb])
                for c in range(KC):
                    S.op("dve", (lambda c=c, yo=yo, n=n: nc.vector.tensor_tensor(
                        out=Y[:, c, yo:yo + n], in0=Y[:, c, yo:yo + n], in1=TMP[3][:, :n], op=ALU.subtract)),
                        reads=[Yb, MUb], writes=[Yb])
                    S.op("dve", (lambda c=c, yo=yo, n=n: nc.vector.tensor_tensor(
                        out=Y[:, c, yo:yo + n], in0=Y[:, c, yo:yo + n], in1=TMP[0][:, :n], op=ALU.mult)),
                        reads=[Yb, RSb], writes=[Yb])
                    S.op("act", (lambda c=c, yo=yo, n=n: nc.scalar.activation(
                        out=Z[:, c, :n], in_=Y[:, c, yo:yo + n], func=AF.Silu,
                        bias=cv("ln_b", c), scale=cv("ln_g", c))),
                        reads=[Yb, Bc], writes=[Zb])
                for oc in range(KC):
                    s2, mc = divmod(oc, 4)
                    i = mm_group([(WP1[s2][:, k, mc * 128:(mc + 1) * 128], Z[:, k, :n]) for k in range(KC)],
                                 n, [W2b[s2], Zb])
                    S.op("dve", (lambda i=i, oc=oc, o=o, n=n: nc.vector.scalar_tensor_tensor(
                        out=H[:, oc, o:o + n], in0=ps[i][:, :n], scalar=cv("b_pw2", oc),
                        in1=H[:, oc, o:o + n], op0=ALU.add, op1=ALU.add)),
                        reads=[P[i], Bc, Hb[ti]], writes=[Hb[ti]])

        S.barrier()
        dump(0)

        SGb = [Buf("sg0"), Buf("sg1")]
        T2b = [Buf("t2a"), Buf("t2b")]
        Ab = [Buf("A0"), Buf("A1")]
        ffn_state = {"sg": 0, "a": 0, "slab": 0}

        def ffn(slabs, tiles, hnb, hb, cw=None):
            st = ffn_state
            if state["r1"] != "a":
                S.barrier()
                state["r1"] = "a"
            b0 = st["slab"] % 2
            wload(b0, slabs[0][1])
            for si, (wdt, _) in enumerate(slabs):
                b = (st["slab"] + si) % 2
                if si + 1 < len(slabs):
                    wload(1 - b, slabs[si + 1][1])
                nm = wdt // 128
                for ti, (o, n) in enumerate(tiles):
                    a = st["a"] % 2
                    st["a"] += 1
                    for mc in range(nm):
                        ig = mm_group([(WP1[b][:, k, mc * 128:(mc + 1) * 128], B1[:, k, o:o + n]) for k in range(KC)],
                                      n, [WBb[b], hnb[ti]])
                        iu = mm_group([(WP2[b][:, k, mc * 128:(mc + 1) * 128], B1[:, k, o:o + n]) for k in range(KC)],
                                      n, [WBb[b], hnb[ti]])
                        sg = st["sg"] % 2
                        st["sg"] += 1
                        S.op("act", (lambda ig=ig, n=n, sg=sg: nc.scalar.activation(
                            out=TMP[1 + sg][:, :n], in_=ps[ig][:, :n], func=AF.Silu)),
                            reads=[P[ig]], writes=[SGb[sg]])
                        if cw is None:
                            S.op("dve", (lambda iu=iu, n=n, sg=sg, a=a, mc=mc: nc.vector.tensor_tensor(
                                out=Abuf[a][:, mc, :n], in0=ps[iu][:, :n], in1=TMP[1 + sg][:, :n], op=ALU.mult)),
                                reads=[P[iu], SGb[sg]], writes=[Ab[a]])
                        else:
                            cwfn, cwb = cw
                            S.op("dve", (lambda n=n, sg=sg, o=o: nc.vector.tensor_tensor(
                                out=TMP[3 + sg][:, :n], in0=TMP[1 + sg][:, :n], in1=cwfn(o, n), op=ALU.mult)),
                                reads=[SGb[sg], cwb], writes=[T2b[sg]])
                            S.op("dve", (lambda iu=iu, n=n, sg=sg, a=a, mc=mc: nc.vector.tensor_tensor(
                                out=Abuf[a][:, mc, :n], in0=ps[iu][:, :n], in1=TMP[3 + sg][:, :n], op=ALU.mult)),
                                reads=[P[iu], T2b[sg]], writes=[Ab[a]])
                    for oc in range(KC):
                        i = mm_group([(WP3[b][:, mc, oc * 128:(oc + 1) * 128], Abuf[a][:, mc, :n]) for mc in range(nm)],
                                     n, [WBb[b], Ab[a]])
                        S.op("dve", (lambda i=i, oc=oc, o=o, n=n: nc.vector.tensor_tensor(
                            out=H[:, oc, o:o + n], in0=ps[i][:, :n], in1=H[:, oc, o:o + n], op=ALU.add)),
                            reads=[P[i], hb[ti]], writes=[hb[ti]])
            st["slab"] += len(slabs)

        def ffn_slabs(wg, wu, wd, dff):
            out = []
            off = 0
            while off < dff:
                wdt = min(512, dff - off)
                nm = wdt // 128
                out.append((wdt, off))
                off += wdt
            res = []
            for wdt, off in out:
                nm = wdt // 128

                def mk(b, wdt=wdt, off=off, nm=nm):
                    return [(WP1[b][:, :, :wdt], kview(wg[:, off:off + wdt])),
                            (WP2[b][:, :, :wdt], kview(wu[:, off:off + wdt])),
                            (WP3[b][:, :nm, :], wd[off:off + wdt, :].rearrange("(m p) n -> p m n", p=128))]
                res.append((wdt, mk))
            return res

        def run_ffn(wg, wu, wd, dff, tiles, hnb, hb, cw=None):
            sl = ffn_slabs(wg, wu, wd, dff)
            base = ffn_state["slab"]
            slabs = [(wdt, mk((base + si) % 2)) for si, (wdt, mk) in enumerate(sl)]
            ffn(slabs, tiles, hnb, hb, cw)

        rmsnorm(TT1, Hb, "ffn_g0", lambda k, o, n: B1[:, k, o:o + n], HNb)
        run_ffn(ffn_wg, ffn_wu, ffn_wd, DFF, TT1, HNb, Hb)

        dump(1)
        spw = S.new_sem("dpw")
        PW = WP3[0][:].rearrange("p a b -> p (a b)")[:, 0:2048].rearrange("p (g k n) -> p g k n", g=4, k=2)
        for g in range(4):
            S.op("pool", (lambda g=g: nc.gpsimd.dma_start(
                out=PW[:, g, :, :], in_=pool_w[g].rearrange("(k p) n -> p k n", p=128))),
                writes=[WBb[0]], dma_sem=spw)
        rmsnorm(TT1, Hb, "mix_g1", lambda k, o, n: B1[:, k, o:o + n], HNb)
        W1f = WP1[0][:].rearrange("p a b -> p (a b)")
        PWS = W1f[:, 0:2048].rearrange("p (g k n) -> p g k n", g=4, k=2)
        PWN = W1f[:, 2048:4096].rearrange("p (g k n) -> p g k n", g=4, k=2)
        for g in range(4):
            S.op("dve", (lambda g=g: nc.vector.tensor_scalar(
                out=PWS[:, g, :, :], in0=PW[:, g, :, :], scalar1=1.0 / POOLW[g], scalar2=None, op0=ALU.mult)),
                reads=[WBb[0]], writes=[WBb[0]])
        S.op("dve", lambda: nc.vector.tensor_scalar(
            out=W1f[:, 2048:4096], in0=WP3[0][:].rearrange("p a b -> p (a b)")[:, 0:2048],
            scalar1=-1.0, scalar2=None, op0=ALU.mult), reads=[WBb[0]], writes=[WBb[0]])
        for ti, (o, n) in enumerate(TT2):
            rd = [HNb[ti]] + ([HNb[ti - 1]] if ti > 0 else [])
            for g in range(4):
                w = POOLW[g]
                for oc in range(2):
                    ch = 2 * g + oc
                    pairs = []
                    for kc in range(2):
                        for dd in range(w):
                            pairs.append((PWS[:, g, kc, oc * 128:(oc + 1) * 128], B1[:, 2 * g + kc, o - dd:o - dd + n]))
                        pairs.append((PWN[:, g, kc, oc * 128:(oc + 1) * 128], B1[:, 2 * g + kc, o:o + n]))
                    i = mm_group(pairs, n, [WBb[0]] + rd)
                    S.op("dve", (lambda i=i, ch=ch, o=o, n=n: nc.vector.scalar_tensor_tensor(
                        out=H[:, ch, o:o + n], in0=ps[i][:, :n], scalar=cv("pool_s", ch), in1=H[:, ch, o:o + n],
                        op0=ALU.mult, op1=ALU.add)),
                        reads=[P[i], Hb[ti], Bc], writes=[Hb[ti]])

        dump(2)
        RSAb = Buf("rsall")
        rmsnorm(TT2, Hb, "ffn_g1", lambda k, o, n: B1[:, k, o:o + n], HNb,
                rs_fn=lambda o, n: RSall[:, o - HALO:o - HALO + n], rs_buf=RSAb)
        RTb = [Buf("rt")] * 16
        rb = RTb[0]
        ir = state["bank"]
        state["bank"] = (ir + 1) % 8
        for j in range(16):
            c0 = HALO + 128 * j
            for k in range(KC):
                first = (j == 0 and k == 0)
                S.op("pe", (lambda j=j, k=k, c0=c0: nc.tensor.matmul(
                    ps[ir][:, 8 * j:8 * j + 8], lhsT=H[:, k, c0:c0 + 128], rhs=wrg[:, k, :],
                    start=(k == 0), stop=(k == KC - 1))),
                    reads=Hb + [Bc] if first else (), writes=[P[ir]] if first else (), inc=False)
            last = (j == 15)
            S.op("pe", (lambda j=j: nc.tensor.matmul(
                ps[ir][:, 128 + 2 * j:128 + 2 * j + 2], lhsT=RSall[:, 128 * j:128 * j + 128], rhs=identf[:, 0:2],
                start=True, stop=True)),
                reads=[RSAb, Bc] + Hb if (j == 0 or last) else (), writes=[P[ir]] if last else (), inc=last)
        RAW = RT[:, :, 0:8]
        S.op("dve", lambda: nc.vector.tensor_copy(
            out=RAW, in_=ps[ir][:, 0:128].rearrange("p (j e) -> p j e", j=16)), reads=[P[ir]], writes=[rb])
        S.op("dve", lambda: nc.vector.tensor_copy(
            out=RT[:, :, 8], in_=ps[ir][:, 128:160].rearrange("p (j c) -> p j c", j=16)[:, :, 0]),
            reads=[P[ir]], writes=[rb])
        S.op("dve", lambda: nc.vector.tensor_reduce(out=RT[:, :, 9], in_=RAW, axis=mybir.AxisListType.X, op=ALU.max),
             reads=[rb], writes=[rb])
        for e in range(NE):
            S.op("dve", (lambda e=e: nc.vector.tensor_tensor(
                out=RT[:, :, 16 + e], in0=RT[:, :, e], in1=RT[:, :, 9], op=ALU.is_equal)), reads=[rb], writes=[rb])
        S.op("dve", lambda: nc.vector.scalar_tensor_tensor(
            out=RT[:, :, 24:32], in0=RT[:, :, 16:24], scalar=-1.0e30, in1=RAW, op0=ALU.mult, op1=ALU.add),
            reads=[rb], writes=[rb])
        S.op("dve", lambda: nc.vector.tensor_reduce(out=RT[:, :, 10], in_=RT[:, :, 24:32], axis=mybir.AxisListType.X,
                                                    op=ALU.max), reads=[rb], writes=[rb])
        for e in range(NE):
            S.op("dve", (lambda e=e: nc.vector.tensor_tensor(
                out=RT[:, :, 32 + e], in0=RT[:, :, 24 + e], in1=RT[:, :, 10], op=ALU.is_equal)), reads=[rb], writes=[rb])
        S.op("dve", lambda: nc.vector.tensor_tensor(out=RT[:, :, 11], in0=RT[:, :, 9], in1=RT[:, :, 10], op=ALU.subtract),
             reads=[rb], writes=[rb])
        S.op("dve", lambda: nc.vector.tensor_tensor(out=RT[:, :, 11], in0=RT[:, :, 11], in1=RT[:, :, 8], op=ALU.mult),
             reads=[rb], writes=[rb])
        S.op("act", lambda: nc.scalar.activation(out=RT[:, :, 12], in_=RT[:, :, 11], func=AF.Sigmoid),
             reads=[rb], writes=[rb])
        S.op("dve", lambda: nc.vector.tensor_scalar(out=RT[:, :, 13], in0=RT[:, :, 12], scalar1=-1.0, scalar2=1.0,
                                                    op0=ALU.mult, op1=ALU.add), reads=[rb], writes=[rb])

        IDXb = Buf("idx")
        allrt = RTb
        S.op("dve", lambda: nc.vector.tensor_tensor(out=Mf[:], in0=RT[:, :, 16:24], in1=RT[:, :, 32:40], op=ALU.add),
             reads=allrt, writes=[IDXb])
        S.op("dve", lambda: nc.vector.tensor_copy(out=Mb, in_=Mf[:].rearrange("p j e -> p (j e)")),
             reads=[IDXb], writes=[IDXb])
        iw = mm_group([(trib[:], Mb)], 128, [Bc, IDXb])
        ic = mm_group([(onesb[:], Mb)], 128, [Bc, IDXb])
        S.op("dve", lambda: nc.vector.memset(OFF[:, 0, :], 0.0), reads=[IDXb], writes=[IDXb])
        for j in range(1, 16):
            S.op("dve", (lambda j=j: nc.vector.tensor_tensor(
                out=OFF[:, j, :], in0=OFF[:, j - 1, :], in1=ps[ic][:, (j - 1) * NE:j * NE], op=ALU.add)),
                reads=[IDXb, P[ic]], writes=[IDXb])
        for e in range(NE):
            S.op("dve", (lambda e=e: nc.vector.memset(EC[:, :, e:e + 1], float(e * CAP))), reads=[IDXb], writes=[IDXb])
        Offl = OFF.rearrange("p j e -> p (j e)")
        SLVl = SLV.rearrange("p j e -> p (j e)")
        ECl = EC.rearrange("p j e -> p (j e)")
        S.op("dve", lambda: nc.vector.tensor_tensor(out=Offl, in0=Offl, in1=ps[iw][:, 0:128], op=ALU.add),
             reads=[IDXb, P[iw]], writes=[IDXb])
        S.op("dve", lambda: nc.vector.tensor_tensor(out=SLVl, in0=Offl, in1=ECl, op=ALU.add), reads=[IDXb], writes=[IDXb])
        S.op("dve", lambda: nc.vector.tensor_scalar(out=Offl, in0=Offl, scalar1=float(CAP), scalar2=1.0e6,
                                                    op0=ALU.is_ge, op1=ALU.mult), reads=[IDXb], writes=[IDXb])
        S.op("dve", lambda: nc.vector.tensor_tensor(out=SLVl, in0=SLVl, in1=Offl, op=ALU.add), reads=[IDXb], writes=[IDXb])
        for (msk, SF, SU, wc) in ((RT[:, :, 16:24], S0F, S0U, 12), (RT[:, :, 32:40], S1F, S1U, 13)):
            S.op("dve", (lambda msk=msk: nc.vector.tensor_tensor(out=Mf[:], in0=msk, in1=SLV, op=ALU.mult)),
                 reads=[IDXb] + allrt, writes=[IDXb])
            S.op("dve", (lambda SF=SF: nc.vector.tensor_reduce(out=SF, in_=Mf[:], axis=mybir.AxisListType.X, op=ALU.add)),
                 reads=[IDXb], writes=[IDXb])
            S.op("dve", (lambda SF=SF, SU=SU: nc.vector.tensor_copy(out=SU, in_=SF)), reads=[IDXb], writes=[IDXb])
            S.op("dve", (lambda SF=SF: nc.vector.tensor_scalar(out=SF, in0=SF, scalar1=float(NSLOT), scalar2=None,
                                                               op0=ALU.is_lt)), reads=[IDXb], writes=[IDXb])
            S.op("dve", (lambda SF=SF, wc=wc: nc.vector.tensor_tensor(out=RT[:, :, wc], in0=RT[:, :, wc], in1=SF, op=ALU.mult)),
                 reads=[IDXb] + allrt, writes=allrt)

        all_slabs = [(e, sl) for e in range(NE) for sl in range(DFE // 512)]

        def slab_parts(e, sl, b):
            off = 512 * sl
            return [(WP1[b][:], kview(moe_wg[e][:, off:off + 512])),
                    (WP2[b][:], kview(moe_wu[e][:, off:off + 512])),
                    (WP3[b][:], moe_wd[e][off:off + 512, :].rearrange("(m p) n -> p m n", p=128))]
        sbase = ffn_state["slab"]
        wload(sbase % 2, slab_parts(0, 0, sbase % 2))

        HT = [B2[:, r * 1024:(r + 1) * 1024] for r in range(4)]
        HTb = [Buf(f"ht{r}") for r in range(4)]
        scs = [S.new_sem(f"dsc{r}") for r in range(4)]
        XSb = Buf("xs")
        XSb.w = dict(XS0b.w)
        prev_sc = []
        for j in range(16):
            c0 = HALO + 128 * j
            r = j % 4
            for half in range(2):
                i = state["bank"]
                state["bank"] = (i + 1) % 8
                for kk in range(4):
                    S.op("pe", (lambda i=i, kk=kk, half=half, c0=c0: nc.tensor.matmul(
                        ps[i][:, kk * 128:(kk + 1) * 128], lhsT=B1[:, 4 * half + kk, c0:c0 + 128], rhs=identb[:],
                        start=True, stop=True)),
                        reads=HNb + [Bc] if kk in (0, 3) else (), writes=[P[i]] if kk in (0, 3) else (), inc=(kk == 3))
                if half == 0:
                    S.op("act", (lambda i=i, r=r: nc.scalar.activation(out=HT[r][:, 0:512], in_=ps[i][:, :], func=AF.Identity)),
                         reads=[P[i]], writes=[HTb[r]], waw=False)
                else:
                    S.op("dve", (lambda i=i, r=r: nc.vector.tensor_copy(out=HT[r][:, 512:1024], in_=ps[i][:, :])),
                         reads=[P[i]], writes=[HTb[r]], waw=False)
            for SU in (S0U, S1U):
                S.op("pool", (lambda SU=SU, j=j, r=r: nc.gpsimd.indirect_dma_start(
                    out=xs_d, out_offset=bass.IndirectOffsetOnAxis(SU[:, j:j + 1], 0), in_=HT[r], in_offset=None,
                    bounds_check=bnd["v"], oob_is_err=False)),
                    reads=[HTb[r], IDXb], writes=[XSb], dma_sem=scs[r], waw=False, after=list(prev_sc))
                prev_sc[:] = [(scs[r], S.cnt[scs[r]])]

        if state["r1"] != "a":
            S.barrier()
            state["r1"] = "a"
        XT = [B1[:].rearrange("p k n -> p (k n)")[:, 12288 + r * 1024: 12288 + (r + 1) * 1024] for r in range(3)]
        XTb = [Buf(f"xt{r}") for r in range(3)]
        xts = [S.new_sem(f"dxt{r}") for r in range(3)]
        XG = [B1[:].rearrange("p k n -> p (k n)")[:, g * 6144:(g + 1) * 6144].rearrange("p (k n) -> p k n", k=KC)
              for g in range(2)]
        XGb = [Buf("xg0"), Buf("xg1")]
        YA = B2f[:, 0:NST * 1024].rearrange("p (s n) -> p s n", s=NST)
        YAb = [Buf(f"ya{st}") for st in range(NST)]
        YSDb = Buf("ysd")
        yst = [S.new_sem("dys0"), S.new_sem("dys1")]
        xti = 0
        evi = 0
        gtiles = [(0, CAP // 2), (CAP // 2, CAP // 2)]
        nst_t = (CAP // 2) // 128
        def emit_xload(e):
            nonlocal xti, evi
            g = e % 2
            for st in range(NST):
                xr = xti % 3
                xti += 1
                S.op("sp", (lambda e=e, st=st, xr=xr: nc.sync.dma_start(
                    out=XT[xr], in_=xs_d[e * CAP + st * 128: e * CAP + (st + 1) * 128, :])),
                    reads=[XSb], writes=[XTb[xr]], dma_sem=xts[xr])
                for half in range(2):
                    i = state["bank"]
                    state["bank"] = (i + 1) % 8
                    for kk in range(4):
                        S.op("pe", (lambda i=i, kk=kk, half=half, xr=xr: nc.tensor.matmul(
                            ps[i][:, kk * 128:(kk + 1) * 128],
                            lhsT=XT[xr][:, (4 * half + kk) * 128:(4 * half + kk + 1) * 128], rhs=identb[:],
                            start=True, stop=True)),
                            reads=[XTb[xr], Bc] if kk in (0, 3) else (), writes=[P[i]] if kk in (0, 3) else (),
                            inc=(kk == 3))
                    dst = XG[g][:, 4 * half:4 * half + 4, st * 128:(st + 1) * 128]
                    srcv = ps[i][:, :].rearrange("p (a b) -> p a b", a=4)
                    evi += 1
                    if evi % 2:
                        S.op("act", (lambda dst=dst, srcv=srcv: nc.scalar.activation(out=dst, in_=srcv, func=AF.Identity)),
                             reads=[P[i]], writes=[XGb[g]], waw=False)
                    else:
                        S.op("dve", (lambda dst=dst, srcv=srcv: nc.vector.tensor_copy(out=dst, in_=srcv)),
                             reads=[P[i]], writes=[XGb[g]], waw=False)
        emit_xload(0)
        for si, (e, sl) in enumerate(all_slabs):
            b = (sbase + si) % 2
            if si + 1 < len(all_slabs):
                wload(1 - b, slab_parts(all_slabs[si + 1][0], all_slabs[si + 1][1], 1 - b))
            g = e % 2
            if sl == 2 and e + 1 < NE:
                emit_xload(e + 1)
            for ti, (o, n) in enumerate(gtiles):
                a = ffn_state["a"] % 2
                ffn_state["a"] += 1
                for mc in range(4):
                    ig = mm_group([(WP1[b][:, k, mc * 128:(mc + 1) * 128], XG[g][:, k, o:o + n]) for k in range(KC)],
                                  n, [WBb[b], XGb[g]])
                    iu = mm_group([(WP2[b][:, k, mc * 128:(mc + 1) * 128], XG[g][:, k, o:o + n]) for k in range(KC)],
                                  n, [WBb[b], XGb[g]])
                    sg = ffn_state["sg"] % 2
                    ffn_state["sg"] += 1
                    S.op("act", (lambda ig=ig, n=n, sg=sg: nc.scalar.activation(
                        out=TMP[1 + sg][:, :n], in_=ps[ig][:, :n], func=AF.Silu)),
                        reads=[P[ig]], writes=[SGb[sg]])
                    S.op("dve", (lambda iu=iu, n=n, sg=sg, a=a, mc=mc: nc.vector.tensor_tensor(
                        out=Abuf[a][:, mc, :n], in0=ps[iu][:, :n], in1=TMP[1 + sg][:, :n], op=ALU.mult)),
                        reads=[P[iu], SGb[sg]], writes=[Ab[a]])
                for s3 in range(nst_t):
                    st = ti * nst_t + s3
                    for half in range(2):
                        i = mm_group([(Abuf[a][:, mc, s3 * 128:(s3 + 1) * 128], WP3[b][:, mc, half * 512:(half + 1) * 512])
                                      for mc in range(4)], 512, [WBb[b], Ab[a]])
                        if sl == 0:
                            S.op("dve", (lambda i=i, st=st, half=half: nc.vector.tensor_copy(
                                out=YA[:, st, half * 512:(half + 1) * 512], in_=ps[i][:, :])),
                                reads=[P[i]], writes=[YAb[st]], waw=(half == 0))
                        else:
                            S.op("dve", (lambda i=i, st=st, half=half: nc.vector.tensor_tensor(
                                out=YA[:, st, half * 512:(half + 1) * 512], in0=ps[i][:, :],
                                in1=YA[:, st, half * 512:(half + 1) * 512], op=ALU.add)),
                                reads=[P[i], YAb[st]], writes=[YAb[st]])
                if sl == DFE // 512 - 1:
                    for s3 in range(nst_t):
                        st = ti * nst_t + s3
                        S.op("sp", (lambda e=e, st=st: nc.sync.dma_start(
                            out=ys_d[e * CAP + st * 128: e * CAP + (st + 1) * 128, :], in_=YA[:, st, :])),
                            reads=[YAb[st]], writes=[YSDb], dma_sem=yst[ti], waw=False)
                    for s3 in range(nst_t):
                        YAb[ti * nst_t + s3].r[yst[ti]] = S.cnt[yst[ti]]
        ffn_state["slab"] += len(all_slabs)

        B1l = B1[:].rearrange("p k n -> p (k n)").bitcast(F32)
        G0 = [B1l[:, r * 1024:(r + 1) * 1024] for r in range(2)]
        G1 = [B1l[:, 2048 + r * 1024: 2048 + (r + 1) * 1024] for r in range(2)]
        RR = [B1l[:, 4096 + r * 1024: 4096 + (r + 1) * 1024] for r in range(2)]
        GBC = B1l[:, 6144:7168]
        G0b = [Buf("g00"), Buf("g01")]
        G1b = [Buf("g10"), Buf("g11")]
        RRb = [Buf("rr0"), Buf("rr1")]
        GBCb = Buf("gbc")
        gsm = [scs[0], scs[1]]
        osem = [S.new_sem("do0"), S.new_sem("do1")]
        sgb = sc
        SSQ = SCR[:, 0:16]
        RS2 = SCR[:, 16:32]
        SSb = Buf("ssq")
        S.barrier()
        S.op("sp", lambda: nc.sync.dma_start(out=GBC, in_=gbc_d), writes=[GBCb], dma_sem=sgb)
        for r in range(2):
            S.op("pool", (lambda r=r: nc.gpsimd.memset(G0[r], 0.0)), writes=[G0b[r]])
            S.op("pool", (lambda r=r: nc.gpsimd.memset(G1[r], 0.0)), writes=[G1b[r]])
        last = []
        for j in range(16):
            c0 = HALO + 128 * j
            r = j % 2
            S.op("pool", (lambda j=j, r=r: nc.gpsimd.indirect_dma_start(
                out=G0[r], out_offset=None, in_=ys_d, in_offset=bass.IndirectOffsetOnAxis(S0U[:, j:j + 1], 0),
                bounds_check=bnd["v"], oob_is_err=False)),
                reads=[YSDb, IDXb], writes=[G0b[r]], dma_sem=gsm[r])
            S.op("pool", (lambda j=j, r=r: nc.gpsimd.indirect_dma_start(
                out=G1[r], out_offset=None, in_=ys_d, in_offset=bass.IndirectOffsetOnAxis(S1U[:, j:j + 1], 0),
                bounds_check=bnd["v"], oob_is_err=False)),
                reads=[YSDb, IDXb], writes=[G1b[r]], dma_sem=gsm[r])
            G0b[r].w = dict(G1b[r].w)
            for half in range(2):
                i = state["bank"]
                state["bank"] = (i + 1) % 8
                for kk in range(4):
                    S.op("pe", (lambda i=i, kk=kk, half=half, c0=c0: nc.tensor.matmul(
                        ps[i][:, kk * 128:(kk + 1) * 128], lhsT=H[:, 4 * half + kk, c0:c0 + 128], rhs=identf[:],
                        start=True, stop=True)),
                        reads=Hb + [Bc] if kk in (0, 3) else (), writes=[P[i]] if kk in (0, 3) else (), inc=(kk == 3))
                S.op("dve", (lambda i=i, j=j, r=r, half=half: nc.vector.scalar_tensor_tensor(
                    out=RR[r][:, half * 512:(half + 1) * 512], in0=G0[r][:, half * 512:(half + 1) * 512],
                    scalar=RT[:, j, 12:13], in1=ps[i][:, :], op0=ALU.mult, op1=ALU.add)),
                    reads=[P[i], G0b[r]] + allrt, writes=[RRb[r]], waw=(half == 0))
            S.op("dve", (lambda j=j, r=r: nc.vector.scalar_tensor_tensor(
                out=RR[r], in0=G1[r], scalar=RT[:, j, 13:14], in1=RR[r], op0=ALU.mult, op1=ALU.add)),
                reads=[G1b[r], RRb[r]] + allrt, writes=[RRb[r]])
            S.op("act", (lambda j=j, r=r: nc.scalar.activation(
                out=G0[r], in_=RR[r], func=AF.Square, accum_out=SSQ[:, j:j + 1])),
                reads=[RRb[r]], writes=[G0b[r], SSb])
            S.op("act", (lambda j=j: nc.scalar.activation(
                out=RS2[:, j:j + 1], in_=SSQ[:, j:j + 1], func=AF.Sqrt, bias=eps_ap(RMS_EPS), scale=1.0 / D)),
                reads=[SSb, Bc], writes=[SSb])
            S.op("dve", (lambda j=j: nc.vector.reciprocal(out=RS2[:, j:j + 1], in_=RS2[:, j:j + 1])),
                 reads=[SSb], writes=[SSb])
            S.op("dve", (lambda j=j, r=r: nc.vector.scalar_tensor_tensor(
                out=G1[r], in0=RR[r], scalar=RS2[:, j:j + 1], in1=GBC, op0=ALU.mult, op1=ALU.mult)),
                reads=[RRb[r], SSb, GBCb], writes=[G1b[r]])
            tok = S.op("sp", (lambda j=j, r=r: nc.sync.dma_start(out=out_d[128 * j:128 * (j + 1), :], in_=G1[r])),
                       reads=[G1b[r]], dma_sem=osem[r])
            last.append(tok)
        S.op("sp", None, after=last, inc=False)
        print("sbuf bytes remaining:", nc.sbuf_bytes_remaining)
        S.run_block()
    return nc


_CACHE = {}


def _prep_inputs(x, meta_tokens, conv_w_pw1, conv_b_pw1, conv_w_dw, conv_b_dw, conv_ln_g, conv_ln_b,
                 conv_w_pw2, conv_b_pw2, pool_w_group, pool_scale, ffn_w_gate, ffn_w_up, ffn_w_down,
                 moe_w_router, moe_w_gate, moe_w_up, moe_w_down, mix_norm_g, ffn_norm_g, final_norm_g):
    f = lambda a: np.ascontiguousarray(np.asarray(a, dtype=np.float32))

    def pk(v):
        v = np.asarray(v, dtype=np.float32).reshape(-1, 128)
        return v.T

    cols = [pk(mix_norm_g[0]), pk(mix_norm_g[1]), pk(ffn_norm_g[0]), pk(ffn_norm_g[1]), pk(final_norm_g),
            pk(conv_b_pw1[0]), pk(conv_b_dw[0]), pk(conv_ln_g[0]), pk(conv_ln_b[0]), pk(conv_b_pw2[0]),
            pk(pool_scale[0])]
    wdw = np.asarray(conv_w_dw[0], dtype=np.float32)
    wdw = wdw.reshape(CONVW, KC, 128).transpose(2, 1, 0).reshape(128, KC * CONVW)
    cvec = f(np.concatenate(cols + [wdw], axis=1))
    assert cvec.shape == (128, NCV), cvec.shape
    wr = np.asarray(moe_w_router[0], dtype=np.float32).reshape(KC, 128, NE).transpose(1, 0, 2).reshape(128, KC * NE)
    xe = np.concatenate([np.zeros((32, D), np.float32), np.asarray(meta_tokens, np.float32),
                         np.asarray(x[0], np.float32)], axis=0)
    shared = {
        "cvec": cvec, "wr": f(wr), "ident": np.eye(128, dtype=np.float32),
        "tri": np.triu(np.ones((128, 128), np.float32), 1),
        "gbc": f(np.broadcast_to(np.asarray(final_norm_g, np.float32)[None, :], (128, D))),
        "w_pw1": f(conv_w_pw1[0]), "w_pw2": f(conv_w_pw2[0]), "pool_w": f(pool_w_group[0]),
        "ffn_wg": f(ffn_w_gate[0]), "ffn_wu": f(ffn_w_up[0]), "ffn_wd": f(ffn_w_down[0]),
        "moe_wg": f(moe_w_gate[0]), "moe_wu": f(moe_w_up[0]), "moe_wd": f(moe_w_down[0]),
    }
    in_maps = []
    for c in range(NCORES):
        m = dict(shared)
        m["xT"] = f(xe[TOK * c: TOK * c + TW].T)
        um = np.ones((128, HALO), np.float32)
        if c == 0:
            um[:, :32] = 0.0
        m["umask"] = um
        in_maps.append(m)
    return in_maps


def kernel(**inputs):
    if "nc" not in _CACHE:
        _CACHE["nc"] = build_program()
    nc = _CACHE["nc"]
    in_maps = _prep_inputs(**inputs)
    res = run_bass_kernel_spmd(nc, in_maps, core_ids=list(range(NCORES)))
    outs = [np.asarray(r["out"]) for r in res.results]
    out = np.concatenate(outs, axis=0).reshape(1, SEQ, D).astype(np.float32)
    return out
```

```python
import numpy as np
import concourse.bass as bass
import concourse.mybir as mybir
from concourse.bass_utils import run_bass_kernel_spmd
from contextlib import ExitStack

F32 = mybir.dt.float32
BF16 = mybir.dt.bfloat16
ALU = mybir.AluOpType
AF = mybir.ActivationFunctionType

NCORES = 8
D = 1024
KC = 8
SEQ = 16384
NMETA = 16
TOK = SEQ // NCORES
HALO = 48
TW = TOK + HALO
DFF = 2816
DFE = 3584
NE = 8
CONVW = 31
RMS_EPS = 1e-6
LN_EPS = 1e-5
POOLW = (2, 4, 8, 16)
CAP = 768
NST = CAP // 128
NSLOT = NE * CAP
U32 = mybir.dt.uint32

TT0 = [(0, 432), (432, 416), (848, 416), (1264, 416), (1680, 416)]
TT1 = [(32, 400)] + TT0[1:]
TT2 = [(48, 384)] + TT0[1:]
TT3 = [(48 + 512 * i, 512) for i in range(4)]

CV = {}
_o = 0
for _n, _w in (("mix_g0", 8), ("mix_g1", 8), ("ffn_g0", 8), ("ffn_g1", 8), ("fin_g", 8),
               ("b_pw1", 16), ("b_dw", 8), ("ln_g", 8), ("ln_b", 8), ("b_pw2", 8),
               ("pool_s", 8), ("w_dw", 8 * CONVW)):
    CV[_n] = _o
    _o += _w
NCV = _o


class Buf:
    __slots__ = ("name", "w", "r")

    def __init__(self, name):
        self.name = name
        self.w = {}
        self.r = {}


class Sched:
    ENGS = ("pe", "act", "dve", "pool", "sp")

    def __init__(self, nc, es):
        self.nc = nc
        self.es = es
        self.eng = {"pe": nc.tensor, "act": nc.scalar, "dve": nc.vector,
                    "pool": nc.gpsimd, "sp": nc.sync}
        self.stream = {e: [] for e in self.ENGS}
        self.sems = {}
        self.cnt = {}
        self.seen = {e: {} for e in self.ENGS}
        for e in self.ENGS:
            self.new_sem(e)

    def new_sem(self, name):
        self.sems[name] = self.es.enter_context(self.nc.semaphore("s_" + name))
        self.cnt[name] = 0
        return name

    def op(self, e, fn, reads=(), writes=(), dma_sem=None, inc=True, after=(), waw=True):
        d = {}

        def add(tok):
            if tok is None:
                return
            s, v = tok
            if d.get(s, 0) < v:
                d[s] = v
        for b in reads:
            for s, v in b.w.items():
                add((s, v))
        for b in writes:
            if waw:
                for s, v in b.w.items():
                    if not (dma_sem is not None and s == dma_sem):
                        add((s, v))
            for s, v in b.r.items():
                add((s, v))
        for tok in after:
            add(tok)
        waits = []
        seen = self.seen[e]
        for s, v in d.items():
            if e == "pe" and s == "pe":
                continue
            if seen.get(s, 0) < v:
                waits.append((s, v))
                seen[s] = v
        if not inc:
            self.stream[e].append((waits, fn, None, 0))
            return None
        if dma_sem is None:
            s, n = e, 1
        else:
            s, n = dma_sem, 16
        self.cnt[s] += n
        tok = (s, self.cnt[s])
        self.stream[e].append((waits, fn, s, n))
        for b in writes:
            if waw:
                b.w = {s: tok[1]}
                b.r = {}
            else:
                b.w[s] = tok[1]
        for b in reads:
            if b.r.get(s, 0) < tok[1]:
                b.r[s] = tok[1]
        return tok

    def barrier(self):
        snap = dict(self.cnt)
        for e in self.ENGS:
            waits = []
            for s, v in snap.items():
                if v == 0 or (e == "pe" and s == "pe"):
                    continue
                if self.seen[e].get(s, 0) < v:
                    waits.append((s, v))
                    self.seen[e][s] = v
            if waits:
                self.stream[e].append((waits, None, None, 0))

    def replay(self, e):
        eng = self.eng[e]
        for waits, fn, s, n in self.stream[e]:
            for ws, wv in waits:
                eng.wait_ge(self.sems[ws], wv)
            if fn is None:
                continue
            ins = fn()
            if s is not None:
                ins.then_inc(self.sems[s], n)

    def run_block(self):
        nc = self.nc
        with nc.Block() as block:
            @block.tensor
            def _(t):
                self.replay("pe")

            @block.scalar
            def _(t):
                self.replay("act")

            @block.vector
            def _(t):
                self.replay("dve")

            @block.gpsimd
            def _(t):
                self.replay("pool")

            @block.sync
            def _(t):
                self.replay("sp")


def build_program(debug=False):
    nc = bass.Bass("TRN2", target_bir_lowering=False)
    dbg_t = [nc.dram_tensor(f"dbg{i}", [D, TW], F32, kind="ExternalOutput").ap() for i in range(4)] if debug else []

    def din(name, shape):
        return nc.dram_tensor(name, list(shape), F32, kind="ExternalInput").ap()

    xT = din("xT", (D, TW))
    umask_d = din("umask", (128, HALO))
    cvec_d = din("cvec", (128, NCV))
    wr_d = din("wr", (128, KC * NE))
    ident_d = din("ident", (128, 128))
    w_pw1 = din("w_pw1", (D, 2 * D))
    w_pw2 = din("w_pw2", (D, D))
    pool_w = din("pool_w", (4, 256, 256))
    ffn_wg = din("ffn_wg", (D, DFF))
    ffn_wu = din("ffn_wu", (D, DFF))
    ffn_wd = din("ffn_wd", (DFF, D))
    moe_wg = din("moe_wg", (NE, D, DFE))
    moe_wu = din("moe_wu", (NE, D, DFE))
    moe_wd = din("moe_wd", (NE, DFE, D))
    tri_d = din("tri", (128, 128))
    gbc_d = din("gbc", (128, D))
    out_d = nc.dram_tensor("out", [TOK, D], F32, kind="ExternalOutput").ap()
    xs_d = nc.dram_tensor("xs_scratch", [NSLOT, D], BF16).ap()
    ys_d = nc.dram_tensor("ys_scratch", [NSLOT, D], F32).ap()

    with ExitStack() as es:
        S = Sched(nc, es)

        def sb(name, shape, dt):
            return es.enter_context(nc.sbuf_tensor("sb_" + name, list(shape), dt))

        H = sb("H", (128, KC, TW), F32)
        B1 = sb("B1", (128, KC, TW), BF16)
        B2 = sb("B2", (128, KC * TW), BF16)
        WP1 = [sb(f"wp1_{b}", (128, KC, 512), BF16) for b in range(2)]
        WP2 = [sb(f"wp2_{b}", (128, KC, 512), BF16) for b in range(2)]
        WP3 = [sb(f"wp3_{b}", (128, 4, 1024), BF16) for b in range(2)]
        R1 = sb("R1", (128, 4096), BF16)
        Z = sb("Z", (128, KC, 416), BF16)
        TMP = [sb(f"tmp{i}", (128, 512), F32) for i in range(5)]
        cvec = sb("cvec", (128, NCV), F32)
        wr = sb("wr", (128, KC, NE), F32)
        wrg = sb("wrg", (128, KC, NE), F32)
        identf = sb("identf", (128, 128), F32)
        identb = sb("identb", (128, 128), BF16)
        onesb = sb("onesb", (128, 128), BF16)
        onesf = sb("onesf", (128, 128), F32)
        umask = sb("umask", (128, HALO), F32)
        ps = [es.enter_context(nc.psum_tensor(f"ps{i}", [128, 512], F32)) for i in range(8)]

        Y = B1[:].bitcast(F32)
        U = B2[:].rearrange("p (k n) -> p k n", k=KC)
        B2f = B2[:].bitcast(F32)
        SQ = R1[:, 0:KC * 432].rearrange("p (k n) -> p k n", k=KC)
        Abuf = [R1[:, i * 2048:(i + 1) * 2048].rearrange("p (m n) -> p m n", m=4) for i in range(2)]
        CWe = [B2f[:, i * 2048:(i + 1) * 2048] for i in range(2)]
        RSall = B2f[:, 6144:8192]
        Zf = Z[:].rearrange("p k n -> p (k n)").bitcast(F32)
        RT = Zf[:, 0:640].rearrange("p (j c) -> p j c", j=16)
        Mf = Zf[:, 768:896].rearrange("p (j e) -> p j e", j=16)
        Mb = Zf[:, 896:960].bitcast(BF16)
        WITHIN = Zf[:, 960:1088]
        OFF = Zf[:, 1088:1216].rearrange("p (j e) -> p j e", j=16)
        SLV = Zf[:, 1216:1344].rearrange("p (j e) -> p j e", j=16)
        S0F = Zf[:, 1344:1360]
        S1F = Zf[:, 1360:1376]
        S0U = Zf[:, 1376:1392].bitcast(U32)
        S1U = Zf[:, 1392:1408].bitcast(U32)
        EC = Zf[:, 1440:1568].rearrange("p (j e) -> p j e", j=16)
        SCR = Zf[:, 1568:1664]
        trib = sb("trib", (128, 128), BF16)
        OUTb = [B2f[:, i * 3456:(i + 1) * 3456].rearrange("p (k n) -> p k n", k=KC) for i in range(2)]
        assert tuple(Y.shape) == (128, KC, TW // 2), Y.shape

        P = [Buf(f"ps{i}") for i in range(8)]
        state = {"bank": 0, "dq": 0, "r1": "sq"}

        def cv(name, j=0):
            c = CV[name] + j
            return cvec[:, c:c + 1]

        def mm_group(pairs, n, reads, rows=128):
            i = state["bank"]
            state["bank"] = (i + 1) % 8
            L = len(pairs)
            for j, (l, r) in enumerate(pairs):
                edge = (j == 0 or j == L - 1)
                S.op("pe",
                     (lambda l=l, r=r, j=j, i=i: nc.tensor.matmul(
                         ps[i][:rows, :n], lhsT=l, rhs=r, start=(j == 0), stop=(j == L - 1))),
                     reads=reads if edge else (), writes=[P[i]] if edge else (),
                     inc=(j == L - 1))
            return i

        Bc = Buf("consts")
        sc = S.new_sem("dconst")
        trif = TMP[3][:, 0:128]
        for dst, src in ((cvec[:], cvec_d), (wr[:].rearrange("p k e -> p (k e)"), wr_d),
                         (identf[:], ident_d), (umask[:], umask_d), (trif, tri_d)):
            S.op("sp", (lambda dst=dst, src=src: nc.sync.dma_start(out=dst, in_=src)),
                 writes=[Bc], dma_sem=sc)
        S.op("dve", lambda: nc.vector.memset(onesb[:], 1.0), writes=[Bc])
        S.op("dve", lambda: nc.vector.memset(onesf[:], 1.0), writes=[Bc])
        S.op("dve", lambda: nc.vector.tensor_copy(out=identb[:], in_=identf[:]), reads=[Bc], writes=[Bc])
        for k in range(KC):
            S.op("dve", (lambda k=k: nc.vector.tensor_scalar(
                out=wrg[:, k, :], in0=wr[:, k, :], scalar1=cv("ffn_g1", k), scalar2=None, op0=ALU.mult)),
                reads=[Bc], writes=[Bc])

        Hb = [Buf(f"H{t}") for t in range(5)]
        sx = S.new_sem("dx")
        dbg_sem = S.new_sem("ddbg") if debug else None

        def dump(i):
            if not debug:
                return
            S.barrier()
            for k in range(KC):
                S.op("sp", (lambda k=k, i=i: nc.sync.dma_start(out=dbg_t[i][k * 128:(k + 1) * 128, :], in_=H[:, k, :])),
                     reads=Hb, dma_sem=dbg_sem)
            S.barrier()
        xTv = xT.rearrange("(k p) n -> p k n", p=128)
        sxs = [sx] + [S.new_sem(f"dx{t}") for t in range(1, 5)]
        for t, (o, n) in enumerate(TT0):
            S.op("sp", (lambda o=o, n=n: nc.sync.dma_start(out=H[:, :, o:o + n], in_=xTv[:, :, o:o + n])),
                 writes=[Hb[t]], dma_sem=sxs[t])

        bnd = {}

        def _mk_bound():
            reg = nc.gpsimd.alloc_register("slot_bound")
            ins = nc.gpsimd.reg_mov(reg, NSLOT - 1)
            bnd["v"] = nc.gpsimd.snap(reg)
            return ins
        S.op("pool", _mk_bound, inc=False)

        XS0b = Buf("xs0")
        ZTb = Buf("zt")
        ZT = TMP[4][:, :].bitcast(BF16)
        szf = S.new_sem("dzf")
        S.op("dve", lambda: nc.vector.memset(TMP[4][:, :], 0.0), writes=[ZTb])
        for q in range(NSLOT // 128):
            S.op("sp", (lambda q=q: nc.sync.dma_start(out=xs_d[q * 128:(q + 1) * 128, :], in_=ZT)),
                 reads=[ZTb], writes=[XS0b], dma_sem=szf)
        S.op("dve", lambda: nc.vector.tensor_copy(out=trib[:], in_=trif[:]), reads=[Bc], writes=[Bc])

        WBb = [Buf("wb0"), Buf("wb1")]
        wsem = [S.new_sem("dw0"), S.new_sem("dw1")]

        def wload(b, parts):
            for dst, src in parts:
                S.op("pool", (lambda dst=dst, src=src: nc.gpsimd.dma_start(out=dst, in_=src)),
                     writes=[WBb[b]], dma_sem=wsem[b])

        def kview(ap):
            return ap.rearrange("(k p) n -> p k n", p=128)

        SQb, RSb = Buf("sq"), Buf("rs")

        def rmsnorm(tiles, tbufs_in, gname, out_fn, out_bufs, eps=RMS_EPS, rs_fn=None, rs_buf=None):
            if state["r1"] != "sq":
                S.barrier()
                state["r1"] = "sq"
            for ti, (o, n) in enumerate(tiles):
                hb = tbufs_in[ti]
                rsap = (lambda o=o, n=n: TMP[0][:, :n]) if rs_fn is None else (lambda o=o, n=n: rs_fn(o, n))
                rsb = RSb if rs_buf is None else rs_buf
                S.op("act", (lambda o=o, n=n: nc.scalar.activation(
                    out=SQ[:, :, :n], in_=H[:, :, o:o + n], func=AF.Square)),
                    reads=[hb], writes=[SQb])
                i = mm_group([(onesb[:], SQ[:, k, :n]) for k in range(KC)], n, [Bc, SQb])
                S.op("act", (lambda i=i, n=n, rsap=rsap: nc.scalar.activation(
                    out=rsap(), in_=ps[i][:, :n], func=AF.Sqrt, bias=eps_ap(eps), scale=1.0 / D)),
                    reads=[P[i], Bc], writes=[rsb])
                S.op("dve", (lambda rsap=rsap: nc.vector.reciprocal(out=rsap(), in_=rsap())),
                     reads=[rsb], writes=[rsb])
                for k in range(KC):
                    S.op("dve", (lambda k=k, o=o, n=n, rsap=rsap: nc.vector.scalar_tensor_tensor(
                        out=out_fn(k, o, n), in0=H[:, k, o:o + n], scalar=cv(gname, k),
                        in1=rsap(), op0=ALU.mult, op1=ALU.mult)),
                        reads=[hb, rsb, Bc], writes=[out_bufs[ti]])

        epsc = sb("epsc", (128, 2), F32)
        S.op("dve", lambda: nc.vector.memset(epsc[:, 0:1], RMS_EPS), writes=[Bc])
        S.op("dve", lambda: nc.vector.memset(epsc[:, 1:2], LN_EPS), writes=[Bc])

        def eps_ap(eps):
            return epsc[:, 0:1] if eps == RMS_EPS else epsc[:, 1:2]

        HNb = [Buf(f"HN{t}") for t in range(5)]
        for s in range(2):
            wload(s, [(WP1[s][:], kview(w_pw1[:, 512 * s:512 * s + 512])),
                      (WP2[s][:], kview(w_pw1[:, D + 512 * s:D + 512 * s + 512]))])
        rmsnorm(TT0, Hb, "mix_g0", lambda k, o, n: B1[:, k, o:o + n], HNb)

        Ub = Buf("U")
        SIGb = [Buf("sig0"), Buf("sig1")]
        sgi = 0
        for s in range(2):
            for ti, (o, n) in enumerate(TT0):
                for mc in range(4):
                    ch = 4 * s + mc
                    ia = mm_group([(WP1[s][:, k, mc * 128:(mc + 1) * 128], B1[:, k, o:o + n]) for k in range(KC)],
                                  n, [WBb[s], HNb[ti]])
                    ig = mm_group([(WP2[s][:, k, mc * 128:(mc + 1) * 128], B1[:, k, o:o + n]) for k in range(KC)],
                                  n, [WBb[s], HNb[ti]])
                    sg = sgi % 2
                    sgi += 1
                    S.op("act", (lambda ig=ig, n=n, ch=ch, sg=sg: nc.scalar.activation(
                        out=TMP[1 + sg][:, :n], in_=ps[ig][:, :n], func=AF.Sigmoid, bias=cv("b_pw1", 8 + ch))),
                        reads=[P[ig], Bc], writes=[SIGb[sg]])
                    S.op("dve", (lambda ia=ia, n=n, ch=ch, sg=sg, o=o: nc.vector.scalar_tensor_tensor(
                        out=U[:, ch, o:o + n], in0=ps[ia][:, :n], scalar=cv("b_pw1", ch),
                        in1=TMP[1 + sg][:, :n], op0=ALU.add, op1=ALU.mult)),
                        reads=[P[ia], SIGb[sg], Bc], writes=[Ub])
        for k in range(KC):
            S.op("dve", (lambda k=k: nc.vector.tensor_tensor(
                out=U[:, k, 0:HALO], in0=U[:, k, 0:HALO], in1=umask[:], op=ALU.mult)),
                reads=[Ub, Bc], writes=[Ub])

        S.barrier()

        W2b = [Buf("w2_0"), Buf("w2_1")]
        w2sem = [S.new_sem("dw2_0"), S.new_sem("dw2_1")]
        for s in range(2):
            S.op("pool", (lambda s=s: nc.gpsimd.dma_start(
                out=WP1[s][:], in_=kview(w_pw2[:, 512 * s:512 * s + 512]))),
                writes=[W2b[s]], dma_sem=w2sem[s])

        DGb = [Buf("dg0"), Buf("dg1")]
        Yb = Buf("Y")
        Zb = Buf("Z")
        MUb, M2b = Buf("mu"), ZTb
        groups = [[0, 1], [2, 3], [4]]
        dgi = 0
        for grp in groups:
            gbase = TT1[grp[0]][0]
            for c in range(KC):
                db = dgi % 2
                dgi += 1
                dg = WP3[db][:].rearrange("p a b -> p (a b)")
                for k in range(CONVW):
                    wcol = cvec[:, CV["w_dw"] + c * CONVW + k: CV["w_dw"] + c * CONVW + k + 1]
                    if k % 3 == 0:
                        S.op("pool", (lambda k=k, dg=dg, wcol=wcol: nc.gpsimd.tensor_scalar(
                            out=dg[:, k * 128:(k + 1) * 128], in0=identf[:], scalar1=wcol,
                            scalar2=0.0, op0=ALU.mult, op1=ALU.add)),
                            reads=[Bc], writes=[DGb[db]], waw=False)
                    else:
                        S.op("dve", (lambda k=k, dg=dg, wcol=wcol: nc.vector.tensor_scalar(
                            out=dg[:, k * 128:(k + 1) * 128], in0=identf[:], scalar1=wcol,
                            scalar2=None, op0=ALU.mult)),
                            reads=[Bc], writes=[DGb[db]], waw=False)
                for ti in grp:
                    o, n = TT1[ti]
                    i = mm_group([(dg[:, k * 128:(k + 1) * 128], U[:, c, o - 30 + k: o - 30 + k + n])
                                  for k in range(CONVW)], n, [DGb[db], Ub])
                    S.op("act", (lambda i=i, n=n, c=c, yo=o - gbase: nc.scalar.activation(
                        out=Y[:, c, yo:yo + n], in_=ps[i][:, :n], func=AF.Identity, bias=cv("b_dw", c))),
                        reads=[P[i], Bc], writes=[Yb])
            for ti in grp:
                o, n = TT1[ti]
                yo = o - gbase
                imu = mm_group([(onesf[:], Y[:, k, yo:yo + n]) for k in range(KC)], n, [Bc, Yb])
                S.op("act", (lambda yo=yo, n=n: nc.scalar.activation(
                    out=SQ[:, :, :n], in_=Y[:, :, yo:yo + n], func=AF.Square)),
                    reads=[Yb], writes=[SQb])
                isq = mm_group([(onesb[:], SQ[:, k, :n]) for k in range(KC)], n, [Bc, SQb])
                S.op("dve", (lambda imu=imu, n=n: nc.vector.tensor_scalar(
                    out=TMP[3][:, :n], in0=ps[imu][:, :n], scalar1=1.0 / D, scalar2=None, op0=ALU.mult)),
                    reads=[P[imu]], writes=[MUb])
                S.op("dve", (lambda n=n: nc.vector.tensor_tensor(
                    out=TMP[4][:, :n], in0=TMP[3][:, :n], in1=TMP[3][:, :n], op=ALU.mult)),
                    reads=[MUb], writes=[M2b])
                S.op("dve", (lambda isq=isq, n=n: nc.vector.scalar_tensor_tensor(
                    out=TMP[4][:, :n], in0=ps[isq][:, :n], scalar=1.0 / D, in1=TMP[4][:, :n],
                    op0=ALU.mult, op1=ALU.subtract)),
                    reads=[P[isq], M2b], writes=[M2b])
                S.op("act", (lambda n=n: nc.scalar.activation(
                    out=TMP[0][:, :n], in_=TMP[4][:, :n], func=AF.Sqrt, bias=eps_ap(LN_EPS), scale=1.0)),
                    reads=[M2b, Bc], writes=[RSb])
                S.op("dve", (lambda n=n: nc.vector.reciprocal(out=TMP[0][:, :n], in_=TMP[0][:, :n])),
                     reads=[RSb], writes=[RSb])
                for c in range(KC):
                    S.op("dve", (lambda c=c, yo=yo, n=n: nc.vector.tensor_tensor(
                        out=Y[:, c, yo:yo + n], in0=Y[:, c, yo:yo + n], in1=TMP[3][:, :n], op=ALU.subtract)),
                        reads=[Yb, MUb], writes=[Yb])
                    S.op("dve", (lambda c=c, yo=yo, n=n: nc.vector.tensor_tensor(
                        out=Y[:, c, yo:yo + n], in0=Y[:, c, yo:yo + n], in1=TMP[0][:, :n], op=ALU.mult)),
                        reads=[Yb, RSb], writes=[Yb])
                    S.op("act", (lambda c=c, yo=yo, n=n: nc.scalar.activation(
                        out=Z[:, c, :n], in_=Y[:, c, yo:yo + n], func=AF.Silu,
                        bias=cv("ln_b", c), scale=cv("ln_g", c))),
                        reads=[Yb, Bc], writes=[Zb])
                for oc in range(KC):
                    s2, mc = divmod(oc, 4)
                    i = mm_group([(WP1[s2][:, k, mc * 128:(mc + 1) * 128], Z[:, k, :n]) for k in range(KC)],
                                 n, [W2b[s2], Zb])
                    S.op("dve", (lambda i=i, oc=oc, o=o, n=n: nc.vector.scalar_tensor_tensor(
                        out=H[:, oc, o:o + n], in0=ps[i][:, :n], scalar=cv("b_pw2", oc),
                        in1=H[:, oc, o:o + n], op0=ALU.add, op1=ALU.add)),
                        reads=[P[i], Bc, Hb[ti]], writes=[Hb[ti]])

        S.barrier()
        dump(0)

        SGb = [Buf("sg0"), Buf("sg1")]
        T2b = [Buf("t2a"), Buf("t2b")]
        Ab = [Buf("A0"), Buf("A1")]
        ffn_state = {"sg": 0, "a": 0, "slab": 0}

        def ffn(slabs, tiles, hnb, hb, cw=None):
            st = ffn_state
            if state["r1"] != "a":
                S.barrier()
                state["r1"] = "a"
            b0 = st["slab"] % 2
            wload(b0, slabs[0][1])
            for si, (wdt, _) in enumerate(slabs):
                b = (st["slab"] + si) % 2
                if si + 1 < len(slabs):
                    wload(1 - b, slabs[si + 1][1])
                nm = wdt // 128
                for ti, (o, n) in enumerate(tiles):
                    a = st["a"] % 2
                    st["a"] += 1
                    for mc in range(nm):
                        ig = mm_group([(WP1[b][:, k, mc * 128:(mc + 1) * 128], B1[:, k, o:o + n]) for k in range(KC)],
                                      n, [WBb[b], hnb[ti]])
                        iu = mm_group([(WP2[b][:, k, mc * 128:(mc + 1) * 128], B1[:, k, o:o + n]) for k in range(KC)],
                                      n, [WBb[b], hnb[ti]])
                        sg = st["sg"] % 2
                        st["sg"] += 1
                        S.op("act", (lambda ig=ig, n=n, sg=sg: nc.scalar.activation(
                            out=TMP[1 + sg][:, :n], in_=ps[ig][:, :n], func=AF.Silu)),
                            reads=[P[ig]], writes=[SGb[sg]])
                        if cw is None:
                            S.op("dve", (lambda iu=iu, n=n, sg=sg, a=a, mc=mc: nc.vector.tensor_tensor(
                                out=Abuf[a][:, mc, :n], in0=ps[iu][:, :n], in1=TMP[1 + sg][:, :n], op=ALU.mult)),
                                reads=[P[iu], SGb[sg]], writes=[Ab[a]])
                        else:
                            cwfn, cwb = cw
                            S.op("dve", (lambda n=n, sg=sg, o=o: nc.vector.tensor_tensor(
                                out=TMP[3 + sg][:, :n], in0=TMP[1 + sg][:, :n], in1=cwfn(o, n), op=ALU.mult)),
                                reads=[SGb[sg], cwb], writes=[T2b[sg]])
                            S.op("dve", (lambda iu=iu, n=n, sg=sg, a=a, mc=mc: nc.vector.tensor_tensor(
                                out=Abuf[a][:, mc, :n], in0=ps[iu][:, :n], in1=TMP[3 + sg][:, :n], op=ALU.mult)),
                                reads=[P[iu], T2b[sg]], writes=[Ab[a]])
                    for oc in range(KC):
                        i = mm_group([(WP3[b][:, mc, oc * 128:(oc + 1) * 128], Abuf[a][:, mc, :n]) for mc in range(nm)],
                                     n, [WBb[b], Ab[a]])
                        S.op("dve", (lambda i=i, oc=oc, o=o, n=n: nc.vector.tensor_tensor(
                            out=H[:, oc, o:o + n], in0=ps[i][:, :n], in1=H[:, oc, o:o + n], op=ALU.add)),
                            reads=[P[i], hb[ti]], writes=[hb[ti]])
            st["slab"] += len(slabs)

        def ffn_slabs(wg, wu, wd, dff):
            out = []
            off = 0
            while off < dff:
                wdt = min(512, dff - off)
                nm = wdt // 128
                out.append((wdt, off))
                off += wdt
            res = []
            for wdt, off in out:
                nm = wdt // 128

                def mk(b, wdt=wdt, off=off, nm=nm):
                    return [(WP1[b][:, :, :wdt], kview(wg[:, off:off + wdt])),
                            (WP2[b][:, :, :wdt], kview(wu[:, off:off + wdt])),
                            (WP3[b][:, :nm, :], wd[off:off + wdt, :].rearrange("(m p) n -> p m n", p=128))]
                res.append((wdt, mk))
            return res

        def run_ffn(wg, wu, wd, dff, tiles, hnb, hb, cw=None):
            sl = ffn_slabs(wg, wu, wd, dff)
            base = ffn_state["slab"]
            slabs = [(wdt, mk((base + si) % 2)) for si, (wdt, mk) in enumerate(sl)]
            ffn(slabs, tiles, hnb, hb, cw)

        rmsnorm(TT1, Hb, "ffn_g0", lambda k, o, n: B1[:, k, o:o + n], HNb)
        run_ffn(ffn_wg, ffn_wu, ffn_wd, DFF, TT1, HNb, Hb)

        dump(1)
        spw = S.new_sem("dpw")
        PW = WP3[0][:].rearrange("p a b -> p (a b)")[:, 0:2048].rearrange("p (g k n) -> p g k n", g=4, k=2)
        for g in range(4):
            S.op("pool", (lambda g=g: nc.gpsimd.dma_start(
                out=PW[:, g, :, :], in_=pool_w[g].rearrange("(k p) n -> p k n", p=128))),
                writes=[WBb[0]], dma_sem=spw)
        rmsnorm(TT1, Hb, "mix_g1", lambda k, o, n: B1[:, k, o:o + n], HNb)
        W1f = WP1[0][:].rearrange("p a b -> p (a b)")
        PWS = W1f[:, 0:2048].rearrange("p (g k n) -> p g k n", g=4, k=2)
        PWN = W1f[:, 2048:4096].rearrange("p (g k n) -> p g k n", g=4, k=2)
        for g in range(4):
            S.op("dve", (lambda g=g: nc.vector.tensor_scalar(
                out=PWS[:, g, :, :], in0=PW[:, g, :, :], scalar1=1.0 / POOLW[g], scalar2=None, op0=ALU.mult)),
                reads=[WBb[0]], writes=[WBb[0]])
        S.op("dve", lambda: nc.vector.tensor_scalar(
            out=W1f[:, 2048:4096], in0=WP3[0][:].rearrange("p a b -> p (a b)")[:, 0:2048],
            scalar1=-1.0, scalar2=None, op0=ALU.mult), reads=[WBb[0]], writes=[WBb[0]])
        for ti, (o, n) in enumerate(TT2):
            rd = [HNb[ti]] + ([HNb[ti - 1]] if ti > 0 else [])
            for g in range(4):
                w = POOLW[g]
                for oc in range(2):
                    ch = 2 * g + oc
                    pairs = []
                    for kc in range(2):
                        for dd in range(w):
                            pairs.append((PWS[:, g, kc, oc * 128:(oc + 1) * 128], B1[:, 2 * g + kc, o - dd:o - dd + n]))
                        pairs.append((PWN[:, g, kc, oc * 128:(oc + 1) * 128], B1[:, 2 * g + kc, o:o + n]))
                    i = mm_group(pairs, n, [WBb[0]] + rd)
                    S.op("dve", (lambda i=i, ch=ch, o=o, n=n: nc.vector.scalar_tensor_tensor(
                        out=H[:, ch, o:o + n], in0=ps[i][:, :n], scalar=cv("pool_s", ch), in1=H[:, ch, o:o + n],
                        op0=ALU.mult, op1=ALU.add)),
                        reads=[P[i], Hb[ti], Bc], writes=[Hb[ti]])

        dump(2)
        RSAb = Buf("rsall")
        rmsnorm(TT2, Hb, "ffn_g1", lambda k, o, n: B1[:, k, o:o + n], HNb,
                rs_fn=lambda o, n: RSall[:, o - HALO:o - HALO + n], rs_buf=RSAb)
        RTb = [Buf("rt")] * 16
        rb = RTb[0]
        ir = state["bank"]
        state["bank"] = (ir + 1) % 8
        for j in range(16):
            c0 = HALO + 128 * j
            for k in range(KC):
                first = (j == 0 and k == 0)
                S.op("pe", (lambda j=j, k=k, c0=c0: nc.tensor.matmul(
                    ps[ir][:, 8 * j:8 * j + 8], lhsT=H[:, k, c0:c0 + 128], rhs=wrg[:, k, :],
                    start=(k == 0), stop=(k == KC - 1))),
                    reads=Hb + [Bc] if first else (), writes=[P[ir]] if first else (), inc=False)
            last = (j == 15)
            S.op("pe", (lambda j=j: nc.tensor.matmul(
                ps[ir][:, 128 + 2 * j:128 + 2 * j + 2], lhsT=RSall[:, 128 * j:128 * j + 128], rhs=identf[:, 0:2],
                start=True, stop=True)),
                reads=[RSAb, Bc] + Hb if (j == 0 or last) else (), writes=[P[ir]] if last else (), inc=last)
        RAW = RT[:, :, 0:8]
        S.op("dve", lambda: nc.vector.tensor_copy(
            out=RAW, in_=ps[ir][:, 0:128].rearrange("p (j e) -> p j e", j=16)), reads=[P[ir]], writes=[rb])
        S.op("dve", lambda: nc.vector.tensor_copy(
            out=RT[:, :, 8], in_=ps[ir][:, 128:160].rearrange("p (j c) -> p j c", j=16)[:, :, 0]),
            reads=[P[ir]], writes=[rb])
        S.op("dve", lambda: nc.vector.tensor_reduce(out=RT[:, :, 9], in_=RAW, axis=mybir.AxisListType.X, op=ALU.max),
             reads=[rb], writes=[rb])
        def first_max_mask(src0, mcol, dst0):
            S.op("dve", lambda: nc.vector.memset(RT[:, :, 14], 0.0), reads=[rb], writes=[rb])
            for e in range(NE):
                S.op("dve", (lambda e=e: nc.vector.tensor_tensor(
                    out=RT[:, :, dst0 + e], in0=RT[:, :, src0 + e], in1=RT[:, :, mcol], op=ALU.is_equal)),
                    reads=[rb], writes=[rb])
                S.op("dve", (lambda e=e: nc.vector.scalar_tensor_tensor(
                    out=RT[:, :, dst0 + e], in0=RT[:, :, 14], scalar=1.0, in1=RT[:, :, dst0 + e],
                    op0=ALU.subtract, op1=ALU.mult)), reads=[rb], writes=[rb])
                S.op("dve", (lambda e=e: nc.vector.tensor_tensor(
                    out=RT[:, :, 14], in0=RT[:, :, 14], in1=RT[:, :, dst0 + e], op=ALU.subtract)),
                    reads=[rb], writes=[rb])
            S.op("dve", lambda: nc.vector.tensor_scalar(
                out=RT[:, :, dst0:dst0 + NE], in0=RT[:, :, dst0:dst0 + NE], scalar1=-1.0, scalar2=None, op0=ALU.mult),
                reads=[rb], writes=[rb])
        first_max_mask(0, 9, 16)
        S.op("dve", lambda: nc.vector.scalar_tensor_tensor(
            out=RT[:, :, 24:32], in0=RT[:, :, 16:24], scalar=-1.0e30, in1=RAW, op0=ALU.mult, op1=ALU.add),
            reads=[rb], writes=[rb])
        S.op("dve", lambda: nc.vector.tensor_reduce(out=RT[:, :, 10], in_=RT[:, :, 24:32], axis=mybir.AxisListType.X,
                                                    op=ALU.max), reads=[rb], writes=[rb])
        first_max_mask(24, 10, 32)
        S.op("dve", lambda: nc.vector.tensor_tensor(out=RT[:, :, 11], in0=RT[:, :, 9], in1=RT[:, :, 10], op=ALU.subtract),
             reads=[rb], writes=[rb])
        S.op("dve", lambda: nc.vector.tensor_tensor(out=RT[:, :, 11], in0=RT[:, :, 11], in1=RT[:, :, 8], op=ALU.mult),
             reads=[rb], writes=[rb])
        S.op("act", lambda: nc.scalar.activation(out=RT[:, :, 12], in_=RT[:, :, 11], func=AF.Sigmoid),
             reads=[rb], writes=[rb])
        S.op("dve", lambda: nc.vector.tensor_scalar(out=RT[:, :, 13], in0=RT[:, :, 12], scalar1=-1.0, scalar2=1.0,
                                                    op0=ALU.mult, op1=ALU.add), reads=[rb], writes=[rb])

        IDXb = Buf("idx")
        allrt = RTb
        S.op("dve", lambda: nc.vector.tensor_tensor(out=Mf[:], in0=RT[:, :, 16:24], in1=RT[:, :, 32:40], op=ALU.add),
             reads=allrt, writes=[IDXb])
        S.op("dve", lambda: nc.vector.tensor_copy(out=Mb, in_=Mf[:].rearrange("p j e -> p (j e)")),
             reads=[IDXb], writes=[IDXb])
        iw = mm_group([(trib[:], Mb)], 128, [Bc, IDXb])
        ic = mm_group([(onesb[:], Mb)], 128, [Bc, IDXb])
        S.op("dve", lambda: nc.vector.memset(OFF[:, 0, :], 0.0), reads=[IDXb], writes=[IDXb])
        for j in range(1, 16):
            S.op("dve", (lambda j=j: nc.vector.tensor_tensor(
                out=OFF[:, j, :], in0=OFF[:, j - 1, :], in1=ps[ic][:, (j - 1) * NE:j * NE], op=ALU.add)),
                reads=[IDXb, P[ic]], writes=[IDXb])
        for e in range(NE):
            S.op("dve", (lambda e=e: nc.vector.memset(EC[:, :, e:e + 1], float(e * CAP))), reads=[IDXb], writes=[IDXb])
        Offl = OFF.rearrange("p j e -> p (j e)")
        SLVl = SLV.rearrange("p j e -> p (j e)")
        ECl = EC.rearrange("p j e -> p (j e)")
        S.op("dve", lambda: nc.vector.tensor_tensor(out=Offl, in0=Offl, in1=ps[iw][:, 0:128], op=ALU.add),
             reads=[IDXb, P[iw]], writes=[IDXb])
        S.op("dve", lambda: nc.vector.tensor_tensor(out=SLVl, in0=Offl, in1=ECl, op=ALU.add), reads=[IDXb], writes=[IDXb])
        S.op("dve", lambda: nc.vector.tensor_scalar(out=Offl, in0=Offl, scalar1=float(CAP), scalar2=1.0e6,
                                                    op0=ALU.is_ge, op1=ALU.mult), reads=[IDXb], writes=[IDXb])
        S.op("dve", lambda: nc.vector.tensor_tensor(out=SLVl, in0=SLVl, in1=Offl, op=ALU.add), reads=[IDXb], writes=[IDXb])
        for (msk, SF, SU, wc) in ((RT[:, :, 16:24], S0F, S0U, 12), (RT[:, :, 32:40], S1F, S1U, 13)):
            S.op("dve", (lambda msk=msk: nc.vector.tensor_tensor(out=Mf[:], in0=msk, in1=SLV, op=ALU.mult)),
                 reads=[IDXb] + allrt, writes=[IDXb])
            S.op("dve", (lambda SF=SF: nc.vector.tensor_reduce(out=SF, in_=Mf[:], axis=mybir.AxisListType.X, op=ALU.add)),
                 reads=[IDXb], writes=[IDXb])
            S.op("dve", (lambda SF=SF, SU=SU: nc.vector.tensor_copy(out=SU, in_=SF)), reads=[IDXb], writes=[IDXb])
            S.op("dve", (lambda SF=SF: nc.vector.tensor_scalar(out=SF, in0=SF, scalar1=float(NSLOT), scalar2=None,
                                                               op0=ALU.is_lt)), reads=[IDXb], writes=[IDXb])
            S.op("dve", (lambda SF=SF, wc=wc: nc.vector.tensor_tensor(out=RT[:, :, wc], in0=RT[:, :, wc], in1=SF, op=ALU.mult)),
                 reads=[IDXb] + allrt, writes=allrt)

        all_slabs = [(e, sl) for e in range(NE) for sl in range(DFE // 512)]

        def slab_parts(e, sl, b):
            off = 512 * sl
            return [(WP1[b][:], kview(moe_wg[e][:, off:off + 512])),
                    (WP2[b][:], kview(moe_wu[e][:, off:off + 512])),
                    (WP3[b][:], moe_wd[e][off:off + 512, :].rearrange("(m p) n -> p m n", p=128))]
        sbase = ffn_state["slab"]
        wload(sbase % 2, slab_parts(0, 0, sbase % 2))

        HT = [B2[:, r * 1024:(r + 1) * 1024] for r in range(4)]
        HTb = [Buf(f"ht{r}") for r in range(4)]
        scs = [S.new_sem(f"dsc{r}") for r in range(4)]
        XSb = Buf("xs")
        XSb.w = dict(XS0b.w)
        prev_sc = []
        for j in range(16):
            c0 = HALO + 128 * j
            r = j % 4
            for half in range(2):
                i = state["bank"]
                state["bank"] = (i + 1) % 8
                for kk in range(4):
                    S.op("pe", (lambda i=i, kk=kk, half=half, c0=c0: nc.tensor.matmul(
                        ps[i][:, kk * 128:(kk + 1) * 128], lhsT=B1[:, 4 * half + kk, c0:c0 + 128], rhs=identb[:],
                        start=True, stop=True)),
                        reads=HNb + [Bc] if kk in (0, 3) else (), writes=[P[i]] if kk in (0, 3) else (), inc=(kk == 3))
                if half == 0:
                    S.op("act", (lambda i=i, r=r: nc.scalar.activation(out=HT[r][:, 0:512], in_=ps[i][:, :], func=AF.Identity)),
                         reads=[P[i]], writes=[HTb[r]], waw=False)
                else:
                    S.op("dve", (lambda i=i, r=r: nc.vector.tensor_copy(out=HT[r][:, 512:1024], in_=ps[i][:, :])),
                         reads=[P[i]], writes=[HTb[r]], waw=False)
            for SU in (S0U, S1U):
                S.op("pool", (lambda SU=SU, j=j, r=r: nc.gpsimd.indirect_dma_start(
                    out=xs_d, out_offset=bass.IndirectOffsetOnAxis(SU[:, j:j + 1], 0), in_=HT[r], in_offset=None,
                    bounds_check=bnd["v"], oob_is_err=False)),
                    reads=[HTb[r], IDXb], writes=[XSb], dma_sem=scs[r], waw=False, after=list(prev_sc))
                prev_sc[:] = [(scs[r], S.cnt[scs[r]])]

        if state["r1"] != "a":
            S.barrier()
            state["r1"] = "a"
        XT = [B1[:].rearrange("p k n -> p (k n)")[:, 12288 + r * 1024: 12288 + (r + 1) * 1024] for r in range(3)]
        XTb = [Buf(f"xt{r}") for r in range(3)]
        xts = [S.new_sem(f"dxt{r}") for r in range(3)]
        XG = [B1[:].rearrange("p k n -> p (k n)")[:, g * 6144:(g + 1) * 6144].rearrange("p (k n) -> p k n", k=KC)
              for g in range(2)]
        XGb = [Buf("xg0"), Buf("xg1")]
        YA = B2f[:, 0:NST * 1024].rearrange("p (s n) -> p s n", s=NST)
        YAb = [Buf(f"ya{st}") for st in range(NST)]
        YSDb = Buf("ysd")
        yst = [S.new_sem("dys0"), S.new_sem("dys1")]
        xti = 0
        evi = 0
        gtiles = [(0, CAP // 2), (CAP // 2, CAP // 2)]
        nst_t = (CAP // 2) // 128
        def emit_xload(e):
            nonlocal xti, evi
            g = e % 2
            for st in range(NST):
                xr = xti % 3
                xti += 1
                S.op("sp", (lambda e=e, st=st, xr=xr: nc.sync.dma_start(
                    out=XT[xr], in_=xs_d[e * CAP + st * 128: e * CAP + (st + 1) * 128, :])),
                    reads=[XSb], writes=[XTb[xr]], dma_sem=xts[xr])
                for half in range(2):
                    i = state["bank"]
                    state["bank"] = (i + 1) % 8
                    for kk in range(4):
                        S.op("pe", (lambda i=i, kk=kk, half=half, xr=xr: nc.tensor.matmul(
                            ps[i][:, kk * 128:(kk + 1) * 128],
                            lhsT=XT[xr][:, (4 * half + kk) * 128:(4 * half + kk + 1) * 128], rhs=identb[:],
                            start=True, stop=True)),
                            reads=[XTb[xr], Bc] if kk in (0, 3) else (), writes=[P[i]] if kk in (0, 3) else (),
                            inc=(kk == 3))
                    dst = XG[g][:, 4 * half:4 * half + 4, st * 128:(st + 1) * 128]
                    srcv = ps[i][:, :].rearrange("p (a b) -> p a b", a=4)
                    evi += 1
                    if evi % 2:
                        S.op("act", (lambda dst=dst, srcv=srcv: nc.scalar.activation(out=dst, in_=srcv, func=AF.Identity)),
                             reads=[P[i]], writes=[XGb[g]], waw=False)
                    else:
                        S.op("dve", (lambda dst=dst, srcv=srcv: nc.vector.tensor_copy(out=dst, in_=srcv)),
                             reads=[P[i]], writes=[XGb[g]], waw=False)
        emit_xload(0)
        for si, (e, sl) in enumerate(all_slabs):
            b = (sbase + si) % 2
            if si + 1 < len(all_slabs):
                wload(1 - b, slab_parts(all_slabs[si + 1][0], all_slabs[si + 1][1], 1 - b))
            g = e % 2
            if sl == 2 and e + 1 < NE:
                emit_xload(e + 1)
            for ti, (o, n) in enumerate(gtiles):
                a = ffn_state["a"] % 2
                ffn_state["a"] += 1
                for mc in range(4):
                    ig = mm_group([(WP1[b][:, k, mc * 128:(mc + 1) * 128], XG[g][:, k, o:o + n]) for k in range(KC)],
                                  n, [WBb[b], XGb[g]])
                    iu = mm_group([(WP2[b][:, k, mc * 128:(mc + 1) * 128], XG[g][:, k, o:o + n]) for k in range(KC)],
                                  n, [WBb[b], XGb[g]])
                    sg = ffn_state["sg"] % 2
                    ffn_state["sg"] += 1
                    S.op("act", (lambda ig=ig, n=n, sg=sg: nc.scalar.activation(
                        out=TMP[1 + sg][:, :n], in_=ps[ig][:, :n], func=AF.Silu)),
                        reads=[P[ig]], writes=[SGb[sg]])
                    S.op("dve", (lambda iu=iu, n=n, sg=sg, a=a, mc=mc: nc.vector.tensor_tensor(
                        out=Abuf[a][:, mc, :n], in0=ps[iu][:, :n], in1=TMP[1 + sg][:, :n], op=ALU.mult)),
                        reads=[P[iu], SGb[sg]], writes=[Ab[a]])
                for s3 in range(nst_t):
                    st = ti * nst_t + s3
                    for half in range(2):
                        i = mm_group([(Abuf[a][:, mc, s3 * 128:(s3 + 1) * 128], WP3[b][:, mc, half * 512:(half + 1) * 512])
                                      for mc in range(4)], 512, [WBb[b], Ab[a]])
                        if sl == 0:
                            S.op("dve", (lambda i=i, st=st, half=half: nc.vector.tensor_copy(
                                out=YA[:, st, half * 512:(half + 1) * 512], in_=ps[i][:, :])),
                                reads=[P[i]], writes=[YAb[st]], waw=(half == 0))
                        else:
                            S.op("dve", (lambda i=i, st=st, half=half: nc.vector.tensor_tensor(
                                out=YA[:, st, half * 512:(half + 1) * 512], in0=ps[i][:, :],
                                in1=YA[:, st, half * 512:(half + 1) * 512], op=ALU.add)),
                                reads=[P[i], YAb[st]], writes=[YAb[st]])
                if sl == DFE // 512 - 1:
                    for s3 in range(nst_t):
                        st = ti * nst_t + s3
                        S.op("sp", (lambda e=e, st=st: nc.sync.dma_start(
                            out=ys_d[e * CAP + st * 128: e * CAP + (st + 1) * 128, :], in_=YA[:, st, :])),
                            reads=[YAb[st]], writes=[YSDb], dma_sem=yst[ti], waw=False)
                    for s3 in range(nst_t):
                        YAb[ti * nst_t + s3].r[yst[ti]] = S.cnt[yst[ti]]
        ffn_state["slab"] += len(all_slabs)

        B1l = B1[:].rearrange("p k n -> p (k n)").bitcast(F32)
        G0 = [B1l[:, r * 1024:(r + 1) * 1024] for r in range(2)]
        G1 = [B1l[:, 2048 + r * 1024: 2048 + (r + 1) * 1024] for r in range(2)]
        RR = [B1l[:, 4096 + r * 1024: 4096 + (r + 1) * 1024] for r in range(2)]
        GBC = B1l[:, 6144:7168]
        G0b = [Buf("g00"), Buf("g01")]
        G1b = [Buf("g10"), Buf("g11")]
        RRb = [Buf("rr0"), Buf("rr1")]
        GBCb = Buf("gbc")
        gsm = [scs[0], scs[1]]
        osem = [S.new_sem("do0"), S.new_sem("do1")]
        sgb = sc
        SSQ = SCR[:, 0:16]
        RS2 = SCR[:, 16:32]
        SSb = Buf("ssq")
        S.barrier()
        S.op("sp", lambda: nc.sync.dma_start(out=GBC, in_=gbc_d), writes=[GBCb], dma_sem=sgb)
        for r in range(2):
            S.op("pool", (lambda r=r: nc.gpsimd.memset(G0[r], 0.0)), writes=[G0b[r]])
            S.op("pool", (lambda r=r: nc.gpsimd.memset(G1[r], 0.0)), writes=[G1b[r]])
        last = []
        for j in range(16):
            c0 = HALO + 128 * j
            r = j % 2
            S.op("pool", (lambda j=j, r=r: nc.gpsimd.indirect_dma_start(
                out=G0[r], out_offset=None, in_=ys_d, in_offset=bass.IndirectOffsetOnAxis(S0U[:, j:j + 1], 0),
                bounds_check=bnd["v"], oob_is_err=False)),
                reads=[YSDb, IDXb], writes=[G0b[r]], dma_sem=gsm[r])
            S.op("pool", (lambda j=j, r=r: nc.gpsimd.indirect_dma_start(
                out=G1[r], out_offset=None, in_=ys_d, in_offset=bass.IndirectOffsetOnAxis(S1U[:, j:j + 1], 0),
                bounds_check=bnd["v"], oob_is_err=False)),
                reads=[YSDb, IDXb], writes=[G1b[r]], dma_sem=gsm[r])
            G0b[r].w = dict(G1b[r].w)
            for half in range(2):
                i = state["bank"]
                state["bank"] = (i + 1) % 8
                for kk in range(4):
                    S.op("pe", (lambda i=i, kk=kk, half=half, c0=c0: nc.tensor.matmul(
                        ps[i][:, kk * 128:(kk + 1) * 128], lhsT=H[:, 4 * half + kk, c0:c0 + 128], rhs=identf[:],
                        start=True, stop=True)),
                        reads=Hb + [Bc] if kk in (0, 3) else (), writes=[P[i]] if kk in (0, 3) else (), inc=(kk == 3))
                S.op("dve", (lambda i=i, j=j, r=r, half=half: nc.vector.scalar_tensor_tensor(
                    out=RR[r][:, half * 512:(half + 1) * 512], in0=G0[r][:, half * 512:(half + 1) * 512],
                    scalar=RT[:, j, 12:13], in1=ps[i][:, :], op0=ALU.mult, op1=ALU.add)),
                    reads=[P[i], G0b[r]] + allrt, writes=[RRb[r]], waw=(half == 0))
            S.op("dve", (lambda j=j, r=r: nc.vector.scalar_tensor_tensor(
                out=RR[r], in0=G1[r], scalar=RT[:, j, 13:14], in1=RR[r], op0=ALU.mult, op1=ALU.add)),
                reads=[G1b[r], RRb[r]] + allrt, writes=[RRb[r]])
            S.op("act", (lambda j=j, r=r: nc.scalar.activation(
                out=G0[r], in_=RR[r], func=AF.Square, accum_out=SSQ[:, j:j + 1])),
                reads=[RRb[r]], writes=[G0b[r], SSb])
            S.op("act", (lambda j=j: nc.scalar.activation(
                out=RS2[:, j:j + 1], in_=SSQ[:, j:j + 1], func=AF.Sqrt, bias=eps_ap(RMS_EPS), scale=1.0 / D)),
                reads=[SSb, Bc], writes=[SSb])
            S.op("dve", (lambda j=j: nc.vector.reciprocal(out=RS2[:, j:j + 1], in_=RS2[:, j:j + 1])),
                 reads=[SSb], writes=[SSb])
            S.op("dve", (lambda j=j, r=r: nc.vector.scalar_tensor_tensor(
                out=G1[r], in0=RR[r], scalar=RS2[:, j:j + 1], in1=GBC, op0=ALU.mult, op1=ALU.mult)),
                reads=[RRb[r], SSb, GBCb], writes=[G1b[r]])
            tok = S.op("sp", (lambda j=j, r=r: nc.sync.dma_start(out=out_d[128 * j:128 * (j + 1), :], in_=G1[r])),
                       reads=[G1b[r]], dma_sem=osem[r])
            last.append(tok)
        S.op("sp", None, after=last, inc=False)
        print("sbuf bytes remaining:", nc.sbuf_bytes_remaining)
        S.run_block()
    return nc


_CACHE = {}


def _prep_inputs(x, meta_tokens, conv_w_pw1, conv_b_pw1, conv_w_dw, conv_b_dw, conv_ln_g, conv_ln_b,
                 conv_w_pw2, conv_b_pw2, pool_w_group, pool_scale, ffn_w_gate, ffn_w_up, ffn_w_down,
                 moe_w_router, moe_w_gate, moe_w_up, moe_w_down, mix_norm_g, ffn_norm_g, final_norm_g):
    f = lambda a: np.ascontiguousarray(np.asarray(a, dtype=np.float32))

    def pk(v):
        v = np.asarray(v, dtype=np.float32).reshape(-1, 128)
        return v.T

    cols = [pk(mix_norm_g[0]), pk(mix_norm_g[1]), pk(ffn_norm_g[0]), pk(ffn_norm_g[1]), pk(final_norm_g),
            pk(conv_b_pw1[0]), pk(conv_b_dw[0]), pk(conv_ln_g[0]), pk(conv_ln_b[0]), pk(conv_b_pw2[0]),
            pk(pool_scale[0])]
    wdw = np.asarray(conv_w_dw[0], dtype=np.float32)
    wdw = wdw.reshape(CONVW, KC, 128).transpose(2, 1, 0).reshape(128, KC * CONVW)
    cvec = f(np.concatenate(cols + [wdw], axis=1))
    assert cvec.shape == (128, NCV), cvec.shape
    wr = np.asarray(moe_w_router[0], dtype=np.float32).reshape(KC, 128, NE).transpose(1, 0, 2).reshape(128, KC * NE)
    xe = np.concatenate([np.zeros((32, D), np.float32), np.asarray(meta_tokens, np.float32),
                         np.asarray(x[0], np.float32)], axis=0)
    shared = {
        "cvec": cvec, "wr": f(wr), "ident": np.eye(128, dtype=np.float32),
        "tri": np.triu(np.ones((128, 128), np.float32), 1),
        "gbc": f(np.broadcast_to(np.asarray(final_norm_g, np.float32)[None, :], (128, D))),
        "w_pw1": f(conv_w_pw1[0]), "w_pw2": f(conv_w_pw2[0]), "pool_w": f(pool_w_group[0]),
        "ffn_wg": f(ffn_w_gate[0]), "ffn_wu": f(ffn_w_up[0]), "ffn_wd": f(ffn_w_down[0]),
        "moe_wg": f(moe_w_gate[0]), "moe_wu": f(moe_w_up[0]), "moe_wd": f(moe_w_down[0]),
    }
    in_maps = []
    for c in range(NCORES):
        m = dict(shared)
        m["xT"] = f(xe[TOK * c: TOK * c + TW].T)
        um = np.ones((128, HALO), np.float32)
        if c == 0:
            um[:, :32] = 0.0
        m["umask"] = um
        in_maps.append(m)
    return in_maps


def kernel(**inputs):
    if "nc" not in _CACHE:
        _CACHE["nc"] = build_program()
    nc = _CACHE["nc"]
    in_maps = _prep_inputs(**inputs)
    res = run_bass_kernel_spmd(nc, in_maps, core_ids=list(range(NCORES)))
    outs = [np.asarray(r["out"]) for r in res.results]
    out = np.concatenate(outs, axis=0).reshape(1, SEQ, D).astype(np.float32)
    return out
```

```python
import numpy as np
import concourse.bass as bass
import concourse.mybir as mybir
from concourse.bass_utils import run_bass_kernel_spmd
from contextlib import ExitStack

F32 = mybir.dt.float32
BF16 = mybir.dt.bfloat16
ALU = mybir.AluOpType
AF = mybir.ActivationFunctionType

NCORES = 8
D = 1024
KC = 8
SEQ = 16384
NMETA = 16
TOK = SEQ // NCORES
HALO = 48
TW = TOK + HALO
DFF = 2816
DFE = 3584
NE = 8
CONVW = 31
RMS_EPS = 1e-6
LN_EPS = 1e-5
POOLW = (2, 4, 8, 16)
CAP = 768
NST = CAP // 128
NSLOT = NE * CAP
U32 = mybir.dt.uint32

TT0 = [(0, 432), (432, 416), (848, 416), (1264, 416), (1680, 416)]
TT1 = [(32, 400)] + TT0[1:]
TT2 = [(48, 384)] + TT0[1:]
TT3 = [(48 + 512 * i, 512) for i in range(4)]

CV = {}
_o = 0
for _n, _w in (("mix_g0", 8), ("mix_g1", 8), ("ffn_g0", 8), ("ffn_g1", 8), ("fin_g", 8),
               ("b_pw1", 16), ("b_dw", 8), ("ln_g", 8), ("ln_b", 8), ("b_pw2", 8),
               ("pool_s", 8), ("w_dw", 8 * CONVW)):
    CV[_n] = _o
    _o += _w
NCV = _o


class Buf:
    __slots__ = ("name", "w", "r")

    def __init__(self, name):
        self.name = name
        self.w = {}
        self.r = {}


class Sched:
    ENGS = ("pe", "act", "dve", "pool", "sp")

    def __init__(self, nc, es):
        self.nc = nc
        self.es = es
        self.eng = {"pe": nc.tensor, "act": nc.scalar, "dve": nc.vector,
                    "pool": nc.gpsimd, "sp": nc.sync}
        self.stream = {e: [] for e in self.ENGS}
        self.sems = {}
        self.cnt = {}
        self.seen = {e: {} for e in self.ENGS}
        for e in self.ENGS:
            self.new_sem(e)

    def new_sem(self, name):
        self.sems[name] = self.es.enter_context(self.nc.semaphore("s_" + name))
        self.cnt[name] = 0
        return name

    def op(self, e, fn, reads=(), writes=(), dma_sem=None, inc=True, after=(), waw=True):
        d = {}

        def add(tok):
            if tok is None:
                return
            s, v = tok
            if d.get(s, 0) < v:
                d[s] = v
        for b in reads:
            for s, v in b.w.items():
                add((s, v))
        for b in writes:
            if waw:
                for s, v in b.w.items():
                    if not (dma_sem is not None and s == dma_sem):
                        add((s, v))
            for s, v in b.r.items():
                add((s, v))
        for tok in after:
            add(tok)
        waits = []
        seen = self.seen[e]
        for s, v in d.items():
            if e == "pe" and s == "pe":
                continue
            if seen.get(s, 0) < v:
                waits.append((s, v))
                seen[s] = v
        if not inc:
            self.stream[e].append((waits, fn, None, 0))
            return None
        if dma_sem is None:
            s, n = e, 1
        else:
            s, n = dma_sem, 16
        self.cnt[s] += n
        tok = (s, self.cnt[s])
        self.stream[e].append((waits, fn, s, n))
        for b in writes:
            if waw:
                b.w = {s: tok[1]}
                b.r = {}
            else:
                b.w[s] = tok[1]
        for b in reads:
            if b.r.get(s, 0) < tok[1]:
                b.r[s] = tok[1]
        return tok

    def barrier(self):
        snap = dict(self.cnt)
        for e in self.ENGS:
            waits = []
            for s, v in snap.items():
                if v == 0 or (e == "pe" and s == "pe"):
                    continue
                if self.seen[e].get(s, 0) < v:
                    waits.append((s, v))
                    self.seen[e][s] = v
            if waits:
                self.stream[e].append((waits, None, None, 0))

    def replay(self, e):
        eng = self.eng[e]
        for waits, fn, s, n in self.stream[e]:
            for ws, wv in waits:
                eng.wait_ge(self.sems[ws], wv)
            if fn is None:
                continue
            ins = fn()
            if s is not None:
                ins.then_inc(self.sems[s], n)

    def run_block(self):
        nc = self.nc
        with nc.Block() as block:
            @block.tensor
            def _(t):
                self.replay("pe")

            @block.scalar
            def _(t):
                self.replay("act")

            @block.vector
            def _(t):
                self.replay("dve")

            @block.gpsimd
            def _(t):
                self.replay("pool")

            @block.sync
            def _(t):
                self.replay("sp")


def build_program(debug=False):
    nc = bass.Bass("TRN2", target_bir_lowering=False)
    dbg_t = [nc.dram_tensor(f"dbg{i}", [D, TW], F32, kind="ExternalOutput").ap() for i in range(4)] if debug else []

    def din(name, shape):
        return nc.dram_tensor(name, list(shape), F32, kind="ExternalInput").ap()

    xT = din("xT", (D, TW))
    umask_d = din("umask", (128, HALO))
    cvec_d = din("cvec", (128, NCV))
    wr_d = din("wr", (128, KC * NE))
    ident_d = din("ident", (128, 128))
    w_pw1 = din("w_pw1", (D, 2 * D))
    w_pw2 = din("w_pw2", (D, D))
    pool_w = din("pool_w", (4, 256, 256))
    ffn_wg = din("ffn_wg", (D, DFF))
    ffn_wu = din("ffn_wu", (D, DFF))
    ffn_wd = din("ffn_wd", (DFF, D))
    moe_wg = din("moe_wg", (NE, D, DFE))
    moe_wu = din("moe_wu", (NE, D, DFE))
    moe_wd = din("moe_wd", (NE, DFE, D))
    tri_d = din("tri", (128, 128))
    gbc_d = din("gbc", (128, D))
    out_d = nc.dram_tensor("out", [TOK, D], F32, kind="ExternalOutput").ap()
    xs_d = nc.dram_tensor("xs_scratch", [NSLOT, D], BF16).ap()
    ys_d = nc.dram_tensor("ys_scratch", [NSLOT, D], F32).ap()

    with ExitStack() as es:
        S = Sched(nc, es)

        def sb(name, shape, dt):
            return es.enter_context(nc.sbuf_tensor("sb_" + name, list(shape), dt))

        H = sb("H", (128, KC, TW), F32)
        B1 = sb("B1", (128, KC, TW), BF16)
        B2 = sb("B2", (128, KC * TW), BF16)
        WP1 = [sb(f"wp1_{b}", (128, KC, 512), BF16) for b in range(2)]
        WP2 = [sb(f"wp2_{b}", (128, KC, 512), BF16) for b in range(2)]
        WP3 = [sb(f"wp3_{b}", (128, 4, 1024), BF16) for b in range(2)]
        R1 = sb("R1", (128, 4096), BF16)
        Z = sb("Z", (128, KC, 416), BF16)
        TMP = [sb(f"tmp{i}", (128, 512), F32) for i in range(5)]
        cvec = sb("cvec", (128, NCV), F32)
        wr = sb("wr", (128, KC, NE), F32)
        wrg = sb("wrg", (128, KC, NE), F32)
        identf = sb("identf", (128, 128), F32)
        identb = sb("identb", (128, 128), BF16)
        onesb = sb("onesb", (128, 128), BF16)
        onesf = sb("onesf", (128, 128), F32)
        umask = sb("umask", (128, HALO), F32)
        ps = [es.enter_context(nc.psum_tensor(f"ps{i}", [128, 512], F32)) for i in range(8)]

        Y = B1[:].bitcast(F32)
        U = B2[:].rearrange("p (k n) -> p k n", k=KC)
        B2f = B2[:].bitcast(F32)
        SQ = R1[:, 0:KC * 432].rearrange("p (k n) -> p k n", k=KC)
        Abuf = [R1[:, i * 2048:(i + 1) * 2048].rearrange("p (m n) -> p m n", m=4) for i in range(2)]
        CWe = [B2f[:, i * 2048:(i + 1) * 2048] for i in range(2)]
        RSall = B2f[:, 6144:8192]
        Zf = Z[:].rearrange("p k n -> p (k n)").bitcast(F32)
        RT = Zf[:, 0:640].rearrange("p (j c) -> p j c", j=16)
        Mf = Zf[:, 768:896].rearrange("p (j e) -> p j e", j=16)
        Mb = Zf[:, 896:960].bitcast(BF16)
        WITHIN = Zf[:, 960:1088]
        OFF = Zf[:, 1088:1216].rearrange("p (j e) -> p j e", j=16)
        SLV = Zf[:, 1216:1344].rearrange("p (j e) -> p j e", j=16)
        S0F = Zf[:, 1344:1360]
        S1F = Zf[:, 1360:1376]
        S0U = Zf[:, 1376:1392].bitcast(U32)
        S1U = Zf[:, 1392:1408].bitcast(U32)
        EC = Zf[:, 1440:1568].rearrange("p (j e) -> p j e", j=16)
        SCR = Zf[:, 1568:1664]
        trib = sb("trib", (128, 128), BF16)
        OUTb = [B2f[:, i * 3456:(i + 1) * 3456].rearrange("p (k n) -> p k n", k=KC) for i in range(2)]
        assert tuple(Y.shape) == (128, KC, TW // 2), Y.shape

        P = [Buf(f"ps{i}") for i in range(8)]
        state = {"bank": 0, "dq": 0, "r1": "sq"}

        def cv(name, j=0):
            c = CV[name] + j
            return cvec[:, c:c + 1]

        def mm_group(pairs, n, reads, rows=128):
            i = state["bank"]
            state["bank"] = (i + 1) % 8
            L = len(pairs)
            for j, (l, r) in enumerate(pairs):
                edge = (j == 0 or j == L - 1)
                S.op("pe",
                     (lambda l=l, r=r, j=j, i=i: nc.tensor.matmul(
                         ps[i][:rows, :n], lhsT=l, rhs=r, start=(j == 0), stop=(j == L - 1))),
                     reads=reads if edge else (), writes=[P[i]] if edge else (),
                     inc=(j == L - 1))
            return i

        Bc = Buf("consts")
        sc = S.new_sem("dconst")
        trif = TMP[3][:, 0:128]
        for dst, src in ((cvec[:], cvec_d), (wr[:].rearrange("p k e -> p (k e)"), wr_d),
                         (identf[:], ident_d), (umask[:], umask_d), (trif, tri_d)):
            S.op("sp", (lambda dst=dst, src=src: nc.sync.dma_start(out=dst, in_=src)),
                 writes=[Bc], dma_sem=sc)
        S.op("dve", lambda: nc.vector.memset(onesb[:], 1.0), writes=[Bc])
        S.op("dve", lambda: nc.vector.memset(onesf[:], 1.0), writes=[Bc])
        S.op("dve", lambda: nc.vector.tensor_copy(out=identb[:], in_=identf[:]), reads=[Bc], writes=[Bc])
        for k in range(KC):
            S.op("dve", (lambda k=k: nc.vector.tensor_scalar(
                out=wrg[:, k, :], in0=wr[:, k, :], scalar1=cv("ffn_g1", k), scalar2=None, op0=ALU.mult)),
                reads=[Bc], writes=[Bc])

        Hb = [Buf(f"H{t}") for t in range(5)]
        sx = S.new_sem("dx")
        dbg_sem = S.new_sem("ddbg") if debug else None

        def dump(i):
            if not debug:
                return
            S.barrier()
            for k in range(KC):
                S.op("sp", (lambda k=k, i=i: nc.sync.dma_start(out=dbg_t[i][k * 128:(k + 1) * 128, :], in_=H[:, k, :])),
                     reads=Hb, dma_sem=dbg_sem)
            S.barrier()
        xTv = xT.rearrange("(k p) n -> p k n", p=128)
        sxs = [sx] + [S.new_sem(f"dx{t}") for t in range(1, 5)]
        for t, (o, n) in enumerate(TT0):
            S.op("sp", (lambda o=o, n=n: nc.sync.dma_start(out=H[:, :, o:o + n], in_=xTv[:, :, o:o + n])),
                 writes=[Hb[t]], dma_sem=sxs[t])

        bnd = {}

        def _mk_bound():
            reg = nc.gpsimd.alloc_register("slot_bound")
            ins = nc.gpsimd.reg_mov(reg, NSLOT - 1)
            bnd["v"] = nc.gpsimd.snap(reg)
            return ins
        S.op("pool", _mk_bound, inc=False)

        XS0b = Buf("xs0")
        ZTb = Buf("zt")
        ZT = TMP[4][:, :].bitcast(BF16)
        szf = S.new_sem("dzf")
        S.op("dve", lambda: nc.vector.memset(TMP[4][:, :], 0.0), writes=[ZTb])
        for q in range(NSLOT // 128):
            S.op("sp", (lambda q=q: nc.sync.dma_start(out=xs_d[q * 128:(q + 1) * 128, :], in_=ZT)),
                 reads=[ZTb], writes=[XS0b], dma_sem=szf)
        S.op("dve", lambda: nc.vector.tensor_copy(out=trib[:], in_=trif[:]), reads=[Bc], writes=[Bc])

        WBb = [Buf("wb0"), Buf("wb1")]
        wsem = [S.new_sem("dw0"), S.new_sem("dw1")]

        def wload(b, parts):
            for dst, src in parts:
                S.op("pool", (lambda dst=dst, src=src: nc.gpsimd.dma_start(out=dst, in_=src)),
                     writes=[WBb[b]], dma_sem=wsem[b])

        def kview(ap):
            return ap.rearrange("(k p) n -> p k n", p=128)

        SQb, RSb = Buf("sq"), Buf("rs")

        def rmsnorm(tiles, tbufs_in, gname, out_fn, out_bufs, eps=RMS_EPS, rs_fn=None, rs_buf=None):
            if state["r1"] != "sq":
                S.barrier()
                state["r1"] = "sq"
            for ti, (o, n) in enumerate(tiles):
                hb = tbufs_in[ti]
                rsap = (lambda o=o, n=n: TMP[0][:, :n]) if rs_fn is None else (lambda o=o, n=n: rs_fn(o, n))
                rsb = RSb if rs_buf is None else rs_buf
                S.op("act", (lambda o=o, n=n: nc.scalar.activation(
                    out=SQ[:, :, :n], in_=H[:, :, o:o + n], func=AF.Square)),
                    reads=[hb], writes=[SQb])
                i = mm_group([(onesb[:], SQ[:, k, :n]) for k in range(KC)], n, [Bc, SQb])
                S.op("act", (lambda i=i, n=n, rsap=rsap: nc.scalar.activation(
                    out=rsap(), in_=ps[i][:, :n], func=AF.Sqrt, bias=eps_ap(eps), scale=1.0 / D)),
                    reads=[P[i], Bc], writes=[rsb])
                S.op("dve", (lambda rsap=rsap: nc.vector.reciprocal(out=rsap(), in_=rsap())),
                     reads=[rsb], writes=[rsb])
                for k in range(KC):
                    S.op("dve", (lambda k=k, o=o, n=n, rsap=rsap: nc.vector.scalar_tensor_tensor(
                        out=out_fn(k, o, n), in0=H[:, k, o:o + n], scalar=cv(gname, k),
                        in1=rsap(), op0=ALU.mult, op1=ALU.mult)),
                        reads=[hb, rsb, Bc], writes=[out_bufs[ti]])

        epsc = sb("epsc", (128, 2), F32)
        S.op("dve", lambda: nc.vector.memset(epsc[:, 0:1], RMS_EPS), writes=[Bc])
        S.op("dve", lambda: nc.vector.memset(epsc[:, 1:2], LN_EPS), writes=[Bc])

        def eps_ap(eps):
            return epsc[:, 0:1] if eps == RMS_EPS else epsc[:, 1:2]

        HNb = [Buf(f"HN{t}") for t in range(5)]
        for s in range(2):
            wload(s, [(WP1[s][:], kview(w_pw1[:, 512 * s:512 * s + 512])),
                      (WP2[s][:], kview(w_pw1[:, D + 512 * s:D + 512 * s + 512]))])
        rmsnorm(TT0, Hb, "mix_g0", lambda k, o, n: B1[:, k, o:o + n], HNb)

        Ub = Buf("U")
        SIGb = [Buf("sig0"), Buf("sig1")]
        sgi = 0
        for s in range(2):
            for ti, (o, n) in enumerate(TT0):
                for mc in range(4):
                    ch = 4 * s + mc
                    ia = mm_group([(WP1[s][:, k, mc * 128:(mc + 1) * 128], B1[:, k, o:o + n]) for k in range(KC)],
                                  n, [WBb[s], HNb[ti]])
                    ig = mm_group([(WP2[s][:, k, mc * 128:(mc + 1) * 128], B1[:, k, o:o + n]) for k in range(KC)],
                                  n, [WBb[s], HNb[ti]])
                    sg = sgi % 2
                    sgi += 1
                    S.op("act", (lambda ig=ig, n=n, ch=ch, sg=sg: nc.scalar.activation(
                        out=TMP[1 + sg][:, :n], in_=ps[ig][:, :n], func=AF.Sigmoid, bias=cv("b_pw1", 8 + ch))),
                        reads=[P[ig], Bc], writes=[SIGb[sg]])
                    S.op("dve", (lambda ia=ia, n=n, ch=ch, sg=sg, o=o: nc.vector.scalar_tensor_tensor(
                        out=U[:, ch, o:o + n], in0=ps[ia][:, :n], scalar=cv("b_pw1", ch),
                        in1=TMP[1 + sg][:, :n], op0=ALU.add, op1=ALU.mult)),
                        reads=[P[ia], SIGb[sg], Bc], writes=[Ub])
        for k in range(KC):
            S.op("dve", (lambda k=k: nc.vector.tensor_tensor(
                out=U[:, k, 0:HALO], in0=U[:, k, 0:HALO], in1=umask[:], op=ALU.mult)),
                reads=[Ub, Bc], writes=[Ub])

        S.barrier()

        W2b = [Buf("w2_0"), Buf("w2_1")]
        w2sem = [S.new_sem("dw2_0"), S.new_sem("dw2_1")]
        for s in range(2):
            S.op("pool", (lambda s=s: nc.gpsimd.dma_start(
                out=WP1[s][:], in_=kview(w_pw2[:, 512 * s:512 * s + 512]))),
                writes=[W2b[s]], dma_sem=w2sem[s])

        DGb = [Buf("dg0"), Buf("dg1")]
        Yb = Buf("Y")
        Zb = Buf("Z")
        MUb, M2b = Buf("mu"), ZTb
        groups = [[0, 1], [2, 3], [4]]
        dgi = 0
        for grp in groups:
            gbase = TT1[grp[0]][0]
            for c in range(KC):
                db = dgi % 2
                dgi += 1
                dg = WP3[db][:].rearrange("p a b -> p (a b)")
                for k in range(CONVW):
                    wcol = cvec[:, CV["w_dw"] + c * CONVW + k: CV["w_dw"] + c * CONVW + k + 1]
                    if k % 3 == 0:
                        S.op("pool", (lambda k=k, dg=dg, wcol=wcol: nc.gpsimd.tensor_scalar(
                            out=dg[:, k * 128:(k + 1) * 128], in0=identf[:], scalar1=wcol,
                            scalar2=0.0, op0=ALU.mult, op1=ALU.add)),
                            reads=[Bc], writes=[DGb[db]], waw=False)
                    else:
                        S.op("dve", (lambda k=k, dg=dg, wcol=wcol: nc.vector.tensor_scalar(
                            out=dg[:, k * 128:(k + 1) * 128], in0=identf[:], scalar1=wcol,
                            scalar2=None, op0=ALU.mult)),
                            reads=[Bc], writes=[DGb[db]], waw=False)
                for ti in grp:
                    o, n = TT1[ti]
                    i = mm_group([(dg[:, k * 128:(k + 1) * 128], U[:, c, o - 30 + k: o - 30 + k + n])
                                  for k in range(CONVW)], n, [DGb[db], Ub])
                    S.op("act", (lambda i=i, n=n, c=c, yo=o - gbase: nc.scalar.activation(
                        out=Y[:, c, yo:yo + n], in_=ps[i][:, :n], func=AF.Identity, bias=cv("b_dw", c))),
                        reads=[P[i], Bc], writes=[Yb])
            for ti in grp:
                o, n = TT1[ti]
                yo = o - gbase
                imu = mm_group([(onesf[:], Y[:, k, yo:yo + n]) for k in range(KC)], n, [Bc, Yb])
                S.op("act", (lambda yo=yo, n=n: nc.scalar.activation(
                    out=SQ[:, :, :n], in_=Y[:, :, yo:yo + n], func=AF.Square)),
                    reads=[Yb], writes=[SQb])
                isq = mm_group([(onesb[:], SQ[:, k, :n]) for k in range(KC)], n, [Bc, SQb])
                S.op("dve", (lambda imu=imu, n=n: nc.vector.tensor_scalar(
                    out=TMP[3][:, :n], in0=ps[imu][:, :n], scalar1=1.0 / D, scalar2=None, op0=ALU.mult)),
                    reads=[P[imu]], writes=[MUb])
                S.op("dve", (lambda n=n: nc.vector.tensor_tensor(
                    out=TMP[4][:, :n], in0=TMP[3][:, :n], in1=TMP[3][:, :n], op=ALU.mult)),
                    reads=[MUb], writes=[M2b])
                S.op("dve", (lambda isq=isq, n=n: nc.vector.scalar_tensor_tensor(
                    out=TMP[4][:, :n], in0=ps[isq][:, :n], scalar=1.0 / D, in1=TMP[4][:, :n],
                    op0=ALU.mult, op1=ALU.subtract)),
                    reads=[P[isq], M2b], writes=[M2b])
                S.op("act", (lambda n=n: nc.scalar.activation(
                    out=TMP[0][:, :n], in_=TMP[4][:, :n], func=AF.Sqrt, bias=eps_ap(LN_EPS), scale=1.0)),
                    reads=[M2b, Bc], writes=[RSb])
                S.op("dve", (lambda n=n: nc.vector.reciprocal(out=TMP[0][:, :n], in_=TMP[0][:, :n])),
                     reads=[RSb], writes=[RSb])
                for c in range(KC):
                    S.op("dve", (lambda c=c, yo=yo, n=n: nc.vector.tensor_tensor(
                        out=Y[:, c, yo:yo + n], in0=Y[:, c, yo:yo + n], in1=TMP[3][:, :n], op=ALU.subtract)),
                        reads=[Yb, MUb], writes=[Yb])
                    S.op("dve", (lambda c=c, yo=yo, n=n: nc.vector.tensor_tensor(
                        out=Y[:, c, yo:yo + n], in0=Y[:, c, yo:yo + n], in1=TMP[0][:, :n], op=ALU.mult)),
                        reads=[Yb, RSb], writes=[Yb])
                    S.op("act", (lambda c=c, yo=yo, n=n: nc.scalar.activation(
                        out=Z[:, c, :n], in_=Y[:, c, yo:yo + n], func=AF.Silu,
                        bias=cv("ln_b", c), scale=cv("ln_g", c))),
                        reads=[Yb, Bc], writes=[Zb])
                for oc in range(KC):
                    s2, mc = divmod(oc, 4)
                    i = mm_group([(WP1[s2][:, k, mc * 128:(mc + 1) * 128], Z[:, k, :n]) for k in range(KC)],
                                 n, [W2b[s2], Zb])
                    S.op("dve", (lambda i=i, oc=oc, o=o, n=n: nc.vector.scalar_tensor_tensor(
                        out=H[:, oc, o:o + n], in0=ps[i][:, :n], scalar=cv("b_pw2", oc),
                        in1=H[:, oc, o:o + n], op0=ALU.add, op1=ALU.add)),
                        reads=[P[i], Bc, Hb[ti]], writes=[Hb[ti]])

        S.barrier()
        dump(0)

        SGb = [Buf("sg0"), Buf("sg1")]
        T2b = [Buf("t2a"), Buf("t2b")]
        Ab = [Buf("A0"), Buf("A1")]
        ffn_state = {"sg": 0, "a": 0, "slab": 0}

        def ffn(slabs, tiles, hnb, hb, cw=None):
            st = ffn_state
            if state["r1"] != "a":
                S.barrier()
                state["r1"] = "a"
            b0 = st["slab"] % 2
            wload(b0, slabs[0][1])
            for si, (wdt, _) in enumerate(slabs):
                b = (st["slab"] + si) % 2
                if si + 1 < len(slabs):
                    wload(1 - b, slabs[si + 1][1])
                nm = wdt // 128
                for ti, (o, n) in enumerate(tiles):
                    a = st["a"] % 2
                    st["a"] += 1
                    for mc in range(nm):
                        ig = mm_group([(WP1[b][:, k, mc * 128:(mc + 1) * 128], B1[:, k, o:o + n]) for k in range(KC)],
                                      n, [WBb[b], hnb[ti]])
                        iu = mm_group([(WP2[b][:, k, mc * 128:(mc + 1) * 128], B1[:, k, o:o + n]) for k in range(KC)],
                                      n, [WBb[b], hnb[ti]])
                        sg = st["sg"] % 2
                        st["sg"] += 1
                        S.op("act", (lambda ig=ig, n=n, sg=sg: nc.scalar.activation(
                            out=TMP[1 + sg][:, :n], in_=ps[ig][:, :n], func=AF.Silu)),
                            reads=[P[ig]], writes=[SGb[sg]])
                        if cw is None:
                            S.op("dve", (lambda iu=iu, n=n, sg=sg, a=a, mc=mc: nc.vector.tensor_tensor(
                                out=Abuf[a][:, mc, :n], in0=ps[iu][:, :n], in1=TMP[1 + sg][:, :n], op=ALU.mult)),
                                reads=[P[iu], SGb[sg]], writes=[Ab[a]])
                        else:
                            cwfn, cwb = cw
                            S.op("dve", (lambda n=n, sg=sg, o=o: nc.vector.tensor_tensor(
                                out=TMP[3 + sg][:, :n], in0=TMP[1 + sg][:, :n], in1=cwfn(o, n), op=ALU.mult)),
                                reads=[SGb[sg], cwb], writes=[T2b[sg]])
                            S.op("dve", (lambda iu=iu, n=n, sg=sg, a=a, mc=mc: nc.vector.tensor_tensor(
                                out=Abuf[a][:, mc, :n], in0=ps[iu][:, :n], in1=TMP[3 + sg][:, :n], op=ALU.mult)),
                                reads=[P[iu], T2b[sg]], writes=[Ab[a]])
                    for oc in range(KC):
                        i = mm_group([(WP3[b][:, mc, oc * 128:(oc + 1) * 128], Abuf[a][:, mc, :n]) for mc in range(nm)],
                                     n, [WBb[b], Ab[a]])
                        S.op("dve", (lambda i=i, oc=oc, o=o, n=n: nc.vector.tensor_tensor(
                            out=H[:, oc, o:o + n], in0=ps[i][:, :n], in1=H[:, oc, o:o + n], op=ALU.add)),
                            reads=[P[i], hb[ti]], writes=[hb[ti]])
            st["slab"] += len(slabs)

        def ffn_slabs(wg, wu, wd, dff):
            out = []
            off = 0
            while off < dff:
                wdt = min(512, dff - off)
                nm = wdt // 128
                out.append((wdt, off))
                off += wdt
            res = []
            for wdt, off in out:
                nm = wdt // 128

                def mk(b, wdt=wdt, off=off, nm=nm):
                    return [(WP1[b][:, :, :wdt], kview(wg[:, off:off + wdt])),
                            (WP2[b][:, :, :wdt], kview(wu[:, off:off + wdt])),
                            (WP3[b][:, :nm, :], wd[off:off + wdt, :].rearrange("(m p) n -> p m n", p=128))]
                res.append((wdt, mk))
            return res

        def run_ffn(wg, wu, wd, dff, tiles, hnb, hb, cw=None):
            sl = ffn_slabs(wg, wu, wd, dff)
            base = ffn_state["slab"]
            slabs = [(wdt, mk((base + si) % 2)) for si, (wdt, mk) in enumerate(sl)]
            ffn(slabs, tiles, hnb, hb, cw)

        rmsnorm(TT1, Hb, "ffn_g0", lambda k, o, n: B1[:, k, o:o + n], HNb)
        run_ffn(ffn_wg, ffn_wu, ffn_wd, DFF, TT1, HNb, Hb)

        dump(1)
        spw = S.new_sem("dpw")
        PW = WP3[0][:].rearrange("p a b -> p (a b)")[:, 0:2048].rearrange("p (g k n) -> p g k n", g=4, k=2)
        for g in range(4):
            S.op("pool", (lambda g=g: nc.gpsimd.dma_start(
                out=PW[:, g, :, :], in_=pool_w[g].rearrange("(k p) n -> p k n", p=128))),
                writes=[WBb[0]], dma_sem=spw)
        rmsnorm(TT1, Hb, "mix_g1", lambda k, o, n: B1[:, k, o:o + n], HNb)
        W1f = WP1[0][:].rearrange("p a b -> p (a b)")
        PWS = W1f[:, 0:2048].rearrange("p (g k n) -> p g k n", g=4, k=2)
        PWN = W1f[:, 2048:4096].rearrange("p (g k n) -> p g k n", g=4, k=2)
        for g in range(4):
            S.op("dve", (lambda g=g: nc.vector.tensor_scalar(
                out=PWS[:, g, :, :], in0=PW[:, g, :, :], scalar1=1.0 / POOLW[g], scalar2=None, op0=ALU.mult)),
                reads=[WBb[0]], writes=[WBb[0]])
        S.op("dve", lambda: nc.vector.tensor_scalar(
            out=W1f[:, 2048:4096], in0=WP3[0][:].rearrange("p a b -> p (a b)")[:, 0:2048],
            scalar1=-1.0, scalar2=None, op0=ALU.mult), reads=[WBb[0]], writes=[WBb[0]])
        for ti, (o, n) in enumerate(TT2):
            rd = [HNb[ti]] + ([HNb[ti - 1]] if ti > 0 else [])
            for g in range(4):
                w = POOLW[g]
                for oc in range(2):
                    ch = 2 * g + oc
                    pairs = []
                    for kc in range(2):
                        for dd in range(w):
                            pairs.append((PWS[:, g, kc, oc * 128:(oc + 1) * 128], B1[:, 2 * g + kc, o - dd:o - dd + n]))
                        pairs.append((PWN[:, g, kc, oc * 128:(oc + 1) * 128], B1[:, 2 * g + kc, o:o + n]))
                    i = mm_group(pairs, n, [WBb[0]] + rd)
                    S.op("dve", (lambda i=i, ch=ch, o=o, n=n: nc.vector.scalar_tensor_tensor(
                        out=H[:, ch, o:o + n], in0=ps[i][:, :n], scalar=cv("pool_s", ch), in1=H[:, ch, o:o + n],
                        op0=ALU.mult, op1=ALU.add)),
                        reads=[P[i], Hb[ti], Bc], writes=[Hb[ti]])

        dump(2)
        RSAb = Buf("rsall")
        rmsnorm(TT2, Hb, "ffn_g1", lambda k, o, n: B1[:, k, o:o + n], HNb,
                rs_fn=lambda o, n: RSall[:, o - HALO:o - HALO + n], rs_buf=RSAb)
        RTb = [Buf("rt")] * 16
        rb = RTb[0]
        ir = state["bank"]
        state["bank"] = (ir + 1) % 8
        for j in range(16):
            c0 = HALO + 128 * j
            for k in range(KC):
                first = (j == 0 and k == 0)
                S.op("pe", (lambda j=j, k=k, c0=c0: nc.tensor.matmul(
                    ps[ir][:, 8 * j:8 * j + 8], lhsT=H[:, k, c0:c0 + 128], rhs=wrg[:, k, :],
                    start=(k == 0), stop=(k == KC - 1))),
                    reads=Hb + [Bc] if first else (), writes=[P[ir]] if first else (), inc=False)
            last = (j == 15)
            S.op("pe", (lambda j=j: nc.tensor.matmul(
                ps[ir][:, 128 + 2 * j:128 + 2 * j + 2], lhsT=RSall[:, 128 * j:128 * j + 128], rhs=identf[:, 0:2],
                start=True, stop=True)),
                reads=[RSAb, Bc] + Hb if (j == 0 or last) else (), writes=[P[ir]] if last else (), inc=last)
        RAW = RT[:, :, 0:8]
        S.op("dve", lambda: nc.vector.tensor_copy(
            out=RAW, in_=ps[ir][:, 0:128].rearrange("p (j e) -> p j e", j=16)), reads=[P[ir]], writes=[rb])
        S.op("dve", lambda: nc.vector.tensor_copy(
            out=RT[:, :, 8], in_=ps[ir][:, 128:160].rearrange("p (j c) -> p j c", j=16)[:, :, 0]),
            reads=[P[ir]], writes=[rb])
        S.op("dve", lambda: nc.vector.tensor_reduce(out=RT[:, :, 9], in_=RAW, axis=mybir.AxisListType.X, op=ALU.max),
             reads=[rb], writes=[rb])
        def first_max_mask(src0, mcol, dst0):
            S.op("dve", lambda: nc.vector.memset(RT[:, :, 14], 0.0), reads=[rb], writes=[rb])
            for e in range(NE):
                S.op("dve", (lambda e=e: nc.vector.tensor_tensor(
                    out=RT[:, :, dst0 + e], in0=RT[:, :, src0 + e], in1=RT[:, :, mcol], op=ALU.is_equal)),
                    reads=[rb], writes=[rb])
                S.op("dve", (lambda e=e: nc.vector.scalar_tensor_tensor(
                    out=RT[:, :, dst0 + e], in0=RT[:, :, 14], scalar=1.0, in1=RT[:, :, dst0 + e],
                    op0=ALU.subtract, op1=ALU.mult)), reads=[rb], writes=[rb])
                S.op("dve", (lambda e=e: nc.vector.tensor_tensor(
                    out=RT[:, :, 14], in0=RT[:, :, 14], in1=RT[:, :, dst0 + e], op=ALU.subtract)),
                    reads=[rb], writes=[rb])
            S.op("dve", lambda: nc.vector.tensor_scalar(
                out=RT[:, :, dst0:dst0 + NE], in0=RT[:, :, dst0:dst0 + NE], scalar1=-1.0, scalar2=None, op0=ALU.mult),
                reads=[rb], writes=[rb])
        first_max_mask(0, 9, 16)
        S.op("dve", lambda: nc.vector.scalar_tensor_tensor(
            out=RT[:, :, 24:32], in0=RT[:, :, 16:24], scalar=-1.0e30, in1=RAW, op0=ALU.mult, op1=ALU.add),
            reads=[rb], writes=[rb])
        S.op("dve", lambda: nc.vector.tensor_reduce(out=RT[:, :, 10], in_=RT[:, :, 24:32], axis=mybir.AxisListType.X,
                                                    op=ALU.max), reads=[rb], writes=[rb])
        first_max_mask(24, 10, 32)
        S.op("dve", lambda: nc.vector.tensor_tensor(out=RT[:, :, 11], in0=RT[:, :, 9], in1=RT[:, :, 10], op=ALU.subtract),
             reads=[rb], writes=[rb])
        S.op("dve", lambda: nc.vector.tensor_tensor(out=RT[:, :, 11], in0=RT[:, :, 11], in1=RT[:, :, 8], op=ALU.mult),
             reads=[rb], writes=[rb])
        S.op("act", lambda: nc.scalar.activation(out=RT[:, :, 12], in_=RT[:, :, 11], func=AF.Sigmoid),
             reads=[rb], writes=[rb])
        S.op("dve", lambda: nc.vector.tensor_scalar(out=RT[:, :, 13], in0=RT[:, :, 12], scalar1=-1.0, scalar2=1.0,
                                                    op0=ALU.mult, op1=ALU.add), reads=[rb], writes=[rb])

        IDXb = Buf("idx")
        allrt = RTb
        S.op("dve", lambda: nc.vector.tensor_tensor(out=Mf[:], in0=RT[:, :, 16:24], in1=RT[:, :, 32:40], op=ALU.add),
             reads=allrt, writes=[IDXb])
        S.op("dve", lambda: nc.vector.tensor_copy(out=Mb, in_=Mf[:].rearrange("p j e -> p (j e)")),
             reads=[IDXb], writes=[IDXb])
        iw = mm_group([(trib[:], Mb)], 128, [Bc, IDXb])
        ic = mm_group([(onesb[:], Mb)], 128, [Bc, IDXb])
        S.op("dve", lambda: nc.vector.memset(OFF[:, 0, :], 0.0), reads=[IDXb], writes=[IDXb])
        for j in range(1, 16):
            S.op("dve", (lambda j=j: nc.vector.tensor_tensor(
                out=OFF[:, j, :], in0=OFF[:, j - 1, :], in1=ps[ic][:, (j - 1) * NE:j * NE], op=ALU.add)),
                reads=[IDXb, P[ic]], writes=[IDXb])
        for e in range(NE):
            S.op("dve", (lambda e=e: nc.vector.memset(EC[:, :, e:e + 1], float(e * CAP))), reads=[IDXb], writes=[IDXb])
        Offl = OFF.rearrange("p j e -> p (j e)")
        SLVl = SLV.rearrange("p j e -> p (j e)")
        ECl = EC.rearrange("p j e -> p (j e)")
        S.op("dve", lambda: nc.vector.tensor_tensor(out=Offl, in0=Offl, in1=ps[iw][:, 0:128], op=ALU.add),
             reads=[IDXb, P[iw]], writes=[IDXb])
        S.op("dve", lambda: nc.vector.tensor_tensor(out=SLVl, in0=Offl, in1=ECl, op=ALU.add), reads=[IDXb], writes=[IDXb])
        S.op("dve", lambda: nc.vector.tensor_scalar(out=Offl, in0=Offl, scalar1=float(CAP), scalar2=1.0e6,
                                                    op0=ALU.is_ge, op1=ALU.mult), reads=[IDXb], writes=[IDXb])
        S.op("dve", lambda: nc.vector.tensor_tensor(out=SLVl, in0=SLVl, in1=Offl, op=ALU.add), reads=[IDXb], writes=[IDXb])
        for (msk, SF, SU, wc) in ((RT[:, :, 16:24], S0F, S0U, 12), (RT[:, :, 32:40], S1F, S1U, 13)):
            S.op("dve", (lambda msk=msk: nc.vector.tensor_tensor(out=Mf[:], in0=msk, in1=SLV, op=ALU.mult)),
                 reads=[IDXb] + allrt, writes=[IDXb])
            S.op("dve", (lambda SF=SF: nc.vector.tensor_reduce(out=SF, in_=Mf[:], axis=mybir.AxisListType.X, op=ALU.add)),
                 reads=[IDXb], writes=[IDXb])
            S.op("dve", (lambda SF=SF, SU=SU: nc.vector.tensor_copy(out=SU, in_=SF)), reads=[IDXb], writes=[IDXb])
            S.op("dve", (lambda SF=SF: nc.vector.tensor_scalar(out=SF, in0=SF, scalar1=float(NSLOT), scalar2=None,
                                                               op0=ALU.is_lt)), reads=[IDXb], writes=[IDXb])
            S.op("dve", (lambda SF=SF, wc=wc: nc.vector.tensor_tensor(out=RT[:, :, wc], in0=RT[:, :, wc], in1=SF, op=ALU.mult)),
                 reads=[IDXb] + allrt, writes=allrt)

        all_slabs = [(e, sl) for e in range(NE) for sl in range(DFE // 512)]

        def slab_parts(e, sl, b):
            off = 512 * sl
            return [(WP1[b][:], kview(moe_wg[e][:, off:off + 512])),
                    (WP2[b][:], kview(moe_wu[e][:, off:off + 512])),
                    (WP3[b][:], moe_wd[e][off:off + 512, :].rearrange("(m p) n -> p m n", p=128))]
        sbase = ffn_state["slab"]
        wload(sbase % 2, slab_parts(0, 0, sbase % 2))

        HT = [B2[:, r * 1024:(r + 1) * 1024] for r in range(4)]
        HTb = [Buf(f"ht{r}") for r in range(4)]
        scs = [S.new_sem(f"dsc{r}") for r in range(4)]
        XSb = Buf("xs")
        XSb.w = dict(XS0b.w)
        prev_sc = []
        for j in range(16):
            c0 = HALO + 128 * j
            r = j % 4
            for half in range(2):
                i = state["bank"]
                state["bank"] = (i + 1) % 8
                for kk in range(4):
                    S.op("pe", (lambda i=i, kk=kk, half=half, c0=c0: nc.tensor.matmul(
                        ps[i][:, kk * 128:(kk + 1) * 128], lhsT=B1[:, 4 * half + kk, c0:c0 + 128], rhs=identb[:],
                        start=True, stop=True)),
                        reads=HNb + [Bc] if kk in (0, 3) else (), writes=[P[i]] if kk in (0, 3) else (), inc=(kk == 3))
                if half == 0:
                    S.op("act", (lambda i=i, r=r: nc.scalar.activation(out=HT[r][:, 0:512], in_=ps[i][:, :], func=AF.Identity)),
                         reads=[P[i]], writes=[HTb[r]], waw=False)
                else:
                    S.op("dve", (lambda i=i, r=r: nc.vector.tensor_copy(out=HT[r][:, 512:1024], in_=ps[i][:, :])),
                         reads=[P[i]], writes=[HTb[r]], waw=False)
            for SU in (S0U, S1U):
                S.op("pool", (lambda SU=SU, j=j, r=r: nc.gpsimd.indirect_dma_start(
                    out=xs_d, out_offset=bass.IndirectOffsetOnAxis(SU[:, j:j + 1], 0), in_=HT[r], in_offset=None,
                    bounds_check=bnd["v"], oob_is_err=False)),
                    reads=[HTb[r], IDXb], writes=[XSb], dma_sem=scs[r], waw=False, after=list(prev_sc))
                prev_sc[:] = [(scs[r], S.cnt[scs[r]])]

        if state["r1"] != "a":
            S.barrier()
            state["r1"] = "a"
        XT = [B1[:].rearrange("p k n -> p (k n)")[:, 12288 + r * 1024: 12288 + (r + 1) * 1024] for r in range(3)]
        XTb = [Buf(f"xt{r}") for r in range(3)]
        xts = [S.new_sem(f"dxt{r}") for r in range(3)]
        XG = [B1[:].rearrange("p k n -> p (k n)")[:, g * 6144:(g + 1) * 6144].rearrange("p (k n) -> p k n", k=KC)
              for g in range(2)]
        XGb = [Buf("xg0"), Buf("xg1")]
        YA = B2f[:, 0:NST * 1024].rearrange("p (s n) -> p s n", s=NST)
        YAb = [Buf(f"ya{st}") for st in range(NST)]
        YSDb = Buf("ysd")
        yst = [S.new_sem("dys0"), S.new_sem("dys1")]
        xti = 0
        evi = 0
        gtiles = [(0, CAP // 2), (CAP // 2, CAP // 2)]
        nst_t = (CAP // 2) // 128
        def emit_xload(e):
            nonlocal xti, evi
            g = e % 2
            for st in range(NST):
                xr = xti % 3
                xti += 1
                S.op("sp", (lambda e=e, st=st, xr=xr: nc.sync.dma_start(
                    out=XT[xr], in_=xs_d[e * CAP + st * 128: e * CAP + (st + 1) * 128, :])),
                    reads=[XSb], writes=[XTb[xr]], dma_sem=xts[xr])
                for half in range(2):
                    i = state["bank"]
                    state["bank"] = (i + 1) % 8
                    for kk in range(4):
                        S.op("pe", (lambda i=i, kk=kk, half=half, xr=xr: nc.tensor.matmul(
                            ps[i][:, kk * 128:(kk + 1) * 128],
                            lhsT=XT[xr][:, (4 * half + kk) * 128:(4 * half + kk + 1) * 128], rhs=identb[:],
                            start=True, stop=True)),
                            reads=[XTb[xr], Bc] if kk in (0, 3) else (), writes=[P[i]] if kk in (0, 3) else (),
                            inc=(kk == 3))
                    dst = XG[g][:, 4 * half:4 * half + 4, st * 128:(st + 1) * 128]
                    srcv = ps[i][:, :].rearrange("p (a b) -> p a b", a=4)
                    evi += 1
                    if evi % 2:
                        S.op("act", (lambda dst=dst, srcv=srcv: nc.scalar.activation(out=dst, in_=srcv, func=AF.Identity)),
                             reads=[P[i]], writes=[XGb[g]], waw=False)
                    else:
                        S.op("dve", (lambda dst=dst, srcv=srcv: nc.vector.tensor_copy(out=dst, in_=srcv)),
                             reads=[P[i]], writes=[XGb[g]], waw=False)
        emit_xload(0)
        for si, (e, sl) in enumerate(all_slabs):
            b = (sbase + si) % 2
            if si + 1 < len(all_slabs):
                wload(1 - b, slab_parts(all_slabs[si + 1][0], all_slabs[si + 1][1], 1 - b))
            g = e % 2
            if sl == 2 and e + 1 < NE:
                emit_xload(e + 1)
            for ti, (o, n) in enumerate(gtiles):
                a = ffn_state["a"] % 2
                ffn_state["a"] += 1
                for mc in range(4):
                    ig = mm_group([(WP1[b][:, k, mc * 128:(mc + 1) * 128], XG[g][:, k, o:o + n]) for k in range(KC)],
                                  n, [WBb[b], XGb[g]])
                    iu = mm_group([(WP2[b][:, k, mc * 128:(mc + 1) * 128], XG[g][:, k, o:o + n]) for k in range(KC)],
                                  n, [WBb[b], XGb[g]])
                    sg = ffn_state["sg"] % 2
                    ffn_state["sg"] += 1
                    S.op("act", (lambda ig=ig, n=n, sg=sg: nc.scalar.activation(
                        out=TMP[1 + sg][:, :n], in_=ps[ig][:, :n], func=AF.Silu)),
                        reads=[P[ig]], writes=[SGb[sg]])
                    S.op("dve", (lambda iu=iu, n=n, sg=sg, a=a, mc=mc: nc.vector.tensor_tensor(
                        out=Abuf[a][:, mc, :n], in0=ps[iu][:, :n], in1=TMP[1 + sg][:, :n], op=ALU.mult)),
                        reads=[P[iu], SGb[sg]], writes=[Ab[a]])
                for s3 in range(nst_t):
                    st = ti * nst_t + s3
                    for half in range(2):
                        i = mm_group([(Abuf[a][:, mc, s3 * 128:(s3 + 1) * 128], WP3[b][:, mc, half * 512:(half + 1) * 512])
                                      for mc in range(4)], 512, [WBb[b], Ab[a]])
                        if sl == 0:
                            S.op("dve", (lambda i=i, st=st, half=half: nc.vector.tensor_copy(
                                out=YA[:, st, half * 512:(half + 1) * 512], in_=ps[i][:, :])),
                                reads=[P[i]], writes=[YAb[st]], waw=(half == 0))
                        else:
                            S.op("dve", (lambda i=i, st=st, half=half: nc.vector.tensor_tensor(
                                out=YA[:, st, half * 512:(half + 1) * 512], in0=ps[i][:, :],
                                in1=YA[:, st, half * 512:(half + 1) * 512], op=ALU.add)),
                                reads=[P[i], YAb[st]], writes=[YAb[st]])
                if sl == DFE // 512 - 1:
                    for s3 in range(nst_t):
                        st = ti * nst_t + s3
                        S.op("sp", (lambda e=e, st=st: nc.sync.dma_start(
                            out=ys_d[e * CAP + st * 128: e * CAP + (st + 1) * 128, :], in_=YA[:, st, :])),
                            reads=[YAb[st]], writes=[YSDb], dma_sem=yst[ti], waw=False)
                    for s3 in range(nst_t):
                        YAb[ti * nst_t + s3].r[yst[ti]] = S.cnt[yst[ti]]
        ffn_state["slab"] += len(all_slabs)

        B1l = B1[:].rearrange("p k n -> p (k n)").bitcast(F32)
        G0 = [B1l[:, r * 1024:(r + 1) * 1024] for r in range(2)]
        G1 = [B1l[:, 2048 + r * 1024: 2048 + (r + 1) * 1024] for r in range(2)]
        RR = [B1l[:, 4096 + r * 1024: 4096 + (r + 1) * 1024] for r in range(2)]
        GBC = B2f[:, 0:1024]
        OUTt = [B2f[:, 1024 + r * 1024: 1024 + (r + 1) * 1024] for r in range(2)]
        OUTtb = [Buf("outt0"), Buf("outt1")]
        G0b = [Buf("g00"), Buf("g01")]
        G1b = [Buf("g10"), Buf("g11")]
        RRb = [Buf("rr0"), Buf("rr1")]
        GBCb = Buf("gbc")
        gsm = [scs[0], scs[1]]
        osem = [S.new_sem("do0"), S.new_sem("do1")]
        sgb = sc
        SSQ = SCR[:, 0:16]
        RS2 = SCR[:, 16:32]
        SSb = Buf("ssq")
        S.barrier()
        S.op("sp", lambda: nc.sync.dma_start(out=GBC, in_=gbc_d), writes=[GBCb], dma_sem=sgb)
        for r in range(2):
            S.op("pool", (lambda r=r: nc.gpsimd.memset(G0[r], 0.0)), writes=[G0b[r]])
            S.op("pool", (lambda r=r: nc.gpsimd.memset(G1[r], 0.0)), writes=[G1b[r]])
        last = []
        for j in range(16):
            c0 = HALO + 128 * j
            r = j % 2
            S.op("pool", (lambda j=j, r=r: nc.gpsimd.indirect_dma_start(
                out=G0[r], out_offset=None, in_=ys_d, in_offset=bass.IndirectOffsetOnAxis(S0U[:, j:j + 1], 0),
                bounds_check=bnd["v"], oob_is_err=False)),
                reads=[YSDb, IDXb], writes=[G0b[r]], dma_sem=gsm[r])
            S.op("pool", (lambda j=j, r=r: nc.gpsimd.indirect_dma_start(
                out=G1[r], out_offset=None, in_=ys_d, in_offset=bass.IndirectOffsetOnAxis(S1U[:, j:j + 1], 0),
                bounds_check=bnd["v"], oob_is_err=False)),
                reads=[YSDb, IDXb], writes=[G1b[r]], dma_sem=gsm[r])
            G0b[r].w = dict(G1b[r].w)
            for half in range(2):
                i = state["bank"]
                state["bank"] = (i + 1) % 8
                for kk in range(4):
                    S.op("pe", (lambda i=i, kk=kk, half=half, c0=c0: nc.tensor.matmul(
                        ps[i][:, kk * 128:(kk + 1) * 128], lhsT=H[:, 4 * half + kk, c0:c0 + 128], rhs=identf[:],
                        start=True, stop=True)),
                        reads=Hb + [Bc] if kk in (0, 3) else (), writes=[P[i]] if kk in (0, 3) else (), inc=(kk == 3))
                S.op("dve", (lambda i=i, j=j, r=r, half=half: nc.vector.scalar_tensor_tensor(
                    out=RR[r][:, half * 512:(half + 1) * 512], in0=G0[r][:, half * 512:(half + 1) * 512],
                    scalar=RT[:, j, 12:13], in1=ps[i][:, :], op0=ALU.mult, op1=ALU.add)),
                    reads=[P[i], G0b[r]] + allrt, writes=[RRb[r]], waw=(half == 0))
            S.op("dve", (lambda j=j, r=r: nc.vector.scalar_tensor_tensor(
                out=RR[r], in0=G1[r], scalar=RT[:, j, 13:14], in1=RR[r], op0=ALU.mult, op1=ALU.add)),
                reads=[G1b[r], RRb[r]] + allrt, writes=[RRb[r]])
            S.op("act", (lambda j=j, r=r: nc.scalar.activation(
                out=OUTt[r], in_=RR[r], func=AF.Square, accum_out=SSQ[:, j:j + 1])),
                reads=[RRb[r]], writes=[OUTtb[r], SSb])
            S.op("act", (lambda j=j: nc.scalar.activation(
                out=RS2[:, j:j + 1], in_=SSQ[:, j:j + 1], func=AF.Sqrt, bias=eps_ap(RMS_EPS), scale=1.0 / D)),
                reads=[SSb, Bc], writes=[SSb])
            S.op("dve", (lambda j=j: nc.vector.reciprocal(out=RS2[:, j:j + 1], in_=RS2[:, j:j + 1])),
                 reads=[SSb], writes=[SSb])
            S.op("dve", (lambda j=j, r=r: nc.vector.scalar_tensor_tensor(
                out=OUTt[r], in0=RR[r], scalar=RS2[:, j:j + 1], in1=GBC, op0=ALU.mult, op1=ALU.mult)),
                reads=[RRb[r], SSb, GBCb], writes=[OUTtb[r]])
            tok = S.op("sp", (lambda j=j, r=r: nc.sync.dma_start(out=out_d[128 * j:128 * (j + 1), :], in_=OUTt[r])),
                       reads=[OUTtb[r]], dma_sem=osem[r])
            last.append(tok)
        S.op("sp", None, after=last, inc=False)
        print("sbuf bytes remaining:", nc.sbuf_bytes_remaining)
        S.run_block()
    return nc


_CACHE = {}


def _prep_inputs(x, meta_tokens, conv_w_pw1, conv_b_pw1, conv_w_dw, conv_b_dw, conv_ln_g, conv_ln_b,
                 conv_w_pw2, conv_b_pw2, pool_w_group, pool_scale, ffn_w_gate, ffn_w_up, ffn_w_down,
                 moe_w_router, moe_w_gate, moe_w_up, moe_w_down, mix_norm_g, ffn_norm_g, final_norm_g):
    f = lambda a: np.ascontiguousarray(np.asarray(a, dtype=np.float32))

    def pk(v):
        v = np.asarray(v, dtype=np.float32).reshape(-1, 128)
        return v.T

    cols = [pk(mix_norm_g[0]), pk(mix_norm_g[1]), pk(ffn_norm_g[0]), pk(ffn_norm_g[1]), pk(final_norm_g),
            pk(conv_b_pw1[0]), pk(conv_b_dw[0]), pk(conv_ln_g[0]), pk(conv_ln_b[0]), pk(conv_b_pw2[0]),
            pk(pool_scale[0])]
    wdw = np.asarray(conv_w_dw[0], dtype=np.float32)
    wdw = wdw.reshape(CONVW, KC, 128).transpose(2, 1, 0).reshape(128, KC * CONVW)
    cvec = f(np.concatenate(cols + [wdw], axis=1))
    assert cvec.shape == (128, NCV), cvec.shape
    wr = np.asarray(moe_w_router[0], dtype=np.float32).reshape(KC, 128, NE).transpose(1, 0, 2).reshape(128, KC * NE)
    xe = np.concatenate([np.zeros((32, D), np.float32), np.asarray(meta_tokens, np.float32),
                         np.asarray(x[0], np.float32)], axis=0)
    shared = {
        "cvec": cvec, "wr": f(wr), "ident": np.eye(128, dtype=np.float32),
        "tri": np.triu(np.ones((128, 128), np.float32), 1),
        "gbc": f(np.broadcast_to(np.asarray(final_norm_g, np.float32)[None, :], (128, D))),
        "w_pw1": f(conv_w_pw1[0]), "w_pw2": f(conv_w_pw2[0]), "pool_w": f(pool_w_group[0]),
        "ffn_wg": f(ffn_w_gate[0]), "ffn_wu": f(ffn_w_up[0]), "ffn_wd": f(ffn_w_down[0]),
        "moe_wg": f(moe_w_gate[0]), "moe_wu": f(moe_w_up[0]), "moe_wd": f(moe_w_down[0]),
    }
    in_maps = []
    for c in range(NCORES):
        m = dict(shared)
        m["xT"] = f(xe[TOK * c: TOK * c + TW].T)
        um = np.ones((128, HALO), np.float32)
        if c == 0:
            um[:, :32] = 0.0
        m["umask"] = um
        in_maps.append(m)
    return in_maps


def kernel(**inputs):
    if "nc" not in _CACHE:
        _CACHE["nc"] = build_program()
    nc = _CACHE["nc"]
    in_maps = _prep_inputs(**inputs)
    res = run_bass_kernel_spmd(nc, in_maps, core_ids=list(range(NCORES)))
    outs = [np.asarray(r["out"]) for r in res.results]
    out = np.concatenate(outs, axis=0).reshape(1, SEQ, D).astype(np.float32)
    return out
```

```python
import numpy as np
import concourse.bass as bass
import concourse.mybir as mybir
from concourse.bass_utils import run_bass_kernel_spmd
from contextlib import ExitStack

F32 = mybir.dt.float32
BF16 = mybir.dt.bfloat16
ALU = mybir.AluOpType
AF = mybir.ActivationFunctionType

NCORES = 8
D = 1024
KC = 8
SEQ = 16384
NMETA = 16
TOK = SEQ // NCORES
HALO = 48
TW = TOK + HALO
DFF = 2816
DFE = 3584
NE = 8
CONVW = 31
RMS_EPS = 1e-6
LN_EPS = 1e-5
POOLW = (2, 4, 8, 16)
CAP = 768
NST = CAP // 128
NSLOT = NE * CAP
U32 = mybir.dt.uint32

TT0 = [(0, 432), (432, 416), (848, 416), (1264, 416), (1680, 416)]
TT1 = [(32, 400)] + TT0[1:]
TT2 = [(48, 384)] + TT0[1:]
TT3 = [(48 + 512 * i, 512) for i in range(4)]

CV = {}
_o = 0
for _n, _w in (("mix_g0", 8), ("mix_g1", 8), ("ffn_g0", 8), ("ffn_g1", 8), ("fin_g", 8),
               ("b_pw1", 16), ("b_dw", 8), ("ln_g", 8), ("ln_b", 8), ("b_pw2", 8),
               ("pool_s", 8), ("w_dw", 8 * CONVW)):
    CV[_n] = _o
    _o += _w
NCV = _o


class Buf:
    __slots__ = ("name", "w", "r")

    def __init__(self, name):
        self.name = name
        self.w = {}
        self.r = {}


class Sched:
    ENGS = ("pe", "act", "dve", "pool", "sp")

    def __init__(self, nc, es):
        self.nc = nc
        self.es = es
        self.eng = {"pe": nc.tensor, "act": nc.scalar, "dve": nc.vector,
                    "pool": nc.gpsimd, "sp": nc.sync}
        self.stream = {e: [] for e in self.ENGS}
        self.sems = {}
        self.cnt = {}
        self.seen = {e: {} for e in self.ENGS}
        for e in self.ENGS:
            self.new_sem(e)

    def new_sem(self, name):
        self.sems[name] = self.es.enter_context(self.nc.semaphore("s_" + name))
        self.cnt[name] = 0
        return name

    def op(self, e, fn, reads=(), writes=(), dma_sem=None, inc=True, after=(), waw=True):
        d = {}

        def add(tok):
            if tok is None:
                return
            s, v = tok
            if d.get(s, 0) < v:
                d[s] = v
        for b in reads:
            for s, v in b.w.items():
                add((s, v))
        for b in writes:
            if waw:
                for s, v in b.w.items():
                    if not (dma_sem is not None and s == dma_sem):
                        add((s, v))
            for s, v in b.r.items():
                add((s, v))
        for tok in after:
            add(tok)
        waits = []
        seen = self.seen[e]
        for s, v in d.items():
            if e == "pe" and s == "pe":
                continue
            if seen.get(s, 0) < v:
                waits.append((s, v))
                seen[s] = v
        if not inc:
            self.stream[e].append((waits, fn, None, 0))
            return None
        if dma_sem is None:
            s, n = e, 1
        else:
            s, n = dma_sem, 16
        self.cnt[s] += n
        tok = (s, self.cnt[s])
        self.stream[e].append((waits, fn, s, n))
        for b in writes:
            if waw:
                b.w = {s: tok[1]}
                b.r = {}
            else:
                b.w[s] = tok[1]
        for b in reads:
            if b.r.get(s, 0) < tok[1]:
                b.r[s] = tok[1]
        return tok

    def barrier(self):
        snap = dict(self.cnt)
        for e in self.ENGS:
            waits = []
            for s, v in snap.items():
                if v == 0 or (e == "pe" and s == "pe"):
                    continue
                if self.seen[e].get(s, 0) < v:
                    waits.append((s, v))
                    self.seen[e][s] = v
            if waits:
                self.stream[e].append((waits, None, None, 0))

    def replay(self, e):
        eng = self.eng[e]
        for waits, fn, s, n in self.stream[e]:
            for ws, wv in waits:
                eng.wait_ge(self.sems[ws], wv)
            if fn is None:
                continue
            ins = fn()
            if s is not None:
                ins.then_inc(self.sems[s], n)

    def run_block(self):
        nc = self.nc
        with nc.Block() as block:
            @block.tensor
            def _(t):
                self.replay("pe")

            @block.scalar
            def _(t):
                self.replay("act")

            @block.vector
            def _(t):
                self.replay("dve")

            @block.gpsimd
            def _(t):
                self.replay("pool")

            @block.sync
            def _(t):
                self.replay("sp")


def build_program(debug=False):
    nc = bass.Bass("TRN2", target_bir_lowering=False)
    dbg_t = [nc.dram_tensor(f"dbg{i}", [D, TW], F32, kind="ExternalOutput").ap() for i in range(4)] if debug else []

    def din(name, shape):
        return nc.dram_tensor(name, list(shape), F32, kind="ExternalInput").ap()

    xT = din("xT", (D, TW))
    umask_d = din("umask", (128, HALO))
    cvec_d = din("cvec", (128, NCV))
    wr_d = din("wr", (128, KC * NE))
    ident_d = din("ident", (128, 128))
    w_pw1 = din("w_pw1", (D, 2 * D))
    w_pw2 = din("w_pw2", (D, D))
    pool_w = din("pool_w", (4, 256, 256))
    ffn_wg = din("ffn_wg", (D, DFF))
    ffn_wu = din("ffn_wu", (D, DFF))
    ffn_wd = din("ffn_wd", (DFF, D))
    moe_wg = din("moe_wg", (NE, D, DFE))
    moe_wu = din("moe_wu", (NE, D, DFE))
    moe_wd = din("moe_wd", (NE, DFE, D))
    tri_d = din("tri", (128, 128))
    gbc_d = din("gbc", (128, D))
    out_d = nc.dram_tensor("out", [TOK, D], F32, kind="ExternalOutput").ap()
    xs_d = nc.dram_tensor("xs_scratch", [NSLOT, D], BF16).ap()
    ys_d = nc.dram_tensor("ys_scratch", [NSLOT, D], F32).ap()

    with ExitStack() as es:
        S = Sched(nc, es)

        def sb(name, shape, dt):
            return es.enter_context(nc.sbuf_tensor("sb_" + name, list(shape), dt))

        H = sb("H", (128, KC, TW), F32)
        B1 = sb("B1", (128, KC, TW), BF16)
        B2 = sb("B2", (128, KC * TW), BF16)
        WP1 = [sb(f"wp1_{b}", (128, KC, 512), BF16) for b in range(2)]
        WP2 = [sb(f"wp2_{b}", (128, KC, 512), BF16) for b in range(2)]
        WP3 = [sb(f"wp3_{b}", (128, 4, 1024), BF16) for b in range(2)]
        R1 = sb("R1", (128, 4096), BF16)
        Z = sb("Z", (128, KC, 416), BF16)
        TMP = [sb(f"tmp{i}", (128, 512), F32) for i in range(5)]
        cvec = sb("cvec", (128, NCV), F32)
        wr = sb("wr", (128, KC, NE), F32)
        wrg = sb("wrg", (128, KC, NE), F32)
        identf = sb("identf", (128, 128), F32)
        identb = sb("identb", (128, 128), BF16)
        onesb = sb("onesb", (128, 128), BF16)
        onesf = sb("onesf", (128, 128), F32)
        umask = sb("umask", (128, HALO), F32)
        ps = [es.enter_context(nc.psum_tensor(f"ps{i}", [128, 512], F32)) for i in range(8)]

        Y = B1[:].bitcast(F32)
        U = B2[:].rearrange("p (k n) -> p k n", k=KC)
        B2f = B2[:].bitcast(F32)
        SQ = R1[:, 0:KC * 432].rearrange("p (k n) -> p k n", k=KC)
        Abuf = [R1[:, i * 2048:(i + 1) * 2048].rearrange("p (m n) -> p m n", m=4) for i in range(2)]
        CWe = [B2f[:, i * 2048:(i + 1) * 2048] for i in range(2)]
        RSall = B2f[:, 6144:8192]
        Zf = Z[:].rearrange("p k n -> p (k n)").bitcast(F32)
        RT = Zf[:, 0:640].rearrange("p (j c) -> p j c", j=16)
        Mf = Zf[:, 768:896].rearrange("p (j e) -> p j e", j=16)
        Mb = Zf[:, 896:960].bitcast(BF16)
        WITHIN = Zf[:, 960:1088]
        OFF = Zf[:, 1088:1216].rearrange("p (j e) -> p j e", j=16)
        SLV = Zf[:, 1216:1344].rearrange("p (j e) -> p j e", j=16)
        S0F = Zf[:, 1344:1360]
        S1F = Zf[:, 1360:1376]
        S0U = Zf[:, 1376:1392].bitcast(U32)
        S1U = Zf[:, 1392:1408].bitcast(U32)
        EC = Zf[:, 1440:1568].rearrange("p (j e) -> p j e", j=16)
        SCR = Zf[:, 1568:1664]
        trib = sb("trib", (128, 128), BF16)
        OUTb = [B2f[:, i * 3456:(i + 1) * 3456].rearrange("p (k n) -> p k n", k=KC) for i in range(2)]
        assert tuple(Y.shape) == (128, KC, TW // 2), Y.shape

        P = [Buf(f"ps{i}") for i in range(8)]
        state = {"bank": 0, "dq": 0, "r1": "sq"}

        def cv(name, j=0):
            c = CV[name] + j
            return cvec[:, c:c + 1]

        def mm_group(pairs, n, reads, rows=128):
            i = state["bank"]
            state["bank"] = (i + 1) % 8
            L = len(pairs)
            for j, (l, r) in enumerate(pairs):
                edge = (j == 0 or j == L - 1)
                S.op("pe",
                     (lambda l=l, r=r, j=j, i=i: nc.tensor.matmul(
                         ps[i][:rows, :n], lhsT=l, rhs=r, start=(j == 0), stop=(j == L - 1))),
                     reads=reads if edge else (), writes=[P[i]] if edge else (),
                     inc=(j == L - 1))
            return i

        Bc = Buf("consts")
        sc = S.new_sem("dconst")
        trif = TMP[3][:, 0:128]
        for dst, src in ((cvec[:], cvec_d), (wr[:].rearrange("p k e -> p (k e)"), wr_d),
                         (identf[:], ident_d), (umask[:], umask_d), (trif, tri_d)):
            S.op("sp", (lambda dst=dst, src=src: nc.sync.dma_start(out=dst, in_=src)),
                 writes=[Bc], dma_sem=sc)
        S.op("dve", lambda: nc.vector.memset(onesb[:], 1.0), writes=[Bc])
        S.op("dve", lambda: nc.vector.memset(onesf[:], 1.0), writes=[Bc])
        S.op("dve", lambda: nc.vector.tensor_copy(out=identb[:], in_=identf[:]), reads=[Bc], writes=[Bc])
        for k in range(KC):
            S.op("dve", (lambda k=k: nc.vector.tensor_scalar(
                out=wrg[:, k, :], in0=wr[:, k, :], scalar1=cv("ffn_g1", k), scalar2=None, op0=ALU.mult)),
                reads=[Bc], writes=[Bc])

        Hb = [Buf(f"H{t}") for t in range(5)]
        sx = S.new_sem("dx")
        dbg_sem = S.new_sem("ddbg") if debug else None

        def dump(i):
            if not debug:
                return
            S.barrier()
            for k in range(KC):
                S.op("sp", (lambda k=k, i=i: nc.sync.dma_start(out=dbg_t[i][k * 128:(k + 1) * 128, :], in_=H[:, k, :])),
                     reads=Hb, dma_sem=dbg_sem)
            S.barrier()
        xTv = xT.rearrange("(k p) n -> p k n", p=128)
        sxs = [sx] + [S.new_sem(f"dx{t}") for t in range(1, 5)]
        for t, (o, n) in enumerate(TT0):
            S.op("sp", (lambda o=o, n=n: nc.sync.dma_start(out=H[:, :, o:o + n], in_=xTv[:, :, o:o + n])),
                 writes=[Hb[t]], dma_sem=sxs[t])

        bnd = {}

        def _mk_bound():
            reg = nc.gpsimd.alloc_register("slot_bound")
            ins = nc.gpsimd.reg_mov(reg, NSLOT - 1)
            bnd["v"] = nc.gpsimd.snap(reg)
            return ins
        S.op("pool", _mk_bound, inc=False)

        XS0b = Buf("xs0")
        ZTb = Buf("zt")
        ZT = TMP[4][:, :].bitcast(BF16)
        szf = S.new_sem("dzf")
        S.op("dve", lambda: nc.vector.memset(TMP[4][:, :], 0.0), writes=[ZTb])
        zsem = [szf, S.new_sem("dzf1"), S.new_sem("dzf2")]
        nb = NSLOT // 128 // 8
        for bq in range(nb):
            zs = zsem[bq % 3]
            thr = []
            if bq >= 2:
                ps_ = zsem[(bq - 2) % 3]
                thr = [(ps_, S.cnt[ps_])]
            for q in range(8 * bq, 8 * bq + 8):
                S.op("sp", (lambda q=q: nc.sync.dma_start(out=xs_d[q * 128:(q + 1) * 128, :], in_=ZT)),
                     reads=[ZTb], writes=[XS0b], dma_sem=zs, after=thr, waw=False)
        S.op("dve", lambda: nc.vector.tensor_copy(out=trib[:], in_=trif[:]), reads=[Bc], writes=[Bc])

        WBb = [Buf("wb0"), Buf("wb1")]
        wsem = [S.new_sem("dw0"), S.new_sem("dw1")]

        def wload(b, parts):
            for dst, src in parts:
                S.op("pool", (lambda dst=dst, src=src: nc.gpsimd.dma_start(out=dst, in_=src)),
                     writes=[WBb[b]], dma_sem=wsem[b])

        def kview(ap):
            return ap.rearrange("(k p) n -> p k n", p=128)

        SQb, RSb = Buf("sq"), Buf("rs")

        def rmsnorm(tiles, tbufs_in, gname, out_fn, out_bufs, eps=RMS_EPS, rs_fn=None, rs_buf=None):
            if state["r1"] != "sq":
                S.barrier()
                state["r1"] = "sq"
            for ti, (o, n) in enumerate(tiles):
                hb = tbufs_in[ti]
                rsap = (lambda o=o, n=n: TMP[0][:, :n]) if rs_fn is None else (lambda o=o, n=n: rs_fn(o, n))
                rsb = RSb if rs_buf is None else rs_buf
                S.op("act", (lambda o=o, n=n: nc.scalar.activation(
                    out=SQ[:, :, :n], in_=H[:, :, o:o + n], func=AF.Square)),
                    reads=[hb], writes=[SQb])
                i = mm_group([(onesb[:], SQ[:, k, :n]) for k in range(KC)], n, [Bc, SQb])
                S.op("act", (lambda i=i, n=n, rsap=rsap: nc.scalar.activation(
                    out=rsap(), in_=ps[i][:, :n], func=AF.Sqrt, bias=eps_ap(eps), scale=1.0 / D)),
                    reads=[P[i], Bc], writes=[rsb])
                S.op("dve", (lambda rsap=rsap: nc.vector.reciprocal(out=rsap(), in_=rsap())),
                     reads=[rsb], writes=[rsb])
                for k in range(KC):
                    S.op("dve", (lambda k=k, o=o, n=n, rsap=rsap: nc.vector.scalar_tensor_tensor(
                        out=out_fn(k, o, n), in0=H[:, k, o:o + n], scalar=cv(gname, k),
                        in1=rsap(), op0=ALU.mult, op1=ALU.mult)),
                        reads=[hb, rsb, Bc], writes=[out_bufs[ti]])

        epsc = sb("epsc", (128, 2), F32)
        S.op("dve", lambda: nc.vector.memset(epsc[:, 0:1], RMS_EPS), writes=[Bc])
        S.op("dve", lambda: nc.vector.memset(epsc[:, 1:2], LN_EPS), writes=[Bc])

        def eps_ap(eps):
            return epsc[:, 0:1] if eps == RMS_EPS else epsc[:, 1:2]

        HNb = [Buf(f"HN{t}") for t in range(5)]
        for s in range(2):
            wload(s, [(WP1[s][:], kview(w_pw1[:, 512 * s:512 * s + 512])),
                      (WP2[s][:], kview(w_pw1[:, D + 512 * s:D + 512 * s + 512]))])
        rmsnorm(TT0, Hb, "mix_g0", lambda k, o, n: B1[:, k, o:o + n], HNb)

        Ub = Buf("U")
        SIGb = [Buf("sig0"), Buf("sig1")]
        sgi = 0
        for s in range(2):
            for ti, (o, n) in enumerate(TT0):
                for mc in range(4):
                    ch = 4 * s + mc
                    ia = mm_group([(WP1[s][:, k, mc * 128:(mc + 1) * 128], B1[:, k, o:o + n]) for k in range(KC)],
                                  n, [WBb[s], HNb[ti]])
                    ig = mm_group([(WP2[s][:, k, mc * 128:(mc + 1) * 128], B1[:, k, o:o + n]) for k in range(KC)],
                                  n, [WBb[s], HNb[ti]])
                    sg = sgi % 2
                    sgi += 1
                    S.op("act", (lambda ig=ig, n=n, ch=ch, sg=sg: nc.scalar.activation(
                        out=TMP[1 + sg][:, :n], in_=ps[ig][:, :n], func=AF.Sigmoid, bias=cv("b_pw1", 8 + ch))),
                        reads=[P[ig], Bc], writes=[SIGb[sg]])
                    S.op("dve", (lambda ia=ia, n=n, ch=ch, sg=sg, o=o: nc.vector.scalar_tensor_tensor(
                        out=U[:, ch, o:o + n], in0=ps[ia][:, :n], scalar=cv("b_pw1", ch),
                        in1=TMP[1 + sg][:, :n], op0=ALU.add, op1=ALU.mult)),
                        reads=[P[ia], SIGb[sg], Bc], writes=[Ub])
        for k in range(KC):
            S.op("dve", (lambda k=k: nc.vector.tensor_tensor(
                out=U[:, k, 0:HALO], in0=U[:, k, 0:HALO], in1=umask[:], op=ALU.mult)),
                reads=[Ub, Bc], writes=[Ub])

        S.barrier()

        W2b = [Buf("w2_0"), Buf("w2_1")]
        w2sem = [S.new_sem("dw2_0"), S.new_sem("dw2_1")]
        for s in range(2):
            S.op("pool", (lambda s=s: nc.gpsimd.dma_start(
                out=WP1[s][:], in_=kview(w_pw2[:, 512 * s:512 * s + 512]))),
                writes=[W2b[s]], dma_sem=w2sem[s])

        DGb = [Buf("dg0"), Buf("dg1")]
        Yb = Buf("Y")
        Zb = Buf("Z")
        MUb, M2b = Buf("mu"), ZTb
        groups = [[0, 1], [2, 3], [4]]
        dgi = 0
        for grp in groups:
            gbase = TT1[grp[0]][0]
            for c in range(KC):
                db = dgi % 2
                dgi += 1
                dg = WP3[db][:].rearrange("p a b -> p (a b)")
                for k in range(CONVW):
                    wcol = cvec[:, CV["w_dw"] + c * CONVW + k: CV["w_dw"] + c * CONVW + k + 1]
                    if k % 3 == 0:
                        S.op("pool", (lambda k=k, dg=dg, wcol=wcol: nc.gpsimd.tensor_scalar(
                            out=dg[:, k * 128:(k + 1) * 128], in0=identf[:], scalar1=wcol,
                            scalar2=0.0, op0=ALU.mult, op1=ALU.add)),
                            reads=[Bc], writes=[DGb[db]], waw=False)
                    else:
                        S.op("dve", (lambda k=k, dg=dg, wcol=wcol: nc.vector.tensor_scalar(
                            out=dg[:, k * 128:(k + 1) * 128], in0=identf[:], scalar1=wcol,
                            scalar2=None, op0=ALU.mult)),
                            reads=[Bc], writes=[DGb[db]], waw=False)
                for ti in grp:
                    o, n = TT1[ti]
                    i = mm_group([(dg[:, k * 128:(k + 1) * 128], U[:, c, o - 30 + k: o - 30 + k + n])
                                  for k in range(CONVW)], n, [DGb[db], Ub])
                    S.op("act", (lambda i=i, n=n, c=c, yo=o - gbase: nc.scalar.activation(
                        out=Y[:, c, yo:yo + n], in_=ps[i][:, :n], func=AF.Identity, bias=cv("b_dw", c))),
                        reads=[P[i], Bc], writes=[Yb])
            for ti in grp:
                o, n = TT1[ti]
                yo = o - gbase
                imu = mm_group([(onesf[:], Y[:, k, yo:yo + n]) for k in range(KC)], n, [Bc, Yb])
                S.op("act", (lambda yo=yo, n=n: nc.scalar.activation(
                    out=SQ[:, :, :n], in_=Y[:, :, yo:yo + n], func=AF.Square)),
                    reads=[Yb], writes=[SQb])
                isq = mm_group([(onesb[:], SQ[:, k, :n]) for k in range(KC)], n, [Bc, SQb])
                S.op("dve", (lambda imu=imu, n=n: nc.vector.tensor_scalar(
                    out=TMP[3][:, :n], in0=ps[imu][:, :n], scalar1=1.0 / D, scalar2=None, op0=ALU.mult)),
                    reads=[P[imu]], writes=[MUb])
                S.op("dve", (lambda n=n: nc.vector.tensor_tensor(
                    out=TMP[4][:, :n], in0=TMP[3][:, :n], in1=TMP[3][:, :n], op=ALU.mult)),
                    reads=[MUb], writes=[M2b])
                S.op("dve", (lambda isq=isq, n=n: nc.vector.scalar_tensor_tensor(
                    out=TMP[4][:, :n], in0=ps[isq][:, :n], scalar=1.0 / D, in1=TMP[4][:, :n],
                    op0=ALU.mult, op1=ALU.subtract)),
                    reads=[P[isq], M2b], writes=[M2b])
                S.op("act", (lambda n=n: nc.scalar.activation(
                    out=TMP[0][:, :n], in_=TMP[4][:, :n], func=AF.Sqrt, bias=eps_ap(LN_EPS), scale=1.0)),
                    reads=[M2b, Bc], writes=[RSb])
                S.op("dve", (lambda n=n: nc.vector.reciprocal(out=TMP[0][:, :n], in_=TMP[0][:, :n])),
                     reads=[RSb], writes=[RSb])
                for c in range(KC):
                    S.op("dve", (lambda c=c, yo=yo, n=n: nc.vector.tensor_tensor(
                        out=Y[:, c, yo:yo + n], in0=Y[:, c, yo:yo + n], in1=TMP[3][:, :n], op=ALU.subtract)),
                        reads=[Yb, MUb], writes=[Yb])
                    S.op("dve", (lambda c=c, yo=yo, n=n: nc.vector.tensor_tensor(
                        out=Y[:, c, yo:yo + n], in0=Y[:, c, yo:yo + n], in1=TMP[0][:, :n], op=ALU.mult)),
                        reads=[Yb, RSb], writes=[Yb])
                    S.op("act", (lambda c=c, yo=yo, n=n: nc.scalar.activation(
                        out=Z[:, c, :n], in_=Y[:, c, yo:yo + n], func=AF.Silu,
                        bias=cv("ln_b", c), scale=cv("ln_g", c))),
                        reads=[Yb, Bc], writes=[Zb])
                for oc in range(KC):
                    s2, mc = divmod(oc, 4)
                    i = mm_group([(WP1[s2][:, k, mc * 128:(mc + 1) * 128], Z[:, k, :n]) for k in range(KC)],
                                 n, [W2b[s2], Zb])
                    S.op("dve", (lambda i=i, oc=oc, o=o, n=n: nc.vector.scalar_tensor_tensor(
                        out=H[:, oc, o:o + n], in0=ps[i][:, :n], scalar=cv("b_pw2", oc),
                        in1=H[:, oc, o:o + n], op0=ALU.add, op1=ALU.add)),
                        reads=[P[i], Bc, Hb[ti]], writes=[Hb[ti]])

        S.barrier()
        dump(0)

        SGb = [Buf("sg0"), Buf("sg1")]
        T2b = [Buf("t2a"), Buf("t2b")]
        Ab = [Buf("A0"), Buf("A1")]
        ffn_state = {"sg": 0, "a": 0, "slab": 0}

        def ffn(slabs, tiles, hnb, hb, cw=None):
            st = ffn_state
            if state["r1"] != "a":
                S.barrier()
                state["r1"] = "a"
            b0 = st["slab"] % 2
            wload(b0, slabs[0][1])
            for si, (wdt, _) in enumerate(slabs):
                b = (st["slab"] + si) % 2
                if si + 1 < len(slabs):
                    wload(1 - b, slabs[si + 1][1])
                nm = wdt // 128
                for ti, (o, n) in enumerate(tiles):
                    a = st["a"] % 2
                    st["a"] += 1
                    for mc in range(nm):
                        ig = mm_group([(WP1[b][:, k, mc * 128:(mc + 1) * 128], B1[:, k, o:o + n]) for k in range(KC)],
                                      n, [WBb[b], hnb[ti]])
                        iu = mm_group([(WP2[b][:, k, mc * 128:(mc + 1) * 128], B1[:, k, o:o + n]) for k in range(KC)],
                                      n, [WBb[b], hnb[ti]])
                        sg = st["sg"] % 2
                        st["sg"] += 1
                        S.op("act", (lambda ig=ig, n=n, sg=sg: nc.scalar.activation(
                            out=TMP[1 + sg][:, :n], in_=ps[ig][:, :n], func=AF.Silu)),
                            reads=[P[ig]], writes=[SGb[sg]])
                        if cw is None:
                            S.op("dve", (lambda iu=iu, n=n, sg=sg, a=a, mc=mc: nc.vector.tensor_tensor(
                                out=Abuf[a][:, mc, :n], in0=ps[iu][:, :n], in1=TMP[1 + sg][:, :n], op=ALU.mult)),
                                reads=[P[iu], SGb[sg]], writes=[Ab[a]])
                        else:
                            cwfn, cwb = cw
                            S.op("dve", (lambda n=n, sg=sg, o=o: nc.vector.tensor_tensor(
                                out=TMP[3 + sg][:, :n], in0=TMP[1 + sg][:, :n], in1=cwfn(o, n), op=ALU.mult)),
                                reads=[SGb[sg], cwb], writes=[T2b[sg]])
                            S.op("dve", (lambda iu=iu, n=n, sg=sg, a=a, mc=mc: nc.vector.tensor_tensor(
                                out=Abuf[a][:, mc, :n], in0=ps[iu][:, :n], in1=TMP[3 + sg][:, :n], op=ALU.mult)),
                                reads=[P[iu], T2b[sg]], writes=[Ab[a]])
                    for oc in range(KC):
                        i = mm_group([(WP3[b][:, mc, oc * 128:(oc + 1) * 128], Abuf[a][:, mc, :n]) for mc in range(nm)],
                                     n, [WBb[b], Ab[a]])
                        S.op("dve", (lambda i=i, oc=oc, o=o, n=n: nc.vector.tensor_tensor(
                            out=H[:, oc, o:o + n], in0=ps[i][:, :n], in1=H[:, oc, o:o + n], op=ALU.add)),
                            reads=[P[i], hb[ti]], writes=[hb[ti]])
            st["slab"] += len(slabs)

        def ffn_slabs(wg, wu, wd, dff):
            out = []
            off = 0
            while off < dff:
                wdt = min(512, dff - off)
                nm = wdt // 128
                out.append((wdt, off))
                off += wdt
            res = []
            for wdt, off in out:
                nm = wdt // 128

                def mk(b, wdt=wdt, off=off, nm=nm):
                    return [(WP1[b][:, :, :wdt], kview(wg[:, off:off + wdt])),
                            (WP2[b][:, :, :wdt], kview(wu[:, off:off + wdt])),
                            (WP3[b][:, :nm, :], wd[off:off + wdt, :].rearrange("(m p) n -> p m n", p=128))]
                res.append((wdt, mk))
            return res

        def run_ffn(wg, wu, wd, dff, tiles, hnb, hb, cw=None):
            sl = ffn_slabs(wg, wu, wd, dff)
            base = ffn_state["slab"]
            slabs = [(wdt, mk((base + si) % 2)) for si, (wdt, mk) in enumerate(sl)]
            ffn(slabs, tiles, hnb, hb, cw)

        rmsnorm(TT1, Hb, "ffn_g0", lambda k, o, n: B1[:, k, o:o + n], HNb)
        run_ffn(ffn_wg, ffn_wu, ffn_wd, DFF, TT1, HNb, Hb)

        dump(1)
        spw = S.new_sem("dpw")
        PW = WP3[0][:].rearrange("p a b -> p (a b)")[:, 0:2048].rearrange("p (g k n) -> p g k n", g=4, k=2)
        for g in range(4):
            S.op("pool", (lambda g=g: nc.gpsimd.dma_start(
                out=PW[:, g, :, :], in_=pool_w[g].rearrange("(k p) n -> p k n", p=128))),
                writes=[WBb[0]], dma_sem=spw)
        rmsnorm(TT1, Hb, "mix_g1", lambda k, o, n: B1[:, k, o:o + n], HNb)
        W1f = WP1[0][:].rearrange("p a b -> p (a b)")
        PWS = W1f[:, 0:2048].rearrange("p (g k n) -> p g k n", g=4, k=2)
        PWN = W1f[:, 2048:4096].rearrange("p (g k n) -> p g k n", g=4, k=2)
        for g in range(4):
            S.op("dve", (lambda g=g: nc.vector.tensor_scalar(
                out=PWS[:, g, :, :], in0=PW[:, g, :, :], scalar1=1.0 / POOLW[g], scalar2=None, op0=ALU.mult)),
                reads=[WBb[0]], writes=[WBb[0]])
        for g in range(4):
            S.op("dve", (lambda g=g: nc.vector.tensor_scalar(
                out=PWN[:, g, :, :], in0=PW[:, g, :, :], scalar1=1.0 / POOLW[g] - 1.0, scalar2=None, op0=ALU.mult)),
                reads=[WBb[0]], writes=[WBb[0]])
        for ti, (o, n) in enumerate(TT2):
            rd = [HNb[ti]] + ([HNb[ti - 1]] if ti > 0 else [])
            for g in range(4):
                w = POOLW[g]
                for oc in range(2):
                    ch = 2 * g + oc
                    pairs = []
                    for kc in range(2):
                        pairs.append((PWN[:, g, kc, oc * 128:(oc + 1) * 128], B1[:, 2 * g + kc, o:o + n]))
                        for dd in range(1, w):
                            pairs.append((PWS[:, g, kc, oc * 128:(oc + 1) * 128], B1[:, 2 * g + kc, o - dd:o - dd + n]))
                    i = mm_group(pairs, n, [WBb[0]] + rd)
                    S.op("dve", (lambda i=i, ch=ch, o=o, n=n: nc.vector.scalar_tensor_tensor(
                        out=H[:, ch, o:o + n], in0=ps[i][:, :n], scalar=cv("pool_s", ch), in1=H[:, ch, o:o + n],
                        op0=ALU.mult, op1=ALU.add)),
                        reads=[P[i], Hb[ti], Bc], writes=[Hb[ti]])

        dump(2)
        RSAb = Buf("rsall")
        rmsnorm(TT2, Hb, "ffn_g1", lambda k, o, n: B1[:, k, o:o + n], HNb,
                rs_fn=lambda o, n: RSall[:, o - HALO:o - HALO + n], rs_buf=RSAb)
        RTb = [Buf("rt")] * 16
        rb = RTb[0]
        ir = state["bank"]
        state["bank"] = (ir + 1) % 8
        for j in range(16):
            c0 = HALO + 128 * j
            for k in range(KC):
                first = (j == 0 and k == 0)
                S.op("pe", (lambda j=j, k=k, c0=c0: nc.tensor.matmul(
                    ps[ir][:, 8 * j:8 * j + 8], lhsT=H[:, k, c0:c0 + 128], rhs=wrg[:, k, :],
                    start=(k == 0), stop=(k == KC - 1))),
                    reads=Hb + [Bc] if first else (), writes=[P[ir]] if first else (), inc=False)
            last = (j == 15)
            S.op("pe", (lambda j=j: nc.tensor.matmul(
                ps[ir][:, 128 + 2 * j:128 + 2 * j + 2], lhsT=RSall[:, 128 * j:128 * j + 128], rhs=identf[:, 0:2],
                start=True, stop=True)),
                reads=[RSAb, Bc] + Hb if (j == 0 or last) else (), writes=[P[ir]] if last else (), inc=last)
        RAW = RT[:, :, 0:8]
        S.op("dve", lambda: nc.vector.tensor_copy(
            out=RAW, in_=ps[ir][:, 0:128].rearrange("p (j e) -> p j e", j=16)), reads=[P[ir]], writes=[rb])
        S.op("dve", lambda: nc.vector.tensor_copy(
            out=RT[:, :, 8], in_=ps[ir][:, 128:160].rearrange("p (j c) -> p j c", j=16)[:, :, 0]),
            reads=[P[ir]], writes=[rb])
        S.op("dve", lambda: nc.vector.tensor_reduce(out=RT[:, :, 9], in_=RAW, axis=mybir.AxisListType.X, op=ALU.max),
             reads=[rb], writes=[rb])
        def first_max_mask(src0, mcol, dst0):
            S.op("dve", lambda: nc.vector.memset(RT[:, :, 14], 0.0), reads=[rb], writes=[rb])
            for e in range(NE):
                S.op("dve", (lambda e=e: nc.vector.tensor_tensor(
                    out=RT[:, :, dst0 + e], in0=RT[:, :, src0 + e], in1=RT[:, :, mcol], op=ALU.is_equal)),
                    reads=[rb], writes=[rb])
                S.op("dve", (lambda e=e: nc.vector.scalar_tensor_tensor(
                    out=RT[:, :, dst0 + e], in0=RT[:, :, 14], scalar=1.0, in1=RT[:, :, dst0 + e],
                    op0=ALU.subtract, op1=ALU.mult)), reads=[rb], writes=[rb])
                S.op("dve", (lambda e=e: nc.vector.tensor_tensor(
                    out=RT[:, :, 14], in0=RT[:, :, 14], in1=RT[:, :, dst0 + e], op=ALU.subtract)),
                    reads=[rb], writes=[rb])
            S.op("dve", lambda: nc.vector.tensor_scalar(
                out=RT[:, :, dst0:dst0 + NE], in0=RT[:, :, dst0:dst0 + NE], scalar1=-1.0, scalar2=None, op0=ALU.mult),
                reads=[rb], writes=[rb])
        first_max_mask(0, 9, 16)
        S.op("dve", lambda: nc.vector.scalar_tensor_tensor(
            out=RT[:, :, 24:32], in0=RT[:, :, 16:24], scalar=-1.0e30, in1=RAW, op0=ALU.mult, op1=ALU.add),
            reads=[rb], writes=[rb])
        S.op("dve", lambda: nc.vector.tensor_reduce(out=RT[:, :, 10], in_=RT[:, :, 24:32], axis=mybir.AxisListType.X,
                                                    op=ALU.max), reads=[rb], writes=[rb])
        first_max_mask(24, 10, 32)
        S.op("dve", lambda: nc.vector.tensor_tensor(out=RT[:, :, 11], in0=RT[:, :, 9], in1=RT[:, :, 10], op=ALU.subtract),
             reads=[rb], writes=[rb])
        S.op("dve", lambda: nc.vector.tensor_tensor(out=RT[:, :, 11], in0=RT[:, :, 11], in1=RT[:, :, 8], op=ALU.mult),
             reads=[rb], writes=[rb])
        S.op("act", lambda: nc.scalar.activation(out=RT[:, :, 12], in_=RT[:, :, 11], func=AF.Sigmoid),
             reads=[rb], writes=[rb])
        S.op("dve", lambda: nc.vector.tensor_scalar(out=RT[:, :, 13], in0=RT[:, :, 12], scalar1=-1.0, scalar2=1.0,
                                                    op0=ALU.mult, op1=ALU.add), reads=[rb], writes=[rb])

        IDXb = Buf("idx")
        allrt = RTb
        S.op("dve", lambda: nc.vector.tensor_tensor(out=Mf[:], in0=RT[:, :, 16:24], in1=RT[:, :, 32:40], op=ALU.add),
             reads=allrt, writes=[IDXb])
        S.op("dve", lambda: nc.vector.tensor_copy(out=Mb, in_=Mf[:].rearrange("p j e -> p (j e)")),
             reads=[IDXb], writes=[IDXb])
        iw = mm_group([(trib[:], Mb)], 128, [Bc, IDXb])
        ic = mm_group([(onesb[:], Mb)], 128, [Bc, IDXb])
        S.op("dve", lambda: nc.vector.memset(OFF[:, 0, :], 0.0), reads=[IDXb], writes=[IDXb])
        for j in range(1, 16):
            S.op("dve", (lambda j=j: nc.vector.tensor_tensor(
                out=OFF[:, j, :], in0=OFF[:, j - 1, :], in1=ps[ic][:, (j - 1) * NE:j * NE], op=ALU.add)),
                reads=[IDXb, P[ic]], writes=[IDXb])
        for e in range(NE):
            S.op("dve", (lambda e=e: nc.vector.memset(EC[:, :, e:e + 1], float(e * CAP))), reads=[IDXb], writes=[IDXb])
        Offl = OFF.rearrange("p j e -> p (j e)")
        SLVl = SLV.rearrange("p j e -> p (j e)")
        ECl = EC.rearrange("p j e -> p (j e)")
        S.op("dve", lambda: nc.vector.tensor_tensor(out=Offl, in0=Offl, in1=ps[iw][:, 0:128], op=ALU.add),
             reads=[IDXb, P[iw]], writes=[IDXb])
        S.op("dve", lambda: nc.vector.tensor_tensor(out=SLVl, in0=Offl, in1=ECl, op=ALU.add), reads=[IDXb], writes=[IDXb])
        S.op("dve", lambda: nc.vector.tensor_scalar(out=Offl, in0=Offl, scalar1=float(CAP), scalar2=1.0e6,
                                                    op0=ALU.is_ge, op1=ALU.mult), reads=[IDXb], writes=[IDXb])
        S.op("dve", lambda: nc.vector.tensor_tensor(out=SLVl, in0=SLVl, in1=Offl, op=ALU.add), reads=[IDXb], writes=[IDXb])
        for (msk, SF, SU, wc) in ((RT[:, :, 16:24], S0F, S0U, 12), (RT[:, :, 32:40], S1F, S1U, 13)):
            S.op("dve", (lambda msk=msk: nc.vector.tensor_tensor(out=Mf[:], in0=msk, in1=SLV, op=ALU.mult)),
                 reads=[IDXb] + allrt, writes=[IDXb])
            S.op("dve", (lambda SF=SF: nc.vector.tensor_reduce(out=SF, in_=Mf[:], axis=mybir.AxisListType.X, op=ALU.add)),
                 reads=[IDXb], writes=[IDXb])
            S.op("dve", (lambda SF=SF, SU=SU: nc.vector.tensor_copy(out=SU, in_=SF)), reads=[IDXb], writes=[IDXb])
            S.op("dve", (lambda SF=SF: nc.vector.tensor_scalar(out=SF, in0=SF, scalar1=float(NSLOT), scalar2=None,
                                                               op0=ALU.is_lt)), reads=[IDXb], writes=[IDXb])
            S.op("dve", (lambda SF=SF, wc=wc: nc.vector.tensor_tensor(out=RT[:, :, wc], in0=RT[:, :, wc], in1=SF, op=ALU.mult)),
                 reads=[IDXb] + allrt, writes=allrt)

        all_slabs = [(e, sl) for e in range(NE) for sl in range(DFE // 512)]

        def slab_parts(e, sl, b):
            off = 512 * sl
            return [(WP1[b][:], kview(moe_wg[e][:, off:off + 512])),
                    (WP2[b][:], kview(moe_wu[e][:, off:off + 512])),
                    (WP3[b][:], moe_wd[e][off:off + 512, :].rearrange("(m p) n -> p m n", p=128))]
        sbase = ffn_state["slab"]
        wload(sbase % 2, slab_parts(0, 0, sbase % 2))

        HT = [B2[:, r * 1024:(r + 1) * 1024] for r in range(4)]
        HTb = [Buf(f"ht{r}") for r in range(4)]
        scs = [S.new_sem(f"dsc{r}") for r in range(4)]
        XSb = Buf("xs")
        XSb.w = dict(XS0b.w)
        prev_sc = []
        for j in range(16):
            c0 = HALO + 128 * j
            r = j % 4
            for half in range(2):
                i = state["bank"]
                state["bank"] = (i + 1) % 8
                for kk in range(4):
                    S.op("pe", (lambda i=i, kk=kk, half=half, c0=c0: nc.tensor.matmul(
                        ps[i][:, kk * 128:(kk + 1) * 128], lhsT=B1[:, 4 * half + kk, c0:c0 + 128], rhs=identb[:],
                        start=True, stop=True)),
                        reads=HNb + [Bc] if kk in (0, 3) else (), writes=[P[i]] if kk in (0, 3) else (), inc=(kk == 3))
                if half == 0:
                    S.op("act", (lambda i=i, r=r: nc.scalar.activation(out=HT[r][:, 0:512], in_=ps[i][:, :], func=AF.Identity)),
                         reads=[P[i]], writes=[HTb[r]], waw=False)
                else:
                    S.op("dve", (lambda i=i, r=r: nc.vector.tensor_copy(out=HT[r][:, 512:1024], in_=ps[i][:, :])),
                         reads=[P[i]], writes=[HTb[r]], waw=False)
            for SU in (S0U, S1U):
                S.op("pool", (lambda SU=SU, j=j, r=r: nc.gpsimd.indirect_dma_start(
                    out=xs_d, out_offset=bass.IndirectOffsetOnAxis(SU[:, j:j + 1], 0), in_=HT[r], in_offset=None,
                    bounds_check=bnd["v"], oob_is_err=False)),
                    reads=[HTb[r], IDXb], writes=[XSb], dma_sem=scs[r], waw=False, after=list(prev_sc))
                prev_sc[:] = [(scs[r], S.cnt[scs[r]])]

        if state["r1"] != "a":
            S.barrier()
            state["r1"] = "a"
        XT = [B1[:].rearrange("p k n -> p (k n)")[:, 12288 + r * 1024: 12288 + (r + 1) * 1024] for r in range(3)]
        XTb = [Buf(f"xt{r}") for r in range(3)]
        xts = [S.new_sem(f"dxt{r}") for r in range(3)]
        XG = [B1[:].rearrange("p k n -> p (k n)")[:, g * 6144:(g + 1) * 6144].rearrange("p (k n) -> p k n", k=KC)
              for g in range(2)]
        XGb = [Buf("xg0"), Buf("xg1")]
        YA = B2f[:, 0:NST * 1024].rearrange("p (s n) -> p s n", s=NST)
        YAb = [Buf(f"ya{st}") for st in range(NST)]
        YSDb = Buf("ysd")
        yst = [S.new_sem("dys0"), S.new_sem("dys1")]
        xti = 0
        evi = 0
        gtiles = [(0, CAP // 2), (CAP // 2, CAP // 2)]
        nst_t = (CAP // 2) // 128
        def emit_xload(e):
            nonlocal xti, evi
            g = e % 2
            for st in range(NST):
                xr = xti % 3
                xti += 1
                S.op("sp", (lambda e=e, st=st, xr=xr: nc.sync.dma_start(
                    out=XT[xr], in_=xs_d[e * CAP + st * 128: e * CAP + (st + 1) * 128, :])),
                    reads=[XSb], writes=[XTb[xr]], dma_sem=xts[xr])
                for half in range(2):
                    i = state["bank"]
                    state["bank"] = (i + 1) % 8
                    for kk in range(4):
                        S.op("pe", (lambda i=i, kk=kk, half=half, xr=xr: nc.tensor.matmul(
                            ps[i][:, kk * 128:(kk + 1) * 128],
                            lhsT=XT[xr][:, (4 * half + kk) * 128:(4 * half + kk + 1) * 128], rhs=identb[:],
                            start=True, stop=True)),
                            reads=[XTb[xr], Bc] if kk in (0, 3) else (), writes=[P[i]] if kk in (0, 3) else (),
                            inc=(kk == 3))
                    dst = XG[g][:, 4 * half:4 * half + 4, st * 128:(st + 1) * 128]
                    srcv = ps[i][:, :].rearrange("p (a b) -> p a b", a=4)
                    evi += 1
                    if evi % 2:
                        S.op("act", (lambda dst=dst, srcv=srcv: nc.scalar.activation(out=dst, in_=srcv, func=AF.Identity)),
                             reads=[P[i]], writes=[XGb[g]], waw=False)
                    else:
                        S.op("dve", (lambda dst=dst, srcv=srcv: nc.vector.tensor_copy(out=dst, in_=srcv)),
                             reads=[P[i]], writes=[XGb[g]], waw=False)
        emit_xload(0)
        for si, (e, sl) in enumerate(all_slabs):
            b = (sbase + si) % 2
            if si + 1 < len(all_slabs):
                wload(1 - b, slab_parts(all_slabs[si + 1][0], all_slabs[si + 1][1], 1 - b))
            g = e % 2
            if sl == 2 and e + 1 < NE:
                emit_xload(e + 1)
            for ti, (o, n) in enumerate(gtiles):
                a = ffn_state["a"] % 2
                ffn_state["a"] += 1
                for mc in range(4):
                    ig = mm_group([(WP1[b][:, k, mc * 128:(mc + 1) * 128], XG[g][:, k, o:o + n]) for k in range(KC)],
                                  n, [WBb[b], XGb[g]])
                    iu = mm_group([(WP2[b][:, k, mc * 128:(mc + 1) * 128], XG[g][:, k, o:o + n]) for k in range(KC)],
                                  n, [WBb[b], XGb[g]])
                    sg = ffn_state["sg"] % 2
                    ffn_state["sg"] += 1
                    S.op("act", (lambda ig=ig, n=n, sg=sg: nc.scalar.activation(
                        out=TMP[1 + sg][:, :n], in_=ps[ig][:, :n], func=AF.Silu)),
                        reads=[P[ig]], writes=[SGb[sg]])
                    S.op("dve", (lambda iu=iu, n=n, sg=sg, a=a, mc=mc: nc.vector.tensor_tensor(
                        out=Abuf[a][:, mc, :n], in0=ps[iu][:, :n], in1=TMP[1 + sg][:, :n], op=ALU.mult)),
                        reads=[P[iu], SGb[sg]], writes=[Ab[a]])
                for s3 in range(nst_t):
                    st = ti * nst_t + s3
                    for half in range(2):
                        i = mm_group([(Abuf[a][:, mc, s3 * 128:(s3 + 1) * 128], WP3[b][:, mc, half * 512:(half + 1) * 512])
                                      for mc in range(4)], 512, [WBb[b], Ab[a]])
                        if sl == 0:
                            S.op("dve", (lambda i=i, st=st, half=half: nc.vector.tensor_copy(
                                out=YA[:, st, half * 512:(half + 1) * 512], in_=ps[i][:, :])),
                                reads=[P[i]], writes=[YAb[st]], waw=(half == 0))
                        else:
                            S.op("dve", (lambda i=i, st=st, half=half: nc.vector.tensor_tensor(
                                out=YA[:, st, half * 512:(half + 1) * 512], in0=ps[i][:, :],
                                in1=YA[:, st, half * 512:(half + 1) * 512], op=ALU.add)),
                                reads=[P[i], YAb[st]], writes=[YAb[st]])
                if sl == DFE // 512 - 1:
                    for s3 in range(nst_t):
                        st = ti * nst_t + s3
                        S.op("sp", (lambda e=e, st=st: nc.sync.dma_start(
                            out=ys_d[e * CAP + st * 128: e * CAP + (st + 1) * 128, :], in_=YA[:, st, :])),
                            reads=[YAb[st]], writes=[YSDb], dma_sem=yst[ti], waw=False)
                    for s3 in range(nst_t):
                        YAb[ti * nst_t + s3].r[yst[ti]] = S.cnt[yst[ti]]
        ffn_state["slab"] += len(all_slabs)

        B1l = B1[:].rearrange("p k n -> p (k n)").bitcast(F32)
        G0 = [B1l[:, r * 1024:(r + 1) * 1024] for r in range(2)]
        G1 = [B1l[:, 2048 + r * 1024: 2048 + (r + 1) * 1024] for r in range(2)]
        RR = [B1l[:, 4096 + r * 1024: 4096 + (r + 1) * 1024] for r in range(2)]
        GBC = B2f[:, 0:1024]
        OUTt = [B2f[:, 1024 + r * 1024: 1024 + (r + 1) * 1024] for r in range(2)]
        OUTtb = [Buf("outt0"), Buf("outt1")]
        G0b = [Buf("g00"), Buf("g01")]
        G1b = [Buf("g10"), Buf("g11")]
        RRb = [Buf("rr0"), Buf("rr1")]
        GBCb = Buf("gbc")
        gsm = [scs[0], scs[1]]
        osem = [S.new_sem("do0"), S.new_sem("do1")]
        sgb = sc
        SSQ = SCR[:, 0:16]
        RS2 = SCR[:, 16:32]
        SSb = Buf("ssq")
        S.barrier()
        S.op("sp", lambda: nc.sync.dma_start(out=GBC, in_=gbc_d), writes=[GBCb], dma_sem=sgb)
        for r in range(2):
            S.op("pool", (lambda r=r: nc.gpsimd.memset(G0[r], 0.0)), writes=[G0b[r]])
            S.op("pool", (lambda r=r: nc.gpsimd.memset(G1[r], 0.0)), writes=[G1b[r]])
        last = []
        for j in range(16):
            c0 = HALO + 128 * j
            r = j % 2
            S.op("pool", (lambda j=j, r=r: nc.gpsimd.indirect_dma_start(
                out=G0[r], out_offset=None, in_=ys_d, in_offset=bass.IndirectOffsetOnAxis(S0U[:, j:j + 1], 0),
                bounds_check=bnd["v"], oob_is_err=False)),
                reads=[YSDb, IDXb], writes=[G0b[r]], dma_sem=gsm[r])
            S.op("pool", (lambda j=j, r=r: nc.gpsimd.indirect_dma_start(
                out=G1[r], out_offset=None, in_=ys_d, in_offset=bass.IndirectOffsetOnAxis(S1U[:, j:j + 1], 0),
                bounds_check=bnd["v"], oob_is_err=False)),
                reads=[YSDb, IDXb], writes=[G1b[r]], dma_sem=gsm[r])
            G0b[r].w = dict(G1b[r].w)
            for half in range(2):
                i = state["bank"]
                state["bank"] = (i + 1) % 8
                for kk in range(4):
                    S.op("pe", (lambda i=i, kk=kk, half=half, c0=c0: nc.tensor.matmul(
                        ps[i][:, kk * 128:(kk + 1) * 128], lhsT=H[:, 4 * half + kk, c0:c0 + 128], rhs=identf[:],
                        start=True, stop=True)),
                        reads=Hb + [Bc] if kk in (0, 3) else (), writes=[P[i]] if kk in (0, 3) else (), inc=(kk == 3))
                S.op("dve", (lambda i=i, j=j, r=r, half=half: nc.vector.scalar_tensor_tensor(
                    out=RR[r][:, half * 512:(half + 1) * 512], in0=G0[r][:, half * 512:(half + 1) * 512],
                    scalar=RT[:, j, 12:13], in1=ps[i][:, :], op0=ALU.mult, op1=ALU.add)),
                    reads=[P[i], G0b[r]] + allrt, writes=[RRb[r]], waw=(half == 0))
            S.op("dve", (lambda j=j, r=r: nc.vector.scalar_tensor_tensor(
                out=RR[r], in0=G1[r], scalar=RT[:, j, 13:14], in1=RR[r], op0=ALU.mult, op1=ALU.add)),
                reads=[G1b[r], RRb[r]] + allrt, writes=[RRb[r]])
            S.op("act", (lambda j=j, r=r: nc.scalar.activation(
                out=OUTt[r], in_=RR[r], func=AF.Square, accum_out=SSQ[:, j:j + 1])),
                reads=[RRb[r]], writes=[OUTtb[r], SSb])
            S.op("act", (lambda j=j: nc.scalar.activation(
                out=RS2[:, j:j + 1], in_=SSQ[:, j:j + 1], func=AF.Sqrt, bias=eps_ap(RMS_EPS), scale=1.0 / D)),
                reads=[SSb, Bc], writes=[SSb])
            S.op("dve", (lambda j=j: nc.vector.reciprocal(out=RS2[:, j:j + 1], in_=RS2[:, j:j + 1])),
                 reads=[SSb], writes=[SSb])
            S.op("dve", (lambda j=j, r=r: nc.vector.scalar_tensor_tensor(
                out=OUTt[r], in0=RR[r], scalar=RS2[:, j:j + 1], in1=GBC, op0=ALU.mult, op1=ALU.mult)),
                reads=[RRb[r], SSb, GBCb], writes=[OUTtb[r]])
            tok = S.op("sp", (lambda j=j, r=r: nc.sync.dma_start(out=out_d[128 * j:128 * (j + 1), :], in_=OUTt[r])),
                       reads=[OUTtb[r]], dma_sem=osem[r])
            last.append(tok)
        S.op("sp", None, after=last, inc=False)
        print("sbuf bytes remaining:", nc.sbuf_bytes_remaining)
        S.run_block()
    return nc


_CACHE = {}


def _prep_inputs(x, meta_tokens, conv_w_pw1, conv_b_pw1, conv_w_dw, conv_b_dw, conv_ln_g, conv_ln_b,
                 conv_w_pw2, conv_b_pw2, pool_w_group, pool_scale, ffn_w_gate, ffn_w_up, ffn_w_down,
                 moe_w_router, moe_w_gate, moe_w_up, moe_w_down, mix_norm_g, ffn_norm_g, final_norm_g):
    f = lambda a: np.ascontiguousarray(np.asarray(a, dtype=np.float32))

    def pk(v):
        v = np.asarray(v, dtype=np.float32).reshape(-1, 128)
        return v.T

    cols = [pk(mix_norm_g[0]), pk(mix_norm_g[1]), pk(ffn_norm_g[0]), pk(ffn_norm_g[1]), pk(final_norm_g),
            pk(conv_b_pw1[0]), pk(conv_b_dw[0]), pk(conv_ln_g[0]), pk(conv_ln_b[0]), pk(conv_b_pw2[0]),
            pk(pool_scale[0])]
    wdw = np.asarray(conv_w_dw[0], dtype=np.float32)
    wdw = wdw.reshape(CONVW, KC, 128).transpose(2, 1, 0).reshape(128, KC * CONVW)
    cvec = f(np.concatenate(cols + [wdw], axis=1))
    assert cvec.shape == (128, NCV), cvec.shape
    wr = np.asarray(moe_w_router[0], dtype=np.float32).reshape(KC, 128, NE).transpose(1, 0, 2).reshape(128, KC * NE)
    xe = np.concatenate([np.zeros((32, D), np.float32), np.asarray(meta_tokens, np.float32),
                         np.asarray(x[0], np.float32)], axis=0)
    shared = {
        "cvec": cvec, "wr": f(wr), "ident": np.eye(128, dtype=np.float32),
        "tri": np.triu(np.ones((128, 128), np.float32), 1),
        "gbc": f(np.broadcast_to(np.asarray(final_norm_g, np.float32)[None, :], (128, D))),
        "w_pw1": f(conv_w_pw1[0]), "w_pw2": f(conv_w_pw2[0]), "pool_w": f(pool_w_group[0]),
        "ffn_wg": f(ffn_w_gate[0]), "ffn_wu": f(ffn_w_up[0]), "ffn_wd": f(ffn_w_down[0]),
        "moe_wg": f(moe_w_gate[0]), "moe_wu": f(moe_w_up[0]), "moe_wd": f(moe_w_down[0]),
    }
    in_maps = []
    for c in range(NCORES):
        m = dict(shared)
        m["xT"] = f(xe[TOK * c: TOK * c + TW].T)
        um = np.ones((128, HALO), np.float32)
        if c == 0:
            um[:, :32] = 0.0
        m["umask"] = um
        in_maps.append(m)
    return in_maps


def kernel(**inputs):
    if "nc" not in _CACHE:
        _CACHE["nc"] = build_program()
    nc = _CACHE["nc"]
    in_maps = _prep_inputs(**inputs)
    res = run_bass_kernel_spmd(nc, in_maps, core_ids=list(range(NCORES)))
    outs = [np.asarray(r["out"]) for r in res.results]
    out = np.concatenate(outs, axis=0).reshape(1, SEQ, D).astype(np.float32)
    return out
```

```python
import numpy as np
import concourse.bass as bass
import concourse.mybir as mybir
from concourse.bass_utils import run_bass_kernel_spmd
from contextlib import ExitStack

F32 = mybir.dt.float32
BF16 = mybir.dt.bfloat16
ALU = mybir.AluOpType
AF = mybir.ActivationFunctionType

NCORES = 8
D = 1024
KC = 8
SEQ = 16384
NMETA = 16
TOK = SEQ // NCORES
HALO = 48
TW = TOK + HALO
DFF = 2816
DFE = 3584
NE = 8
CONVW = 31
RMS_EPS = 1e-6
LN_EPS = 1e-5
POOLW = (2, 4, 8, 16)
CAP = 768
NST = CAP // 128
NSLOT = NE * CAP
U32 = mybir.dt.uint32

TT0 = [(0, 432), (432, 416), (848, 416), (1264, 416), (1680, 416)]
TT1 = [(32, 400)] + TT0[1:]
TT2 = [(48, 384)] + TT0[1:]
TT3 = [(48 + 512 * i, 512) for i in range(4)]

CV = {}
_o = 0
for _n, _w in (("mix_g0", 8), ("mix_g1", 8), ("ffn_g0", 8), ("ffn_g1", 8), ("fin_g", 8),
               ("b_pw1", 16), ("b_dw", 8), ("ln_g", 8), ("ln_b", 8), ("b_pw2", 8),
               ("pool_s", 8), ("w_dw", 8 * CONVW)):
    CV[_n] = _o
    _o += _w
NCV = _o


class Buf:
    __slots__ = ("name", "w", "r")

    def __init__(self, name):
        self.name = name
        self.w = {}
        self.r = {}


class Sched:
    ENGS = ("pe", "act", "dve", "pool", "sp")

    def __init__(self, nc, es):
        self.nc = nc
        self.es = es
        self.eng = {"pe": nc.tensor, "act": nc.scalar, "dve": nc.vector,
                    "pool": nc.gpsimd, "sp": nc.sync}
        self.stream = {e: [] for e in self.ENGS}
        self.sems = {}
        self.cnt = {}
        self.seen = {e: {} for e in self.ENGS}
        for e in self.ENGS:
            self.new_sem(e)

    def new_sem(self, name):
        self.sems[name] = self.es.enter_context(self.nc.semaphore("s_" + name))
        self.cnt[name] = 0
        return name

    def op(self, e, fn, reads=(), writes=(), dma_sem=None, inc=True, after=(), waw=True):
        d = {}

        def add(tok):
            if tok is None:
                return
            s, v = tok
            if d.get(s, 0) < v:
                d[s] = v
        for b in reads:
            for s, v in b.w.items():
                add((s, v))
        for b in writes:
            if waw:
                for s, v in b.w.items():
                    if not (dma_sem is not None and s == dma_sem):
                        add((s, v))
            for s, v in b.r.items():
                add((s, v))
        for tok in after:
            add(tok)
        waits = []
        seen = self.seen[e]
        for s, v in d.items():
            if e == "pe" and s == "pe":
                continue
            if seen.get(s, 0) < v:
                waits.append((s, v))
                seen[s] = v
        if not inc:
            self.stream[e].append((waits, fn, None, 0))
            return None
        if dma_sem is None:
            s, n = e, 1
        else:
            s, n = dma_sem, 16
        self.cnt[s] += n
        tok = (s, self.cnt[s])
        self.stream[e].append((waits, fn, s, n))
        for b in writes:
            if waw:
                b.w = {s: tok[1]}
                b.r = {}
            else:
                b.w[s] = tok[1]
        for b in reads:
            if b.r.get(s, 0) < tok[1]:
                b.r[s] = tok[1]
        return tok

    def barrier(self):
        snap = dict(self.cnt)
        for e in self.ENGS:
            waits = []
            for s, v in snap.items():
                if v == 0 or (e == "pe" and s == "pe"):
                    continue
                if self.seen[e].get(s, 0) < v:
                    waits.append((s, v))
                    self.seen[e][s] = v
            if waits:
                self.stream[e].append((waits, None, None, 0))

    def replay(self, e):
        eng = self.eng[e]
        for waits, fn, s, n in self.stream[e]:
            for ws, wv in waits:
                eng.wait_ge(self.sems[ws], wv)
            if fn is None:
                continue
            ins = fn()
            if s is not None:
                ins.then_inc(self.sems[s], n)

    def run_block(self):
        nc = self.nc
        with nc.Block() as block:
            @block.tensor
            def _(t):
                self.replay("pe")

            @block.scalar
            def _(t):
                self.replay("act")

            @block.vector
            def _(t):
                self.replay("dve")

            @block.gpsimd
            def _(t):
                self.replay("pool")

            @block.sync
            def _(t):
                self.replay("sp")


def build_program(debug=False):
    nc = bass.Bass("TRN2", target_bir_lowering=False)
    dbg_t = [nc.dram_tensor(f"dbg{i}", [D, TW], F32, kind="ExternalOutput").ap() for i in range(4)] if debug else []

    def din(name, shape):
        return nc.dram_tensor(name, list(shape), F32, kind="ExternalInput").ap()

    xT = din("xT", (D, TW))
    umask_d = din("umask", (128, HALO))
    cvec_d = din("cvec", (128, NCV))
    wr_d = din("wr", (128, KC * NE))
    ident_d = din("ident", (128, 128))
    w_pw1 = din("w_pw1", (D, 2 * D))
    w_pw2 = din("w_pw2", (D, D))
    pool_w = din("pool_w", (4, 256, 256))
    ffn_wg = din("ffn_wg", (D, DFF))
    ffn_wu = din("ffn_wu", (D, DFF))
    ffn_wd = din("ffn_wd", (DFF, D))
    moe_wg = din("moe_wg", (NE, D, DFE))
    moe_wu = din("moe_wu", (NE, D, DFE))
    moe_wd = din("moe_wd", (NE, DFE, D))
    tri_d = din("tri", (128, 128))
    gbc_d = din("gbc", (128, D))
    out_d = nc.dram_tensor("out", [TOK, D], F32, kind="ExternalOutput").ap()
    xs_d = nc.dram_tensor("xs_scratch", [NSLOT, D], BF16).ap()
    ys_d = nc.dram_tensor("ys_scratch", [NSLOT, D], F32).ap()

    with ExitStack() as es:
        S = Sched(nc, es)

        def sb(name, shape, dt):
            return es.enter_context(nc.sbuf_tensor("sb_" + name, list(shape), dt))

        H = sb("H", (128, KC, TW), F32)
        B1 = sb("B1", (128, KC, TW), BF16)
        B2 = sb("B2", (128, KC * TW), BF16)
        WP1 = [sb(f"wp1_{b}", (128, KC, 512), BF16) for b in range(2)]
        WP2 = [sb(f"wp2_{b}", (128, KC, 512), BF16) for b in range(2)]
        WP3 = [sb(f"wp3_{b}", (128, 4, 1024), BF16) for b in range(2)]
        R1 = sb("R1", (128, 4096), BF16)
        Z = sb("Z", (128, KC, 416), BF16)
        TMP = [sb(f"tmp{i}", (128, 512), F32) for i in range(5)]
        cvec = sb("cvec", (128, NCV), F32)
        wr = sb("wr", (128, KC, NE), F32)
        wrg = sb("wrg", (128, KC, NE), F32)
        identf = sb("identf", (128, 128), F32)
        identb = sb("identb", (128, 128), BF16)
        onesb = sb("onesb", (128, 128), BF16)
        onesf = sb("onesf", (128, 128), F32)
        umask = sb("umask", (128, HALO), F32)
        ps = [es.enter_context(nc.psum_tensor(f"ps{i}", [128, 512], F32)) for i in range(8)]

        Y = B1[:].bitcast(F32)
        U = B2[:].rearrange("p (k n) -> p k n", k=KC)
        B2f = B2[:].bitcast(F32)
        SQ = R1[:, 0:KC * 432].rearrange("p (k n) -> p k n", k=KC)
        Abuf = [R1[:, i * 2048:(i + 1) * 2048].rearrange("p (m n) -> p m n", m=4) for i in range(2)]
        CWe = [B2f[:, i * 2048:(i + 1) * 2048] for i in range(2)]
        RSall = B2f[:, 6144:8192]
        Zf = Z[:].rearrange("p k n -> p (k n)").bitcast(F32)
        RT = Zf[:, 0:640].rearrange("p (j c) -> p j c", j=16)
        Mf = Zf[:, 768:896].rearrange("p (j e) -> p j e", j=16)
        Mb = Zf[:, 896:960].bitcast(BF16)
        WITHIN = Zf[:, 960:1088]
        OFF = Zf[:, 1088:1216].rearrange("p (j e) -> p j e", j=16)
        SLV = Zf[:, 1216:1344].rearrange("p (j e) -> p j e", j=16)
        S0F = Zf[:, 1344:1360]
        S1F = Zf[:, 1360:1376]
        S0U = Zf[:, 1376:1392].bitcast(U32)
        S1U = Zf[:, 1392:1408].bitcast(U32)
        EC = Zf[:, 1440:1568].rearrange("p (j e) -> p j e", j=16)
        SCR = Zf[:, 1568:1664]
        trib = sb("trib", (128, 128), BF16)
        OUTb = [B2f[:, i * 3456:(i + 1) * 3456].rearrange("p (k n) -> p k n", k=KC) for i in range(2)]
        assert tuple(Y.shape) == (128, KC, TW // 2), Y.shape

        P = [Buf(f"ps{i}") for i in range(8)]
        state = {"bank": 0, "dq": 0, "r1": "sq"}

        def cv(name, j=0):
            c = CV[name] + j
            return cvec[:, c:c + 1]

        def mm_group(pairs, n, reads, rows=128):
            i = state["bank"]
            state["bank"] = (i + 1) % 8
            L = len(pairs)
            for j, (l, r) in enumerate(pairs):
                edge = (j == 0 or j == L - 1)
                S.op("pe",
                     (lambda l=l, r=r, j=j, i=i: nc.tensor.matmul(
                         ps[i][:rows, :n], lhsT=l, rhs=r, start=(j == 0), stop=(j == L - 1))),
                     reads=reads if edge else (), writes=[P[i]] if edge else (),
                     inc=(j == L - 1))
            return i

        Bc = Buf("consts")
        sc = S.new_sem("dconst")
        trif = TMP[3][:, 0:128]
        for dst, src in ((cvec[:], cvec_d), (wr[:].rearrange("p k e -> p (k e)"), wr_d),
                         (identf[:], ident_d), (umask[:], umask_d), (trif, tri_d)):
            S.op("sp", (lambda dst=dst, src=src: nc.sync.dma_start(out=dst, in_=src)),
                 writes=[Bc], dma_sem=sc)
        S.op("dve", lambda: nc.vector.memset(onesb[:], 1.0), writes=[Bc])
        S.op("dve", lambda: nc.vector.memset(onesf[:], 1.0), writes=[Bc])
        S.op("dve", lambda: nc.vector.tensor_copy(out=identb[:], in_=identf[:]), reads=[Bc], writes=[Bc])
        for k in range(KC):
            S.op("dve", (lambda k=k: nc.vector.tensor_scalar(
                out=wrg[:, k, :], in0=wr[:, k, :], scalar1=cv("ffn_g1", k), scalar2=None, op0=ALU.mult)),
                reads=[Bc], writes=[Bc])

        Hb = [Buf(f"H{t}") for t in range(5)]
        sx = S.new_sem("dx")
        dbg_sem = S.new_sem("ddbg") if debug else None

        def dump(i):
            if not debug:
                return
            S.barrier()
            for k in range(KC):
                S.op("sp", (lambda k=k, i=i: nc.sync.dma_start(out=dbg_t[i][k * 128:(k + 1) * 128, :], in_=H[:, k, :])),
                     reads=Hb, dma_sem=dbg_sem)
            S.barrier()
        xTv = xT.rearrange("(k p) n -> p k n", p=128)
        sxs = [sx] + [S.new_sem(f"dx{t}") for t in range(1, 5)]
        for t, (o, n) in enumerate(TT0):
            S.op("sp", (lambda o=o, n=n: nc.sync.dma_start(out=H[:, :, o:o + n], in_=xTv[:, :, o:o + n])),
                 writes=[Hb[t]], dma_sem=sxs[t])

        bnd = {}

        def _mk_bound():
            reg = nc.gpsimd.alloc_register("slot_bound")
            ins = nc.gpsimd.reg_mov(reg, NSLOT - 1)
            bnd["v"] = nc.gpsimd.snap(reg)
            return ins
        S.op("pool", _mk_bound, inc=False)

        XS0b = Buf("xs0")
        ZTb = Buf("zt")
        ZT = TMP[4][:, :].bitcast(BF16)
        szf = S.new_sem("dzf")
        S.op("dve", lambda: nc.vector.memset(TMP[4][:, :], 0.0), writes=[ZTb])
        zsem = [szf, S.new_sem("dzf1"), S.new_sem("dzf2")]
        nb = NSLOT // 128 // 8
        for bq in range(nb):
            zs = zsem[bq % 3]
            thr = []
            if bq >= 2:
                ps_ = zsem[(bq - 2) % 3]
                thr = [(ps_, S.cnt[ps_])]
            for q in range(8 * bq, 8 * bq + 8):
                S.op("sp", (lambda q=q: nc.sync.dma_start(out=xs_d[q * 128:(q + 1) * 128, :], in_=ZT)),
                     reads=[ZTb], writes=[XS0b], dma_sem=zs, after=thr, waw=False)
        S.op("dve", lambda: nc.vector.tensor_copy(out=trib[:], in_=trif[:]), reads=[Bc], writes=[Bc])

        WBb = [Buf("wb0"), Buf("wb1")]
        wsem = [S.new_sem("dw0"), S.new_sem("dw1")]

        def wload(b, parts):
            for dst, src in parts:
                S.op("pool", (lambda dst=dst, src=src: nc.gpsimd.dma_start(out=dst, in_=src)),
                     writes=[WBb[b]], dma_sem=wsem[b])

        def kview(ap):
            return ap.rearrange("(k p) n -> p k n", p=128)

        SQb, RSb = Buf("sq"), Buf("rs")

        def rmsnorm(tiles, tbufs_in, gname, out_fn, out_bufs, eps=RMS_EPS, rs_fn=None, rs_buf=None):
            if state["r1"] != "sq":
                S.barrier()
                state["r1"] = "sq"
            for ti, (o, n) in enumerate(tiles):
                hb = tbufs_in[ti]
                rsap = (lambda o=o, n=n: TMP[0][:, :n]) if rs_fn is None else (lambda o=o, n=n: rs_fn(o, n))
                rsb = RSb if rs_buf is None else rs_buf
                S.op("act", (lambda o=o, n=n: nc.scalar.activation(
                    out=SQ[:, :, :n], in_=H[:, :, o:o + n], func=AF.Square)),
                    reads=[hb], writes=[SQb])
                i = mm_group([(onesb[:], SQ[:, k, :n]) for k in range(KC)], n, [Bc, SQb])
                S.op("act", (lambda i=i, n=n, rsap=rsap: nc.scalar.activation(
                    out=rsap(), in_=ps[i][:, :n], func=AF.Sqrt, bias=eps_ap(eps), scale=1.0 / D)),
                    reads=[P[i], Bc], writes=[rsb])
                S.op("dve", (lambda rsap=rsap: nc.vector.reciprocal(out=rsap(), in_=rsap())),
                     reads=[rsb], writes=[rsb])
                for k in range(KC):
                    S.op("dve", (lambda k=k, o=o, n=n, rsap=rsap: nc.vector.scalar_tensor_tensor(
                        out=out_fn(k, o, n), in0=H[:, k, o:o + n], scalar=cv(gname, k),
                        in1=rsap(), op0=ALU.mult, op1=ALU.mult)),
                        reads=[hb, rsb, Bc], writes=[out_bufs[ti]])

        epsc = sb("epsc", (128, 2), F32)
        S.op("dve", lambda: nc.vector.memset(epsc[:, 0:1], RMS_EPS), writes=[Bc])
        S.op("dve", lambda: nc.vector.memset(epsc[:, 1:2], LN_EPS), writes=[Bc])

        def eps_ap(eps):
            return epsc[:, 0:1] if eps == RMS_EPS else epsc[:, 1:2]

        HNb = [Buf(f"HN{t}") for t in range(5)]
        for s in range(2):
            wload(s, [(WP1[s][:], kview(w_pw1[:, 512 * s:512 * s + 512])),
                      (WP2[s][:], kview(w_pw1[:, D + 512 * s:D + 512 * s + 512]))])
        rmsnorm(TT0, Hb, "mix_g0", lambda k, o, n: B1[:, k, o:o + n], HNb)

        Ub = Buf("U")
        SIGb = [Buf("sig0"), Buf("sig1")]
        sgi = 0
        for s in range(2):
            for ti, (o, n) in enumerate(TT0):
                for mc in range(4):
                    ch = 4 * s + mc
                    ia = mm_group([(WP1[s][:, k, mc * 128:(mc + 1) * 128], B1[:, k, o:o + n]) for k in range(KC)],
                                  n, [WBb[s], HNb[ti]])
                    ig = mm_group([(WP2[s][:, k, mc * 128:(mc + 1) * 128], B1[:, k, o:o + n]) for k in range(KC)],
                                  n, [WBb[s], HNb[ti]])
                    sg = sgi % 2
                    sgi += 1
                    S.op("act", (lambda ig=ig, n=n, ch=ch, sg=sg: nc.scalar.activation(
                        out=TMP[1 + sg][:, :n], in_=ps[ig][:, :n], func=AF.Sigmoid, bias=cv("b_pw1", 8 + ch))),
                        reads=[P[ig], Bc], writes=[SIGb[sg]])
                    S.op("dve", (lambda ia=ia, n=n, ch=ch, sg=sg, o=o: nc.vector.scalar_tensor_tensor(
                        out=U[:, ch, o:o + n], in0=ps[ia][:, :n], scalar=cv("b_pw1", ch),
                        in1=TMP[1 + sg][:, :n], op0=ALU.add, op1=ALU.mult)),
                        reads=[P[ia], SIGb[sg], Bc], writes=[Ub])
        for k in range(KC):
            S.op("dve", (lambda k=k: nc.vector.tensor_tensor(
                out=U[:, k, 0:HALO], in0=U[:, k, 0:HALO], in1=umask[:], op=ALU.mult)),
                reads=[Ub, Bc], writes=[Ub])

        S.barrier()

        W2b = [Buf("w2_0"), Buf("w2_1")]
        w2sem = [S.new_sem("dw2_0"), S.new_sem("dw2_1")]
        for s in range(2):
            S.op("pool", (lambda s=s: nc.gpsimd.dma_start(
                out=WP1[s][:], in_=kview(w_pw2[:, 512 * s:512 * s + 512]))),
                writes=[W2b[s]], dma_sem=w2sem[s])

        DGb = [Buf("dg0"), Buf("dg1")]
        Yb = Buf("Y")
        Zb = Buf("Z")
        MUb, M2b = Buf("mu"), ZTb
        groups = [[0, 1], [2, 3], [4]]
        dgi = 0
        for grp in groups:
            gbase = TT1[grp[0]][0]
            for c in range(KC):
                db = dgi % 2
                dgi += 1
                dg = WP3[db][:].rearrange("p a b -> p (a b)")
                for k in range(CONVW):
                    wcol = cvec[:, CV["w_dw"] + c * CONVW + k: CV["w_dw"] + c * CONVW + k + 1]
                    if k % 3 == 0:
                        S.op("pool", (lambda k=k, dg=dg, wcol=wcol: nc.gpsimd.tensor_scalar(
                            out=dg[:, k * 128:(k + 1) * 128], in0=identf[:], scalar1=wcol,
                            scalar2=0.0, op0=ALU.mult, op1=ALU.add)),
                            reads=[Bc], writes=[DGb[db]], waw=False)
                    else:
                        S.op("dve", (lambda k=k, dg=dg, wcol=wcol: nc.vector.tensor_scalar(
                            out=dg[:, k * 128:(k + 1) * 128], in0=identf[:], scalar1=wcol,
                            scalar2=None, op0=ALU.mult)),
                            reads=[Bc], writes=[DGb[db]], waw=False)
                for ti in grp:
                    o, n = TT1[ti]
                    i = mm_group([(dg[:, k * 128:(k + 1) * 128], U[:, c, o - 30 + k: o - 30 + k + n])
                                  for k in range(CONVW)], n, [DGb[db], Ub])
                    S.op("act", (lambda i=i, n=n, c=c, yo=o - gbase: nc.scalar.activation(
                        out=Y[:, c, yo:yo + n], in_=ps[i][:, :n], func=AF.Identity, bias=cv("b_dw", c))),
                        reads=[P[i], Bc], writes=[Yb])
            for ti in grp:
                o, n = TT1[ti]
                yo = o - gbase
                imu = mm_group([(onesf[:], Y[:, k, yo:yo + n]) for k in range(KC)], n, [Bc, Yb])
                S.op("act", (lambda yo=yo, n=n: nc.scalar.activation(
                    out=SQ[:, :, :n], in_=Y[:, :, yo:yo + n], func=AF.Square)),
                    reads=[Yb], writes=[SQb])
                isq = mm_group([(onesb[:], SQ[:, k, :n]) for k in range(KC)], n, [Bc, SQb])
                S.op("dve", (lambda imu=imu, n=n: nc.vector.tensor_scalar(
                    out=TMP[3][:, :n], in0=ps[imu][:, :n], scalar1=1.0 / D, scalar2=None, op0=ALU.mult)),
                    reads=[P[imu]], writes=[MUb])
                S.op("dve", (lambda n=n: nc.vector.tensor_tensor(
                    out=TMP[4][:, :n], in0=TMP[3][:, :n], in1=TMP[3][:, :n], op=ALU.mult)),
                    reads=[MUb], writes=[M2b])
                S.op("dve", (lambda isq=isq, n=n: nc.vector.scalar_tensor_tensor(
                    out=TMP[4][:, :n], in0=ps[isq][:, :n], scalar=1.0 / D, in1=TMP[4][:, :n],
                    op0=ALU.mult, op1=ALU.subtract)),
                    reads=[P[isq], M2b], writes=[M2b])
                S.op("act", (lambda n=n: nc.scalar.activation(
                    out=TMP[0][:, :n], in_=TMP[4][:, :n], func=AF.Sqrt, bias=eps_ap(LN_EPS), scale=1.0)),
                    reads=[M2b, Bc], writes=[RSb])
                S.op("dve", (lambda n=n: nc.vector.reciprocal(out=TMP[0][:, :n], in_=TMP[0][:, :n])),
                     reads=[RSb], writes=[RSb])
                for c in range(KC):
                    S.op("dve", (lambda c=c, yo=yo, n=n: nc.vector.tensor_tensor(
                        out=Y[:, c, yo:yo + n], in0=Y[:, c, yo:yo + n], in1=TMP[3][:, :n], op=ALU.subtract)),
                        reads=[Yb, MUb], writes=[Yb])
                    S.op("dve", (lambda c=c, yo=yo, n=n: nc.vector.tensor_tensor(
                        out=Y[:, c, yo:yo + n], in0=Y[:, c, yo:yo + n], in1=TMP[0][:, :n], op=ALU.mult)),
                        reads=[Yb, RSb], writes=[Yb])
                    S.op("act", (lambda c=c, yo=yo, n=n: nc.scalar.activation(
                        out=Z[:, c, :n], in_=Y[:, c, yo:yo + n], func=AF.Silu,
                        bias=cv("ln_b", c), scale=cv("ln_g", c))),
                        reads=[Yb, Bc], writes=[Zb])
                for oc in range(KC):
                    s2, mc = divmod(oc, 4)
                    i = mm_group([(WP1[s2][:, k, mc * 128:(mc + 1) * 128], Z[:, k, :n]) for k in range(KC)],
                                 n, [W2b[s2], Zb])
                    S.op("dve", (lambda i=i, oc=oc, o=o, n=n: nc.vector.scalar_tensor_tensor(
                        out=H[:, oc, o:o + n], in0=ps[i][:, :n], scalar=cv("b_pw2", oc),
                        in1=H[:, oc, o:o + n], op0=ALU.add, op1=ALU.add)),
                        reads=[P[i], Bc, Hb[ti]], writes=[Hb[ti]])

        S.barrier()
        dump(0)

        SGb = [Buf("sg0"), Buf("sg1")]
        T2b = [Buf("t2a"), Buf("t2b")]
        Ab = [Buf("A0"), Buf("A1")]
        ffn_state = {"sg": 0, "a": 0, "slab": 0}

        def ffn(slabs, tiles, hnb, hb, cw=None):
            st = ffn_state
            if state["r1"] != "a":
                S.barrier()
                state["r1"] = "a"
            b0 = st["slab"] % 2
            wload(b0, slabs[0][1])
            for si, (wdt, _) in enumerate(slabs):
                b = (st["slab"] + si) % 2
                if si + 1 < len(slabs):
                    wload(1 - b, slabs[si + 1][1])
                nm = wdt // 128
                for ti, (o, n) in enumerate(tiles):
                    a = st["a"] % 2
                    st["a"] += 1
                    for mc in range(nm):
                        ig = mm_group([(WP1[b][:, k, mc * 128:(mc + 1) * 128], B1[:, k, o:o + n]) for k in range(KC)],
                                      n, [WBb[b], hnb[ti]])
                        iu = mm_group([(WP2[b][:, k, mc * 128:(mc + 1) * 128], B1[:, k, o:o + n]) for k in range(KC)],
                                      n, [WBb[b], hnb[ti]])
                        sg = st["sg"] % 2
                        st["sg"] += 1
                        S.op("act", (lambda ig=ig, n=n, sg=sg: nc.scalar.activation(
                            out=TMP[1 + sg][:, :n], in_=ps[ig][:, :n], func=AF.Silu)),
                            reads=[P[ig]], writes=[SGb[sg]])
                        if cw is None:
                            S.op("dve", (lambda iu=iu, n=n, sg=sg, a=a, mc=mc: nc.vector.tensor_tensor(
                                out=Abuf[a][:, mc, :n], in0=ps[iu][:, :n], in1=TMP[1 + sg][:, :n], op=ALU.mult)),
                                reads=[P[iu], SGb[sg]], writes=[Ab[a]])
                        else:
                            cwfn, cwb = cw
                            S.op("dve", (lambda n=n, sg=sg, o=o: nc.vector.tensor_tensor(
                                out=TMP[3 + sg][:, :n], in0=TMP[1 + sg][:, :n], in1=cwfn(o, n), op=ALU.mult)),
                                reads=[SGb[sg], cwb], writes=[T2b[sg]])
                            S.op("dve", (lambda iu=iu, n=n, sg=sg, a=a, mc=mc: nc.vector.tensor_tensor(
                                out=Abuf[a][:, mc, :n], in0=ps[iu][:, :n], in1=TMP[3 + sg][:, :n], op=ALU.mult)),
                                reads=[P[iu], T2b[sg]], writes=[Ab[a]])
                    for oc in range(KC):
                        i = mm_group([(WP3[b][:, mc, oc * 128:(oc + 1) * 128], Abuf[a][:, mc, :n]) for mc in range(nm)],
                                     n, [WBb[b], Ab[a]])
                        S.op("dve", (lambda i=i, oc=oc, o=o, n=n: nc.vector.tensor_tensor(
                            out=H[:, oc, o:o + n], in0=ps[i][:, :n], in1=H[:, oc, o:o + n], op=ALU.add)),
                            reads=[P[i], hb[ti]], writes=[hb[ti]])
            st["slab"] += len(slabs)

        def ffn_slabs(wg, wu, wd, dff):
            out = []
            off = 0
            while off < dff:
                wdt = min(512, dff - off)
                nm = wdt // 128
                out.append((wdt, off))
                off += wdt
            res = []
            for wdt, off in out:
                nm = wdt // 128

                def mk(b, wdt=wdt, off=off, nm=nm):
                    return [(WP1[b][:, :, :wdt], kview(wg[:, off:off + wdt])),
                            (WP2[b][:, :, :wdt], kview(wu[:, off:off + wdt])),
                            (WP3[b][:, :nm, :], wd[off:off + wdt, :].rearrange("(m p) n -> p m n", p=128))]
                res.append((wdt, mk))
            return res

        def run_ffn(wg, wu, wd, dff, tiles, hnb, hb, cw=None):
            sl = ffn_slabs(wg, wu, wd, dff)
            base = ffn_state["slab"]
            slabs = [(wdt, mk((base + si) % 2)) for si, (wdt, mk) in enumerate(sl)]
            ffn(slabs, tiles, hnb, hb, cw)

        rmsnorm(TT1, Hb, "ffn_g0", lambda k, o, n: B1[:, k, o:o + n], HNb)
        run_ffn(ffn_wg, ffn_wu, ffn_wd, DFF, TT1, HNb, Hb)

        dump(1)
        spw = S.new_sem("dpw")
        PW = WP3[0][:].rearrange("p a b -> p (a b)")[:, 0:2048].rearrange("p (g k n) -> p g k n", g=4, k=2)
        for g in range(4):
            S.op("pool", (lambda g=g: nc.gpsimd.dma_start(
                out=PW[:, g, :, :], in_=pool_w[g].rearrange("(k p) n -> p k n", p=128))),
                writes=[WBb[0]], dma_sem=spw)
        rmsnorm(TT1, Hb, "mix_g1", lambda k, o, n: B1[:, k, o:o + n], HNb)
        W1f = WP1[0][:].rearrange("p a b -> p (a b)")
        PWS = W1f[:, 0:2048].rearrange("p (g k n) -> p g k n", g=4, k=2)
        PWN = W1f[:, 2048:4096].rearrange("p (g k n) -> p g k n", g=4, k=2)
        for g in range(4):
            S.op("dve", (lambda g=g: nc.vector.tensor_scalar(
                out=PWS[:, g, :, :], in0=PW[:, g, :, :], scalar1=1.0 / POOLW[g], scalar2=None, op0=ALU.mult)),
                reads=[WBb[0]], writes=[WBb[0]])
        for g in range(4):
            S.op("dve", (lambda g=g: nc.vector.tensor_scalar(
                out=PWN[:, g, :, :], in0=PW[:, g, :, :], scalar1=1.0 / POOLW[g] - 1.0, scalar2=None, op0=ALU.mult)),
                reads=[WBb[0]], writes=[WBb[0]])
        for ti, (o, n) in enumerate(TT2):
            rd = [HNb[ti]] + ([HNb[ti - 1]] if ti > 0 else [])
            for g in range(4):
                w = POOLW[g]
                for oc in range(2):
                    ch = 2 * g + oc
                    pairs = []
                    for kc in range(2):
                        pairs.append((PWN[:, g, kc, oc * 128:(oc + 1) * 128], B1[:, 2 * g + kc, o:o + n]))
                        for dd in range(1, w):
                            pairs.append((PWS[:, g, kc, oc * 128:(oc + 1) * 128], B1[:, 2 * g + kc, o - dd:o - dd + n]))
                    i = mm_group(pairs, n, [WBb[0]] + rd)
                    S.op("dve", (lambda i=i, ch=ch, o=o, n=n: nc.vector.scalar_tensor_tensor(
                        out=H[:, ch, o:o + n], in0=ps[i][:, :n], scalar=cv("pool_s", ch), in1=H[:, ch, o:o + n],
                        op0=ALU.mult, op1=ALU.add)),
                        reads=[P[i], Hb[ti], Bc], writes=[Hb[ti]])

        dump(2)
        RSAb = Buf("rsall")
        rmsnorm(TT2, Hb, "ffn_g1", lambda k, o, n: B1[:, k, o:o + n], HNb,
                rs_fn=lambda o, n: RSall[:, o - HALO:o - HALO + n], rs_buf=RSAb)
        RTb = [Buf("rt")] * 16
        rb = RTb[0]
        ir = state["bank"]
        state["bank"] = (ir + 1) % 8
        for j in range(16):
            c0 = HALO + 128 * j
            for k in range(KC):
                first = (j == 0 and k == 0)
                S.op("pe", (lambda j=j, k=k, c0=c0: nc.tensor.matmul(
                    ps[ir][:, 8 * j:8 * j + 8], lhsT=H[:, k, c0:c0 + 128], rhs=wrg[:, k, :],
                    start=(k == 0), stop=(k == KC - 1))),
                    reads=Hb + [Bc] if first else (), writes=[P[ir]] if first else (), inc=False)
            last = (j == 15)
            S.op("pe", (lambda j=j: nc.tensor.matmul(
                ps[ir][:, 128 + 2 * j:128 + 2 * j + 2], lhsT=RSall[:, 128 * j:128 * j + 128], rhs=identf[:, 0:2],
                start=True, stop=True)),
                reads=[RSAb, Bc] + Hb if (j == 0 or last) else (), writes=[P[ir]] if last else (), inc=last)
        RAW = RT[:, :, 0:8]
        S.op("dve", lambda: nc.vector.tensor_copy(
            out=RAW, in_=ps[ir][:, 0:128].rearrange("p (j e) -> p j e", j=16)), reads=[P[ir]], writes=[rb])
        S.op("dve", lambda: nc.vector.tensor_copy(
            out=RT[:, :, 8], in_=ps[ir][:, 128:160].rearrange("p (j c) -> p j c", j=16)[:, :, 0]),
            reads=[P[ir]], writes=[rb])
        S.op("dve", lambda: nc.vector.tensor_reduce(out=RT[:, :, 9], in_=RAW, axis=mybir.AxisListType.X, op=ALU.max),
             reads=[rb], writes=[rb])
        def first_max_mask(src0, mcol, dst0):
            S.op("dve", lambda: nc.vector.memset(RT[:, :, 14], 0.0), reads=[rb], writes=[rb])
            for e in range(NE):
                S.op("dve", (lambda e=e: nc.vector.tensor_tensor(
                    out=RT[:, :, dst0 + e], in0=RT[:, :, src0 + e], in1=RT[:, :, mcol], op=ALU.is_equal)),
                    reads=[rb], writes=[rb])
                S.op("dve", (lambda e=e: nc.vector.scalar_tensor_tensor(
                    out=RT[:, :, dst0 + e], in0=RT[:, :, 14], scalar=1.0, in1=RT[:, :, dst0 + e],
                    op0=ALU.subtract, op1=ALU.mult)), reads=[rb], writes=[rb])
                S.op("dve", (lambda e=e: nc.vector.tensor_tensor(
                    out=RT[:, :, 14], in0=RT[:, :, 14], in1=RT[:, :, dst0 + e], op=ALU.subtract)),
                    reads=[rb], writes=[rb])
            S.op("dve", lambda: nc.vector.tensor_scalar(
                out=RT[:, :, dst0:dst0 + NE], in0=RT[:, :, dst0:dst0 + NE], scalar1=-1.0, scalar2=None, op0=ALU.mult),
                reads=[rb], writes=[rb])
        first_max_mask(0, 9, 16)
        S.op("dve", lambda: nc.vector.scalar_tensor_tensor(
            out=RT[:, :, 24:32], in0=RT[:, :, 16:24], scalar=-1.0e30, in1=RAW, op0=ALU.mult, op1=ALU.add),
            reads=[rb], writes=[rb])
        S.op("dve", lambda: nc.vector.tensor_reduce(out=RT[:, :, 10], in_=RT[:, :, 24:32], axis=mybir.AxisListType.X,
                                                    op=ALU.max), reads=[rb], writes=[rb])
        first_max_mask(24, 10, 32)
        S.op("dve", lambda: nc.vector.tensor_tensor(out=RT[:, :, 11], in0=RT[:, :, 9], in1=RT[:, :, 10], op=ALU.subtract),
             reads=[rb], writes=[rb])
        S.op("dve", lambda: nc.vector.tensor_tensor(out=RT[:, :, 11], in0=RT[:, :, 11], in1=RT[:, :, 8], op=ALU.mult),
             reads=[rb], writes=[rb])
        S.op("act", lambda: nc.scalar.activation(out=RT[:, :, 12], in_=RT[:, :, 11], func=AF.Sigmoid),
             reads=[rb], writes=[rb])
        S.op("dve", lambda: nc.vector.tensor_scalar(out=RT[:, :, 13], in0=RT[:, :, 12], scalar1=-1.0, scalar2=1.0,
                                                    op0=ALU.mult, op1=ALU.add), reads=[rb], writes=[rb])

        IDXb = Buf("idx")
        allrt = RTb
        S.op("dve", lambda: nc.vector.tensor_tensor(out=Mf[:], in0=RT[:, :, 16:24], in1=RT[:, :, 32:40], op=ALU.add),
             reads=allrt, writes=[IDXb])
        S.op("dve", lambda: nc.vector.tensor_copy(out=Mb, in_=Mf[:].rearrange("p j e -> p (j e)")),
             reads=[IDXb], writes=[IDXb])
        iw = mm_group([(trib[:], Mb)], 128, [Bc, IDXb])
        ic = mm_group([(onesb[:], Mb)], 128, [Bc, IDXb])
        S.op("dve", lambda: nc.vector.memset(OFF[:, 0, :], 0.0), reads=[IDXb], writes=[IDXb])
        for j in range(1, 16):
            S.op("dve", (lambda j=j: nc.vector.tensor_tensor(
                out=OFF[:, j, :], in0=OFF[:, j - 1, :], in1=ps[ic][:, (j - 1) * NE:j * NE], op=ALU.add)),
                reads=[IDXb, P[ic]], writes=[IDXb])
        for e in range(NE):
            S.op("dve", (lambda e=e: nc.vector.memset(EC[:, :, e:e + 1], float(e * CAP))), reads=[IDXb], writes=[IDXb])
        Offl = OFF.rearrange("p j e -> p (j e)")
        SLVl = SLV.rearrange("p j e -> p (j e)")
        ECl = EC.rearrange("p j e -> p (j e)")
        S.op("dve", lambda: nc.vector.tensor_tensor(out=Offl, in0=Offl, in1=ps[iw][:, 0:128], op=ALU.add),
             reads=[IDXb, P[iw]], writes=[IDXb])
        S.op("dve", lambda: nc.vector.tensor_tensor(out=SLVl, in0=Offl, in1=ECl, op=ALU.add), reads=[IDXb], writes=[IDXb])
        S.op("dve", lambda: nc.vector.tensor_scalar(out=Offl, in0=Offl, scalar1=float(CAP), scalar2=1.0e6,
                                                    op0=ALU.is_ge, op1=ALU.mult), reads=[IDXb], writes=[IDXb])
        S.op("dve", lambda: nc.vector.tensor_tensor(out=SLVl, in0=SLVl, in1=Offl, op=ALU.add), reads=[IDXb], writes=[IDXb])
        for (msk, SF, SU, wc) in ((RT[:, :, 16:24], S0F, S0U, 12), (RT[:, :, 32:40], S1F, S1U, 13)):
            S.op("dve", (lambda msk=msk: nc.vector.tensor_tensor(out=Mf[:], in0=msk, in1=SLV, op=ALU.mult)),
                 reads=[IDXb] + allrt, writes=[IDXb])
            S.op("dve", (lambda SF=SF: nc.vector.tensor_reduce(out=SF, in_=Mf[:], axis=mybir.AxisListType.X, op=ALU.add)),
                 reads=[IDXb], writes=[IDXb])
            S.op("dve", (lambda SF=SF, SU=SU: nc.vector.tensor_copy(out=SU, in_=SF)), reads=[IDXb], writes=[IDXb])
            S.op("dve", (lambda SF=SF: nc.vector.tensor_scalar(out=SF, in0=SF, scalar1=float(NSLOT), scalar2=None,
                                                               op0=ALU.is_lt)), reads=[IDXb], writes=[IDXb])
            S.op("dve", (lambda SF=SF, wc=wc: nc.vector.tensor_tensor(out=RT[:, :, wc], in0=RT[:, :, wc], in1=SF, op=ALU.mult)),
                 reads=[IDXb] + allrt, writes=allrt)

        all_slabs = [(e, sl) for e in range(NE) for sl in range(DFE // 512)]

        def slab_parts(e, sl, b):
            off = 512 * sl
            return [(WP1[b][:], kview(moe_wg[e][:, off:off + 512])),
                    (WP2[b][:], kview(moe_wu[e][:, off:off + 512])),
                    (WP3[b][:], moe_wd[e][off:off + 512, :].rearrange("(m p) n -> p m n", p=128))]
        sbase = ffn_state["slab"]
        wload(sbase % 2, slab_parts(0, 0, sbase % 2))

        HT = [B2[:, r * 1024:(r + 1) * 1024] for r in range(4)]
        HTb = [Buf(f"ht{r}") for r in range(4)]
        scs = [S.new_sem(f"dsc{r}") for r in range(4)]
        XSb = Buf("xs")
        XSb.w = dict(XS0b.w)
        prev_sc = []
        for j in range(16):
            c0 = HALO + 128 * j
            r = j % 4
            for half in range(2):
                i = state["bank"]
                state["bank"] = (i + 1) % 8
                for kk in range(4):
                    S.op("pe", (lambda i=i, kk=kk, half=half, c0=c0: nc.tensor.matmul(
                        ps[i][:, kk * 128:(kk + 1) * 128], lhsT=B1[:, 4 * half + kk, c0:c0 + 128], rhs=identb[:],
                        start=True, stop=True)),
                        reads=HNb + [Bc] if kk in (0, 3) else (), writes=[P[i]] if kk in (0, 3) else (), inc=(kk == 3))
                if half == 0:
                    S.op("act", (lambda i=i, r=r: nc.scalar.activation(out=HT[r][:, 0:512], in_=ps[i][:, :], func=AF.Identity)),
                         reads=[P[i]], writes=[HTb[r]], waw=False)
                else:
                    S.op("dve", (lambda i=i, r=r: nc.vector.tensor_copy(out=HT[r][:, 512:1024], in_=ps[i][:, :])),
                         reads=[P[i]], writes=[HTb[r]], waw=False)
            for SU in (S0U, S1U):
                S.op("pool", (lambda SU=SU, j=j, r=r: nc.gpsimd.indirect_dma_start(
                    out=xs_d, out_offset=bass.IndirectOffsetOnAxis(SU[:, j:j + 1], 0), in_=HT[r], in_offset=None,
                    bounds_check=bnd["v"], oob_is_err=False)),
                    reads=[HTb[r], IDXb], writes=[XSb], dma_sem=scs[r], waw=False, after=list(prev_sc))
                prev_sc[:] = [(scs[r], S.cnt[scs[r]])]

        if state["r1"] != "a":
            S.barrier()
            state["r1"] = "a"
        XT = [B1[:].rearrange("p k n -> p (k n)")[:, 12288 + r * 1024: 12288 + (r + 1) * 1024] for r in range(3)]
        XTb = [Buf(f"xt{r}") for r in range(3)]
        xts = [S.new_sem(f"dxt{r}") for r in range(3)]
        XG = [B1[:].rearrange("p k n -> p (k n)")[:, g * 6144:(g + 1) * 6144].rearrange("p (k n) -> p k n", k=KC)
              for g in range(2)]
        XGb = [Buf("xg0"), Buf("xg1")]
        YA = B2f[:, 0:NST * 1024].rearrange("p (s n) -> p s n", s=NST)
        YAb = [Buf(f"ya{st}") for st in range(NST)]
        YSDb = Buf("ysd")
        yst = [S.new_sem("dys0"), S.new_sem("dys1")]
        xti = 0
        evi = 0
        gtiles = [(0, CAP // 2), (CAP // 2, CAP // 2)]
        nst_t = (CAP // 2) // 128
        def emit_xload(e):
            nonlocal xti, evi
            g = e % 2
            for st in range(NST):
                xr = xti % 3
                xti += 1
                S.op("sp", (lambda e=e, st=st, xr=xr: nc.sync.dma_start(
                    out=XT[xr], in_=xs_d[e * CAP + st * 128: e * CAP + (st + 1) * 128, :])),
                    reads=[XSb], writes=[XTb[xr]], dma_sem=xts[xr])
                for half in range(2):
                    i = state["bank"]
                    state["bank"] = (i + 1) % 8
                    for kk in range(4):
                        S.op("pe", (lambda i=i, kk=kk, half=half, xr=xr: nc.tensor.matmul(
                            ps[i][:, kk * 128:(kk + 1) * 128],
                            lhsT=XT[xr][:, (4 * half + kk) * 128:(4 * half + kk + 1) * 128], rhs=identb[:],
                            start=True, stop=True)),
                            reads=[XTb[xr], Bc] if kk in (0, 3) else (), writes=[P[i]] if kk in (0, 3) else (),
                            inc=(kk == 3))
                    dst = XG[g][:, 4 * half:4 * half + 4, st * 128:(st + 1) * 128]
                    srcv = ps[i][:, :].rearrange("p (a b) -> p a b", a=4)
                    evi += 1
                    if evi % 2:
                        S.op("act", (lambda dst=dst, srcv=srcv: nc.scalar.activation(out=dst, in_=srcv, func=AF.Identity)),
                             reads=[P[i]], writes=[XGb[g]], waw=False)
                    else:
                        S.op("dve", (lambda dst=dst, srcv=srcv: nc.vector.tensor_copy(out=dst, in_=srcv)),
                             reads=[P[i]], writes=[XGb[g]], waw=False)
        emit_xload(0)
        for si, (e, sl) in enumerate(all_slabs):
            b = (sbase + si) % 2
            if si + 1 < len(all_slabs):
                wload(1 - b, slab_parts(all_slabs[si + 1][0], all_slabs[si + 1][1], 1 - b))
            g = e % 2
            if sl == 2 and e + 1 < NE:
                emit_xload(e + 1)
            tile_a = {}
            for ti, (o, n) in enumerate(gtiles):
                a = ffn_state["a"] % 2
                ffn_state["a"] += 1
                tile_a[ti] = a
                for mc in range(4):
                    ig = mm_group([(WP1[b][:, k, mc * 128:(mc + 1) * 128], XG[g][:, k, o:o + n]) for k in range(KC)],
                                  n, [WBb[b], XGb[g]])
                    iu = mm_group([(WP2[b][:, k, mc * 128:(mc + 1) * 128], XG[g][:, k, o:o + n]) for k in range(KC)],
                                  n, [WBb[b], XGb[g]])
                    sg = ffn_state["sg"] % 2
                    ffn_state["sg"] += 1
                    S.op("act", (lambda ig=ig, n=n, sg=sg: nc.scalar.activation(
                        out=TMP[1 + sg][:, :n], in_=ps[ig][:, :n], func=AF.Silu)),
                        reads=[P[ig]], writes=[SGb[sg]])
                    S.op("dve", (lambda iu=iu, n=n, sg=sg, a=a, mc=mc: nc.vector.tensor_tensor(
                        out=Abuf[a][:, mc, :n], in0=ps[iu][:, :n], in1=TMP[1 + sg][:, :n], op=ALU.mult)),
                        reads=[P[iu], SGb[sg]], writes=[Ab[a]])
            for ti, (o, n) in enumerate(gtiles):
                a = tile_a[ti]
                for s3 in range(nst_t):
                    st = ti * nst_t + s3
                    for half in range(2):
                        i = mm_group([(Abuf[a][:, mc, s3 * 128:(s3 + 1) * 128], WP3[b][:, mc, half * 512:(half + 1) * 512])
                                      for mc in range(4)], 512, [WBb[b], Ab[a]])
                        if sl == 0:
                            S.op("dve", (lambda i=i, st=st, half=half: nc.vector.tensor_copy(
                                out=YA[:, st, half * 512:(half + 1) * 512], in_=ps[i][:, :])),
                                reads=[P[i]], writes=[YAb[st]], waw=(half == 0))
                        else:
                            S.op("dve", (lambda i=i, st=st, half=half: nc.vector.tensor_tensor(
                                out=YA[:, st, half * 512:(half + 1) * 512], in0=ps[i][:, :],
                                in1=YA[:, st, half * 512:(half + 1) * 512], op=ALU.add)),
                                reads=[P[i], YAb[st]], writes=[YAb[st]])
                if sl == DFE // 512 - 1:
                    for s3 in range(nst_t):
                        st = ti * nst_t + s3
                        S.op("sp", (lambda e=e, st=st: nc.sync.dma_start(
                            out=ys_d[e * CAP + st * 128: e * CAP + (st + 1) * 128, :], in_=YA[:, st, :])),
                            reads=[YAb[st]], writes=[YSDb], dma_sem=yst[ti], waw=False)
                    for s3 in range(nst_t):
                        YAb[ti * nst_t + s3].r[yst[ti]] = S.cnt[yst[ti]]
        ffn_state["slab"] += len(all_slabs)

        B1l = B1[:].rearrange("p k n -> p (k n)").bitcast(F32)
        G0 = [B1l[:, r * 1024:(r + 1) * 1024] for r in range(2)]
        G1 = [B1l[:, 2048 + r * 1024: 2048 + (r + 1) * 1024] for r in range(2)]
        RR = [B1l[:, 4096 + r * 1024: 4096 + (r + 1) * 1024] for r in range(2)]
        GBC = B2f[:, 0:1024]
        OUTt = [B2f[:, 1024 + r * 1024: 1024 + (r + 1) * 1024] for r in range(2)]
        OUTtb = [Buf("outt0"), Buf("outt1")]
        G0b = [Buf("g00"), Buf("g01")]
        G1b = [Buf("g10"), Buf("g11")]
        RRb = [Buf("rr0"), Buf("rr1")]
        GBCb = Buf("gbc")
        gsm = [scs[0], scs[1]]
        osem = [S.new_sem("do0"), S.new_sem("do1")]
        sgb = sc
        SSQ = SCR[:, 0:16]
        RS2 = SCR[:, 16:32]
        SSb = Buf("ssq")
        S.barrier()
        S.op("sp", lambda: nc.sync.dma_start(out=GBC, in_=gbc_d), writes=[GBCb], dma_sem=sgb)
        for r in range(2):
            S.op("pool", (lambda r=r: nc.gpsimd.memset(G0[r], 0.0)), writes=[G0b[r]])
            S.op("pool", (lambda r=r: nc.gpsimd.memset(G1[r], 0.0)), writes=[G1b[r]])
        last = []
        for j in range(16):
            c0 = HALO + 128 * j
            r = j % 2
            S.op("pool", (lambda j=j, r=r: nc.gpsimd.indirect_dma_start(
                out=G0[r], out_offset=None, in_=ys_d, in_offset=bass.IndirectOffsetOnAxis(S0U[:, j:j + 1], 0),
                bounds_check=bnd["v"], oob_is_err=False)),
                reads=[YSDb, IDXb], writes=[G0b[r]], dma_sem=gsm[r])
            S.op("pool", (lambda j=j, r=r: nc.gpsimd.indirect_dma_start(
                out=G1[r], out_offset=None, in_=ys_d, in_offset=bass.IndirectOffsetOnAxis(S1U[:, j:j + 1], 0),
                bounds_check=bnd["v"], oob_is_err=False)),
                reads=[YSDb, IDXb], writes=[G1b[r]], dma_sem=gsm[r])
            G0b[r].w = dict(G1b[r].w)
            for half in range(2):
                i = state["bank"]
                state["bank"] = (i + 1) % 8
                for kk in range(4):
                    S.op("pe", (lambda i=i, kk=kk, half=half, c0=c0: nc.tensor.matmul(
                        ps[i][:, kk * 128:(kk + 1) * 128], lhsT=H[:, 4 * half + kk, c0:c0 + 128], rhs=identf[:],
                        start=True, stop=True)),
                        reads=Hb + [Bc] if kk in (0, 3) else (), writes=[P[i]] if kk in (0, 3) else (), inc=(kk == 3))
                S.op("dve", (lambda i=i, j=j, r=r, half=half: nc.vector.scalar_tensor_tensor(
                    out=RR[r][:, half * 512:(half + 1) * 512], in0=G0[r][:, half * 512:(half + 1) * 512],
                    scalar=RT[:, j, 12:13], in1=ps[i][:, :], op0=ALU.mult, op1=ALU.add)),
                    reads=[P[i], G0b[r]] + allrt, writes=[RRb[r]], waw=(half == 0))
            S.op("dve", (lambda j=j, r=r: nc.vector.scalar_tensor_tensor(
                out=RR[r], in0=G1[r], scalar=RT[:, j, 13:14], in1=RR[r], op0=ALU.mult, op1=ALU.add)),
                reads=[G1b[r], RRb[r]] + allrt, writes=[RRb[r]])
            S.op("act", (lambda j=j, r=r: nc.scalar.activation(
                out=OUTt[r], in_=RR[r], func=AF.Square, accum_out=SSQ[:, j:j + 1])),
                reads=[RRb[r]], writes=[OUTtb[r], SSb])
            S.op("act", (lambda j=j: nc.scalar.activation(
                out=RS2[:, j:j + 1], in_=SSQ[:, j:j + 1], func=AF.Sqrt, bias=eps_ap(RMS_EPS), scale=1.0 / D)),
                reads=[SSb, Bc], writes=[SSb])
            S.op("dve", (lambda j=j: nc.vector.reciprocal(out=RS2[:, j:j + 1], in_=RS2[:, j:j + 1])),
                 reads=[SSb], writes=[SSb])
            S.op("dve", (lambda j=j, r=r: nc.vector.scalar_tensor_tensor(
                out=OUTt[r], in0=RR[r], scalar=RS2[:, j:j + 1], in1=GBC, op0=ALU.mult, op1=ALU.mult)),
                reads=[RRb[r], SSb, GBCb], writes=[OUTtb[r]])
            tok = S.op("sp", (lambda j=j, r=r: nc.sync.dma_start(out=out_d[128 * j:128 * (j + 1), :], in_=OUTt[r])),
                       reads=[OUTtb[r]], dma_sem=osem[r])
            last.append(tok)
        S.op("sp", None, after=last, inc=False)
        print("sbuf bytes remaining:", nc.sbuf_bytes_remaining)
        S.run_block()
    return nc


_CACHE = {}


def _prep_inputs(x, meta_tokens, conv_w_pw1, conv_b_pw1, conv_w_dw, conv_b_dw, conv_ln_g, conv_ln_b,
                 conv_w_pw2, conv_b_pw2, pool_w_group, pool_scale, ffn_w_gate, ffn_w_up, ffn_w_down,
                 moe_w_router, moe_w_gate, moe_w_up, moe_w_down, mix_norm_g, ffn_norm_g, final_norm_g):
    f = lambda a: np.ascontiguousarray(np.asarray(a, dtype=np.float32))

    def pk(v):
        v = np.asarray(v, dtype=np.float32).reshape(-1, 128)
        return v.T

    cols = [pk(mix_norm_g[0]), pk(mix_norm_g[1]), pk(ffn_norm_g[0]), pk(ffn_norm_g[1]), pk(final_norm_g),
            pk(conv_b_pw1[0]), pk(conv_b_dw[0]), pk(conv_ln_g[0]), pk(conv_ln_b[0]), pk(conv_b_pw2[0]),
            pk(pool_scale[0])]
    wdw = np.asarray(conv_w_dw[0], dtype=np.float32)
    wdw = wdw.reshape(CONVW, KC, 128).transpose(2, 1, 0).reshape(128, KC * CONVW)
    cvec = f(np.concatenate(cols + [wdw], axis=1))
    assert cvec.shape == (128, NCV), cvec.shape
    wr = np.asarray(moe_w_router[0], dtype=np.float32).reshape(KC, 128, NE).transpose(1, 0, 2).reshape(128, KC * NE)
    xe = np.concatenate([np.zeros((32, D), np.float32), np.asarray(meta_tokens, np.float32),
                         np.asarray(x[0], np.float32)], axis=0)
    shared = {
        "cvec": cvec, "wr": f(wr), "ident": np.eye(128, dtype=np.float32),
        "tri": np.triu(np.ones((128, 128), np.float32), 1),
        "gbc": f(np.broadcast_to(np.asarray(final_norm_g, np.float32)[None, :], (128, D))),
        "w_pw1": f(conv_w_pw1[0]), "w_pw2": f(conv_w_pw2[0]), "pool_w": f(pool_w_group[0]),
        "ffn_wg": f(ffn_w_gate[0]), "ffn_wu": f(ffn_w_up[0]), "ffn_wd": f(ffn_w_down[0]),
        "moe_wg": f(moe_w_gate[0]), "moe_wu": f(moe_w_up[0]), "moe_wd": f(moe_w_down[0]),
    }
    in_maps = []
    for c in range(NCORES):
        m = dict(shared)
        m["xT"] = f(xe[TOK * c: TOK * c + TW].T)
        um = np.ones((128, HALO), np.float32)
        if c == 0:
            um[:, :32] = 0.0
        m["umask"] = um
        in_maps.append(m)
    return in_maps


def kernel(**inputs):
    if "nc" not in _CACHE:
        _CACHE["nc"] = build_program()
    nc = _CACHE["nc"]
    in_maps = _prep_inputs(**inputs)
    res = run_bass_kernel_spmd(nc, in_maps, core_ids=list(range(NCORES)))
    outs = [np.asarray(r["out"]) for r in res.results]
    out = np.concatenate(outs, axis=0).reshape(1, SEQ, D).astype(np.float32)
    return out
```

```python
import numpy as np
import concourse.bass as bass
import concourse.mybir as mybir
from concourse.bass_utils import run_bass_kernel_spmd
from contextlib import ExitStack

F32 = mybir.dt.float32
BF16 = mybir.dt.bfloat16
ALU = mybir.AluOpType
AF = mybir.ActivationFunctionType

NCORES = 8
D = 1024
KC = 8
SEQ = 16384
NMETA = 16
TOK = SEQ // NCORES
HALO = 48
TW = TOK + HALO
DFF = 2816
DFE = 3584
NE = 8
CONVW = 31
RMS_EPS = 1e-6
LN_EPS = 1e-5
POOLW = (2, 4, 8, 16)
CAP = 768
NST = CAP // 128
NSLOT = NE * CAP
U32 = mybir.dt.uint32

TT0 = [(0, 432), (432, 416), (848, 416), (1264, 416), (1680, 416)]
TT1 = [(32, 400)] + TT0[1:]
TT2 = [(48, 384)] + TT0[1:]
TT3 = [(48 + 512 * i, 512) for i in range(4)]

CV = {}
_o = 0
for _n, _w in (("mix_g0", 8), ("mix_g1", 8), ("ffn_g0", 8), ("ffn_g1", 8), ("fin_g", 8),
               ("b_pw1", 16), ("b_dw", 8), ("ln_g", 8), ("ln_b", 8), ("b_pw2", 8),
               ("pool_s", 8), ("w_dw", 8 * CONVW)):
    CV[_n] = _o
    _o += _w
NCV = _o


class Buf:
    __slots__ = ("name", "w", "r")

    def __init__(self, name):
        self.name = name
        self.w = {}
        self.r = {}


class Sched:
    ENGS = ("pe", "act", "dve", "pool", "sp")

    def __init__(self, nc, es):
        self.nc = nc
        self.es = es
        self.eng = {"pe": nc.tensor, "act": nc.scalar, "dve": nc.vector,
                    "pool": nc.gpsimd, "sp": nc.sync}
        self.stream = {e: [] for e in self.ENGS}
        self.sems = {}
        self.cnt = {}
        self.seen = {e: {} for e in self.ENGS}
        for e in self.ENGS:
            self.new_sem(e)

    def new_sem(self, name):
        self.sems[name] = self.es.enter_context(self.nc.semaphore("s_" + name))
        self.cnt[name] = 0
        return name

    def op(self, e, fn, reads=(), writes=(), dma_sem=None, inc=True, after=(), waw=True):
        d = {}

        def add(tok):
            if tok is None:
                return
            s, v = tok
            if d.get(s, 0) < v:
                d[s] = v
        for b in reads:
            for s, v in b.w.items():
                add((s, v))
        for b in writes:
            if waw:
                for s, v in b.w.items():
                    if not (dma_sem is not None and s == dma_sem):
                        add((s, v))
            for s, v in b.r.items():
                add((s, v))
        for tok in after:
            add(tok)
        waits = []
        seen = self.seen[e]
        for s, v in d.items():
            if e == "pe" and s == "pe":
                continue
            if seen.get(s, 0) < v:
                waits.append((s, v))
                seen[s] = v
        if not inc:
            self.stream[e].append((waits, fn, None, 0))
            return None
        if dma_sem is None:
            s, n = e, 1
        else:
            s, n = dma_sem, 16
        self.cnt[s] += n
        tok = (s, self.cnt[s])
        self.stream[e].append((waits, fn, s, n))
        for b in writes:
            if waw:
                b.w = {s: tok[1]}
                b.r = {}
            else:
                b.w[s] = tok[1]
        for b in reads:
            if b.r.get(s, 0) < tok[1]:
                b.r[s] = tok[1]
        return tok

    def barrier(self):
        snap = dict(self.cnt)
        for e in self.ENGS:
            waits = []
            for s, v in snap.items():
                if v == 0 or (e == "pe" and s == "pe"):
                    continue
                if self.seen[e].get(s, 0) < v:
                    waits.append((s, v))
                    self.seen[e][s] = v
            if waits:
                self.stream[e].append((waits, None, None, 0))

    def replay(self, e):
        eng = self.eng[e]
        for waits, fn, s, n in self.stream[e]:
            for ws, wv in waits:
                eng.wait_ge(self.sems[ws], wv)
            if fn is None:
                continue
            ins = fn()
            if s is not None:
                ins.then_inc(self.sems[s], n)

    def run_block(self):
        nc = self.nc
        with nc.Block() as block:
            @block.tensor
            def _(t):
                self.replay("pe")

            @block.scalar
            def _(t):
                self.replay("act")

            @block.vector
            def _(t):
                self.replay("dve")

            @block.gpsimd
            def _(t):
                self.replay("pool")

            @block.sync
            def _(t):
                self.replay("sp")


def build_program(debug=False):
    nc = bass.Bass("TRN2", target_bir_lowering=False)
    dbg_t = [nc.dram_tensor(f"dbg{i}", [D, TW], F32, kind="ExternalOutput").ap() for i in range(4)] if debug else []

    def din(name, shape):
        return nc.dram_tensor(name, list(shape), F32, kind="ExternalInput").ap()

    xT = din("xT", (D, TW))
    umask_d = din("umask", (128, HALO))
    cvec_d = din("cvec", (128, NCV))
    wr_d = din("wr", (128, KC * NE))
    ident_d = din("ident", (128, 128))
    w_pw1 = din("w_pw1", (D, 2 * D))
    w_pw2 = din("w_pw2", (D, D))
    pool_w = din("pool_w", (4, 256, 256))
    ffn_wg = din("ffn_wg", (D, DFF))
    ffn_wu = din("ffn_wu", (D, DFF))
    ffn_wd = din("ffn_wd", (DFF, D))
    moe_wg = din("moe_wg", (NE, D, DFE))
    moe_wu = din("moe_wu", (NE, D, DFE))
    moe_wd = din("moe_wd", (NE, DFE, D))
    tri_d = din("tri", (128, 128))
    gbc_d = din("gbc", (128, D))
    out_d = nc.dram_tensor("out", [TOK, D], F32, kind="ExternalOutput").ap()
    xs_d = nc.dram_tensor("xs_scratch", [NSLOT, D], BF16).ap()
    ys_d = nc.dram_tensor("ys_scratch", [NSLOT, D], F32).ap()

    with ExitStack() as es:
        S = Sched(nc, es)

        def sb(name, shape, dt):
            return es.enter_context(nc.sbuf_tensor("sb_" + name, list(shape), dt))

        H = sb("H", (128, KC, TW), F32)
        B1 = sb("B1", (128, KC, TW), BF16)
        B2 = sb("B2", (128, KC * TW), BF16)
        WP1 = [sb(f"wp1_{b}", (128, KC, 512), BF16) for b in range(2)]
        WP2 = [sb(f"wp2_{b}", (128, KC, 512), BF16) for b in range(2)]
        WP3 = [sb(f"wp3_{b}", (128, 4, 1024), BF16) for b in range(2)]
        R1 = sb("R1", (128, 4096), BF16)
        Z = sb("Z", (128, KC, 416), BF16)
        TMP = [sb(f"tmp{i}", (128, 512), F32) for i in range(5)]
        cvec = sb("cvec", (128, NCV), F32)
        wr = sb("wr", (128, KC, NE), F32)
        wrg = sb("wrg", (128, KC, NE), F32)
        identf = sb("identf", (128, 128), F32)
        identb = sb("identb", (128, 128), BF16)
        onesb = sb("onesb", (128, 128), BF16)
        onesf = sb("onesf", (128, 128), F32)
        umask = sb("umask", (128, HALO), F32)
        ps = [es.enter_context(nc.psum_tensor(f"ps{i}", [128, 512], F32)) for i in range(8)]

        Y = B1[:].bitcast(F32)
        U = B2[:].rearrange("p (k n) -> p k n", k=KC)
        B2f = B2[:].bitcast(F32)
        SQ = R1[:, 0:KC * 432].rearrange("p (k n) -> p k n", k=KC)
        Abuf = [R1[:, i * 2048:(i + 1) * 2048].rearrange("p (m n) -> p m n", m=4) for i in range(2)]
        CWe = [B2f[:, i * 2048:(i + 1) * 2048] for i in range(2)]
        RSall = B2f[:, 6144:8192]
        Zf = Z[:].rearrange("p k n -> p (k n)").bitcast(F32)
        RT = Zf[:, 0:640].rearrange("p (j c) -> p j c", j=16)
        Mf = Zf[:, 768:896].rearrange("p (j e) -> p j e", j=16)
        Mb = Zf[:, 896:960].bitcast(BF16)
        WITHIN = Zf[:, 960:1088]
        OFF = Zf[:, 1088:1216].rearrange("p (j e) -> p j e", j=16)
        SLV = Zf[:, 1216:1344].rearrange("p (j e) -> p j e", j=16)
        S0F = Zf[:, 1344:1360]
        S1F = Zf[:, 1360:1376]
        S0U = Zf[:, 1376:1392].bitcast(U32)
        S1U = Zf[:, 1392:1408].bitcast(U32)
        EC = Zf[:, 1440:1568].rearrange("p (j e) -> p j e", j=16)
        SCR = Zf[:, 1568:1664]
        trib = sb("trib", (128, 128), BF16)
        OUTb = [B2f[:, i * 3456:(i + 1) * 3456].rearrange("p (k n) -> p k n", k=KC) for i in range(2)]
        assert tuple(Y.shape) == (128, KC, TW // 2), Y.shape

        P = [Buf(f"ps{i}") for i in range(8)]
        state = {"bank": 0, "dq": 0, "r1": "sq"}

        def cv(name, j=0):
            c = CV[name] + j
            return cvec[:, c:c + 1]

        def mm_group(pairs, n, reads, rows=128):
            i = state["bank"]
            state["bank"] = (i + 1) % 8
            L = len(pairs)
            for j, (l, r) in enumerate(pairs):
                edge = (j == 0 or j == L - 1)
                S.op("pe",
                     (lambda l=l, r=r, j=j, i=i: nc.tensor.matmul(
                         ps[i][:rows, :n], lhsT=l, rhs=r, start=(j == 0), stop=(j == L - 1))),
                     reads=reads if edge else (), writes=[P[i]] if edge else (),
                     inc=(j == L - 1))
            return i

        Bc = Buf("consts")
        sc = S.new_sem("dconst")
        trif = TMP[3][:, 0:128]
        for dst, src in ((cvec[:], cvec_d), (wr[:].rearrange("p k e -> p (k e)"), wr_d),
                         (identf[:], ident_d), (umask[:], umask_d), (trif, tri_d)):
            S.op("sp", (lambda dst=dst, src=src: nc.sync.dma_start(out=dst, in_=src)),
                 writes=[Bc], dma_sem=sc)
        S.op("dve", lambda: nc.vector.memset(onesb[:], 1.0), writes=[Bc])
        S.op("dve", lambda: nc.vector.memset(onesf[:], 1.0), writes=[Bc])
        S.op("dve", lambda: nc.vector.tensor_copy(out=identb[:], in_=identf[:]), reads=[Bc], writes=[Bc])
        for k in range(KC):
            S.op("dve", (lambda k=k: nc.vector.tensor_scalar(
                out=wrg[:, k, :], in0=wr[:, k, :], scalar1=cv("ffn_g1", k), scalar2=None, op0=ALU.mult)),
                reads=[Bc], writes=[Bc])

        Hb = [Buf(f"H{t}") for t in range(5)]
        sx = S.new_sem("dx")
        dbg_sem = S.new_sem("ddbg") if debug else None

        def dump(i):
            if not debug:
                return
            S.barrier()
            for k in range(KC):
                S.op("sp", (lambda k=k, i=i: nc.sync.dma_start(out=dbg_t[i][k * 128:(k + 1) * 128, :], in_=H[:, k, :])),
                     reads=Hb, dma_sem=dbg_sem)
            S.barrier()
        xTv = xT.rearrange("(k p) n -> p k n", p=128)
        sxs = [sx] + [S.new_sem(f"dx{t}") for t in range(1, 5)]
        for t, (o, n) in enumerate(TT0):
            S.op("sp", (lambda o=o, n=n: nc.sync.dma_start(out=H[:, :, o:o + n], in_=xTv[:, :, o:o + n])),
                 writes=[Hb[t]], dma_sem=sxs[t])

        bnd = {}

        def _mk_bound():
            reg = nc.gpsimd.alloc_register("slot_bound")
            ins = nc.gpsimd.reg_mov(reg, NSLOT - 1)
            bnd["v"] = nc.gpsimd.snap(reg)
            return ins
        S.op("pool", _mk_bound, inc=False)

        XS0b = Buf("xs0")
        ZTb = Buf("zt")
        ZT = TMP[4][:, :].bitcast(BF16)
        szf = S.new_sem("dzf")
        S.op("dve", lambda: nc.vector.memset(TMP[4][:, :], 0.0), writes=[ZTb])
        zsem = [szf, S.new_sem("dzf1"), S.new_sem("dzf2")]
        nb = NSLOT // 128 // 8
        for bq in range(nb):
            zs = zsem[bq % 3]
            thr = []
            if bq >= 2:
                ps_ = zsem[(bq - 2) % 3]
                thr = [(ps_, S.cnt[ps_])]
            for q in range(8 * bq, 8 * bq + 8):
                S.op("sp", (lambda q=q: nc.sync.dma_start(out=xs_d[q * 128:(q + 1) * 128, :], in_=ZT)),
                     reads=[ZTb], writes=[XS0b], dma_sem=zs, after=thr, waw=False)
        S.op("dve", lambda: nc.vector.tensor_copy(out=trib[:], in_=trif[:]), reads=[Bc], writes=[Bc])

        WBb = [Buf("wb0"), Buf("wb1")]
        wsem = [S.new_sem("dw0"), S.new_sem("dw1")]

        def wload(b, parts):
            for dst, src in parts:
                S.op("pool", (lambda dst=dst, src=src: nc.gpsimd.dma_start(out=dst, in_=src)),
                     writes=[WBb[b]], dma_sem=wsem[b])

        def kview(ap):
            return ap.rearrange("(k p) n -> p k n", p=128)

        SQb, RSb = Buf("sq"), Buf("rs")

        def rmsnorm(tiles, tbufs_in, gname, out_fn, out_bufs, eps=RMS_EPS, rs_fn=None, rs_buf=None):
            if state["r1"] != "sq":
                S.barrier()
                state["r1"] = "sq"
            for ti, (o, n) in enumerate(tiles):
                hb = tbufs_in[ti]
                rsap = (lambda o=o, n=n: TMP[0][:, :n]) if rs_fn is None else (lambda o=o, n=n: rs_fn(o, n))
                rsb = RSb if rs_buf is None else rs_buf
                S.op("act", (lambda o=o, n=n: nc.scalar.activation(
                    out=SQ[:, :, :n], in_=H[:, :, o:o + n], func=AF.Square)),
                    reads=[hb], writes=[SQb])
                i = mm_group([(onesb[:], SQ[:, k, :n]) for k in range(KC)], n, [Bc, SQb])
                S.op("act", (lambda i=i, n=n, rsap=rsap: nc.scalar.activation(
                    out=rsap(), in_=ps[i][:, :n], func=AF.Sqrt, bias=eps_ap(eps), scale=1.0 / D)),
                    reads=[P[i], Bc], writes=[rsb])
                S.op("dve", (lambda rsap=rsap: nc.vector.reciprocal(out=rsap(), in_=rsap())),
                     reads=[rsb], writes=[rsb])
                for k in range(KC):
                    S.op("dve", (lambda k=k, o=o, n=n, rsap=rsap: nc.vector.scalar_tensor_tensor(
                        out=out_fn(k, o, n), in0=H[:, k, o:o + n], scalar=cv(gname, k),
                        in1=rsap(), op0=ALU.mult, op1=ALU.mult)),
                        reads=[hb, rsb, Bc], writes=[out_bufs[ti]])

        epsc = sb("epsc", (128, 2), F32)
        S.op("dve", lambda: nc.vector.memset(epsc[:, 0:1], RMS_EPS), writes=[Bc])
        S.op("dve", lambda: nc.vector.memset(epsc[:, 1:2], LN_EPS), writes=[Bc])

        def eps_ap(eps):
            return epsc[:, 0:1] if eps == RMS_EPS else epsc[:, 1:2]

        HNb = [Buf(f"HN{t}") for t in range(5)]
        for s in range(2):
            wload(s, [(WP1[s][:], kview(w_pw1[:, 512 * s:512 * s + 512])),
                      (WP2[s][:], kview(w_pw1[:, D + 512 * s:D + 512 * s + 512]))])
        rmsnorm(TT0, Hb, "mix_g0", lambda k, o, n: B1[:, k, o:o + n], HNb)

        Ub = Buf("U")
        SIGb = [Buf("sig0"), Buf("sig1")]
        sgi = 0
        for s in range(2):
            for ti, (o, n) in enumerate(TT0):
                for mc in range(4):
                    ch = 4 * s + mc
                    ia = mm_group([(WP1[s][:, k, mc * 128:(mc + 1) * 128], B1[:, k, o:o + n]) for k in range(KC)],
                                  n, [WBb[s], HNb[ti]])
                    ig = mm_group([(WP2[s][:, k, mc * 128:(mc + 1) * 128], B1[:, k, o:o + n]) for k in range(KC)],
                                  n, [WBb[s], HNb[ti]])
                    sg = sgi % 2
                    sgi += 1
                    S.op("act", (lambda ig=ig, n=n, ch=ch, sg=sg: nc.scalar.activation(
                        out=TMP[1 + sg][:, :n], in_=ps[ig][:, :n], func=AF.Sigmoid, bias=cv("b_pw1", 8 + ch))),
                        reads=[P[ig], Bc], writes=[SIGb[sg]])
                    S.op("dve", (lambda ia=ia, n=n, ch=ch, sg=sg, o=o: nc.vector.scalar_tensor_tensor(
                        out=U[:, ch, o:o + n], in0=ps[ia][:, :n], scalar=cv("b_pw1", ch),
                        in1=TMP[1 + sg][:, :n], op0=ALU.add, op1=ALU.mult)),
                        reads=[P[ia], SIGb[sg], Bc], writes=[Ub])
        for k in range(KC):
            S.op("dve", (lambda k=k: nc.vector.tensor_tensor(
                out=U[:, k, 0:HALO], in0=U[:, k, 0:HALO], in1=umask[:], op=ALU.mult)),
                reads=[Ub, Bc], writes=[Ub])

        S.barrier()

        W2b = [Buf("w2_0"), Buf("w2_1")]
        w2sem = [S.new_sem("dw2_0"), S.new_sem("dw2_1")]
        for s in range(2):
            S.op("pool", (lambda s=s: nc.gpsimd.dma_start(
                out=WP1[s][:], in_=kview(w_pw2[:, 512 * s:512 * s + 512]))),
                writes=[W2b[s]], dma_sem=w2sem[s])

        DGb = [Buf("dg0"), Buf("dg1")]
        Yb = Buf("Y")
        Zb = Buf("Z")
        MUb, M2b = Buf("mu"), ZTb
        groups = [[0, 1], [2, 3], [4]]
        dgi = 0
        for grp in groups:
            gbase = TT1[grp[0]][0]
            for c in range(KC):
                db = dgi % 2
                dgi += 1
                dg = WP3[db][:].rearrange("p a b -> p (a b)")
                for k in range(CONVW):
                    wcol = cvec[:, CV["w_dw"] + c * CONVW + k: CV["w_dw"] + c * CONVW + k + 1]
                    if k % 3 == 0:
                        S.op("pool", (lambda k=k, dg=dg, wcol=wcol: nc.gpsimd.tensor_scalar(
                            out=dg[:, k * 128:(k + 1) * 128], in0=identf[:], scalar1=wcol,
                            scalar2=0.0, op0=ALU.mult, op1=ALU.add)),
                            reads=[Bc], writes=[DGb[db]], waw=False)
                    else:
                        S.op("dve", (lambda k=k, dg=dg, wcol=wcol: nc.vector.tensor_scalar(
                            out=dg[:, k * 128:(k + 1) * 128], in0=identf[:], scalar1=wcol,
                            scalar2=None, op0=ALU.mult)),
                            reads=[Bc], writes=[DGb[db]], waw=False)
                for ti in grp:
                    o, n = TT1[ti]
                    i = mm_group([(dg[:, k * 128:(k + 1) * 128], U[:, c, o - 30 + k: o - 30 + k + n])
                                  for k in range(CONVW)], n, [DGb[db], Ub])
                    S.op("act", (lambda i=i, n=n, c=c, yo=o - gbase: nc.scalar.activation(
                        out=Y[:, c, yo:yo + n], in_=ps[i][:, :n], func=AF.Identity, bias=cv("b_dw", c))),
                        reads=[P[i], Bc], writes=[Yb])
            for ti in grp:
                o, n = TT1[ti]
                yo = o - gbase
                imu = mm_group([(onesf[:], Y[:, k, yo:yo + n]) for k in range(KC)], n, [Bc, Yb])
                S.op("act", (lambda yo=yo, n=n: nc.scalar.activation(
                    out=SQ[:, :, :n], in_=Y[:, :, yo:yo + n], func=AF.Square)),
                    reads=[Yb], writes=[SQb])
                isq = mm_group([(onesb[:], SQ[:, k, :n]) for k in range(KC)], n, [Bc, SQb])
                S.op("dve", (lambda imu=imu, n=n: nc.vector.tensor_scalar(
                    out=TMP[3][:, :n], in0=ps[imu][:, :n], scalar1=1.0 / D, scalar2=None, op0=ALU.mult)),
                    reads=[P[imu]], writes=[MUb])
                S.op("dve", (lambda n=n: nc.vector.tensor_tensor(
                    out=TMP[4][:, :n], in0=TMP[3][:, :n], in1=TMP[3][:, :n], op=ALU.mult)),
                    reads=[MUb], writes=[M2b])
                S.op("dve", (lambda isq=isq, n=n: nc.vector.scalar_tensor_tensor(
                    out=TMP[4][:, :n], in0=ps[isq][:, :n], scalar=1.0 / D, in1=TMP[4][:, :n],
                    op0=ALU.mult, op1=ALU.subtract)),
                    reads=[P[isq], M2b], writes=[M2b])
                S.op("act", (lambda n=n: nc.scalar.activation(
                    out=TMP[0][:, :n], in_=TMP[4][:, :n], func=AF.Sqrt, bias=eps_ap(LN_EPS), scale=1.0)),
                    reads=[M2b, Bc], writes=[RSb])
                S.op("dve", (lambda n=n: nc.vector.reciprocal(out=TMP[0][:, :n], in_=TMP[0][:, :n])),
                     reads=[RSb], writes=[RSb])
                for c in range(KC):
                    S.op("dve", (lambda c=c, yo=yo, n=n: nc.vector.tensor_tensor(
                        out=Y[:, c, yo:yo + n], in0=Y[:, c, yo:yo + n], in1=TMP[3][:, :n], op=ALU.subtract)),
                        reads=[Yb, MUb], writes=[Yb])
                    S.op("dve", (lambda c=c, yo=yo, n=n: nc.vector.tensor_tensor(
                        out=Y[:, c, yo:yo + n], in0=Y[:, c, yo:yo + n], in1=TMP[0][:, :n], op=ALU.mult)),
                        reads=[Yb, RSb], writes=[Yb])
                    S.op("act", (lambda c=c, yo=yo, n=n: nc.scalar.activation(
                        out=Z[:, c, :n], in_=Y[:, c, yo:yo + n], func=AF.Silu,
                        bias=cv("ln_b", c), scale=cv("ln_g", c))),
                        reads=[Yb, Bc], writes=[Zb])
                for oc in range(KC):
                    s2, mc = divmod(oc, 4)
                    i = mm_group([(WP1[s2][:, k, mc * 128:(mc + 1) * 128], Z[:, k, :n]) for k in range(KC)],
                                 n, [W2b[s2], Zb])
                    S.op("dve", (lambda i=i, oc=oc, o=o, n=n: nc.vector.scalar_tensor_tensor(
                        out=H[:, oc, o:o + n], in0=ps[i][:, :n], scalar=cv("b_pw2", oc),
                        in1=H[:, oc, o:o + n], op0=ALU.add, op1=ALU.add)),
                        reads=[P[i], Bc, Hb[ti]], writes=[Hb[ti]])

        S.barrier()
        dump(0)

        SGb = [Buf("sg0"), Buf("sg1")]
        T2b = [Buf("t2a"), Buf("t2b")]
        Ab = [Buf("A0"), Buf("A1")]
        ffn_state = {"sg": 0, "a": 0, "slab": 0}

        def ffn(slabs, tiles, hnb, hb, cw=None):
            st = ffn_state
            if state["r1"] != "a":
                S.barrier()
                state["r1"] = "a"
            b0 = st["slab"] % 2
            wload(b0, slabs[0][1])
            for si, (wdt, _) in enumerate(slabs):
                b = (st["slab"] + si) % 2
                if si + 1 < len(slabs):
                    wload(1 - b, slabs[si + 1][1])
                nm = wdt // 128
                tile_a = {}

                def emit_gu(ti):
                    o, n = tiles[ti]
                    a = st["a"] % 2
                    st["a"] += 1
                    tile_a[ti] = a
                    for mc in range(nm):
                        ig = mm_group([(WP1[b][:, k, mc * 128:(mc + 1) * 128], B1[:, k, o:o + n]) for k in range(KC)],
                                      n, [WBb[b], hnb[ti]])
                        iu = mm_group([(WP2[b][:, k, mc * 128:(mc + 1) * 128], B1[:, k, o:o + n]) for k in range(KC)],
                                      n, [WBb[b], hnb[ti]])
                        sg = st["sg"] % 2
                        st["sg"] += 1
                        S.op("act", (lambda ig=ig, n=n, sg=sg: nc.scalar.activation(
                            out=TMP[1 + sg][:, :n], in_=ps[ig][:, :n], func=AF.Silu)),
                            reads=[P[ig]], writes=[SGb[sg]])
                        if cw is None:
                            S.op("dve", (lambda iu=iu, n=n, sg=sg, a=a, mc=mc: nc.vector.tensor_tensor(
                                out=Abuf[a][:, mc, :n], in0=ps[iu][:, :n], in1=TMP[1 + sg][:, :n], op=ALU.mult)),
                                reads=[P[iu], SGb[sg]], writes=[Ab[a]])
                        else:
                            cwfn, cwb = cw
                            S.op("dve", (lambda n=n, sg=sg, o=o: nc.vector.tensor_tensor(
                                out=TMP[3 + sg][:, :n], in0=TMP[1 + sg][:, :n], in1=cwfn(o, n), op=ALU.mult)),
                                reads=[SGb[sg], cwb], writes=[T2b[sg]])
                            S.op("dve", (lambda iu=iu, n=n, sg=sg, a=a, mc=mc: nc.vector.tensor_tensor(
                                out=Abuf[a][:, mc, :n], in0=ps[iu][:, :n], in1=TMP[3 + sg][:, :n], op=ALU.mult)),
                                reads=[P[iu], T2b[sg]], writes=[Ab[a]])

                def emit_dn(ti):
                    o, n = tiles[ti]
                    a = tile_a[ti]
                    for oc in range(KC):
                        i = mm_group([(WP3[b][:, mc, oc * 128:(oc + 1) * 128], Abuf[a][:, mc, :n]) for mc in range(nm)],
                                     n, [WBb[b], Ab[a]])
                        S.op("dve", (lambda i=i, oc=oc, o=o, n=n: nc.vector.tensor_tensor(
                            out=H[:, oc, o:o + n], in0=ps[i][:, :n], in1=H[:, oc, o:o + n], op=ALU.add)),
                            reads=[P[i], hb[ti]], writes=[hb[ti]])

                emit_gu(0)
                for ti in range(len(tiles)):
                    if ti + 1 < len(tiles):
                        emit_gu(ti + 1)
                    emit_dn(ti)
            st["slab"] += len(slabs)

        def ffn_slabs(wg, wu, wd, dff):
            out = []
            off = 0
            while off < dff:
                wdt = min(512, dff - off)
                nm = wdt // 128
                out.append((wdt, off))
                off += wdt
            res = []
            for wdt, off in out:
                nm = wdt // 128

                def mk(b, wdt=wdt, off=off, nm=nm):
                    return [(WP1[b][:, :, :wdt], kview(wg[:, off:off + wdt])),
                            (WP2[b][:, :, :wdt], kview(wu[:, off:off + wdt])),
                            (WP3[b][:, :nm, :], wd[off:off + wdt, :].rearrange("(m p) n -> p m n", p=128))]
                res.append((wdt, mk))
            return res

        def run_ffn(wg, wu, wd, dff, tiles, hnb, hb, cw=None):
            sl = ffn_slabs(wg, wu, wd, dff)
            base = ffn_state["slab"]
            slabs = [(wdt, mk((base + si) % 2)) for si, (wdt, mk) in enumerate(sl)]
            ffn(slabs, tiles, hnb, hb, cw)

        rmsnorm(TT1, Hb, "ffn_g0", lambda k, o, n: B1[:, k, o:o + n], HNb)
        run_ffn(ffn_wg, ffn_wu, ffn_wd, DFF, TT1, HNb, Hb)

        dump(1)
        spw = S.new_sem("dpw")
        PW = WP3[0][:].rearrange("p a b -> p (a b)")[:, 0:2048].rearrange("p (g k n) -> p g k n", g=4, k=2)
        for g in range(4):
            S.op("pool", (lambda g=g: nc.gpsimd.dma_start(
                out=PW[:, g, :, :], in_=pool_w[g].rearrange("(k p) n -> p k n", p=128))),
                writes=[WBb[0]], dma_sem=spw)
        rmsnorm(TT1, Hb, "mix_g1", lambda k, o, n: B1[:, k, o:o + n], HNb)
        W1f = WP1[0][:].rearrange("p a b -> p (a b)")
        PWS = W1f[:, 0:2048].rearrange("p (g k n) -> p g k n", g=4, k=2)
        PWN = W1f[:, 2048:4096].rearrange("p (g k n) -> p g k n", g=4, k=2)
        for g in range(4):
            S.op("dve", (lambda g=g: nc.vector.tensor_scalar(
                out=PWS[:, g, :, :], in0=PW[:, g, :, :], scalar1=1.0 / POOLW[g], scalar2=None, op0=ALU.mult)),
                reads=[WBb[0]], writes=[WBb[0]])
        for g in range(4):
            S.op("dve", (lambda g=g: nc.vector.tensor_scalar(
                out=PWN[:, g, :, :], in0=PW[:, g, :, :], scalar1=1.0 / POOLW[g] - 1.0, scalar2=None, op0=ALU.mult)),
                reads=[WBb[0]], writes=[WBb[0]])
        for ti, (o, n) in enumerate(TT2):
            rd = [HNb[ti]] + ([HNb[ti - 1]] if ti > 0 else [])
            for g in range(4):
                w = POOLW[g]
                for oc in range(2):
                    ch = 2 * g + oc
                    pairs = []
                    for kc in range(2):
                        pairs.append((PWN[:, g, kc, oc * 128:(oc + 1) * 128], B1[:, 2 * g + kc, o:o + n]))
                        for dd in range(1, w):
                            pairs.append((PWS[:, g, kc, oc * 128:(oc + 1) * 128], B1[:, 2 * g + kc, o - dd:o - dd + n]))
                    i = mm_group(pairs, n, [WBb[0]] + rd)
                    S.op("dve", (lambda i=i, ch=ch, o=o, n=n: nc.vector.scalar_tensor_tensor(
                        out=H[:, ch, o:o + n], in0=ps[i][:, :n], scalar=cv("pool_s", ch), in1=H[:, ch, o:o + n],
                        op0=ALU.mult, op1=ALU.add)),
                        reads=[P[i], Hb[ti], Bc], writes=[Hb[ti]])

        dump(2)
        RSAb = Buf("rsall")
        rmsnorm(TT2, Hb, "ffn_g1", lambda k, o, n: B1[:, k, o:o + n], HNb,
                rs_fn=lambda o, n: RSall[:, o - HALO:o - HALO + n], rs_buf=RSAb)
        RTb = [Buf("rt")] * 16
        rb = RTb[0]
        ir = state["bank"]
        state["bank"] = (ir + 1) % 8
        for j in range(16):
            c0 = HALO + 128 * j
            for k in range(KC):
                first = (j == 0 and k == 0)
                S.op("pe", (lambda j=j, k=k, c0=c0: nc.tensor.matmul(
                    ps[ir][:, 8 * j:8 * j + 8], lhsT=H[:, k, c0:c0 + 128], rhs=wrg[:, k, :],
                    start=(k == 0), stop=(k == KC - 1))),
                    reads=Hb + [Bc] if first else (), writes=[P[ir]] if first else (), inc=False)
            last = (j == 15)
            S.op("pe", (lambda j=j: nc.tensor.matmul(
                ps[ir][:, 128 + 2 * j:128 + 2 * j + 2], lhsT=RSall[:, 128 * j:128 * j + 128], rhs=identf[:, 0:2],
                start=True, stop=True)),
                reads=[RSAb, Bc] + Hb if (j == 0 or last) else (), writes=[P[ir]] if last else (), inc=last)
        RAW = RT[:, :, 0:8]
        S.op("dve", lambda: nc.vector.tensor_copy(
            out=RAW, in_=ps[ir][:, 0:128].rearrange("p (j e) -> p j e", j=16)), reads=[P[ir]], writes=[rb])
        S.op("dve", lambda: nc.vector.tensor_copy(
            out=RT[:, :, 8], in_=ps[ir][:, 128:160].rearrange("p (j c) -> p j c", j=16)[:, :, 0]),
            reads=[P[ir]], writes=[rb])
        S.op("dve", lambda: nc.vector.tensor_reduce(out=RT[:, :, 9], in_=RAW, axis=mybir.AxisListType.X, op=ALU.max),
             reads=[rb], writes=[rb])
        def first_max_mask(src0, mcol, dst0):
            S.op("dve", lambda: nc.vector.memset(RT[:, :, 14], 0.0), reads=[rb], writes=[rb])
            for e in range(NE):
                S.op("dve", (lambda e=e: nc.vector.tensor_tensor(
                    out=RT[:, :, dst0 + e], in0=RT[:, :, src0 + e], in1=RT[:, :, mcol], op=ALU.is_equal)),
                    reads=[rb], writes=[rb])
                S.op("dve", (lambda e=e: nc.vector.scalar_tensor_tensor(
                    out=RT[:, :, dst0 + e], in0=RT[:, :, 14], scalar=1.0, in1=RT[:, :, dst0 + e],
                    op0=ALU.subtract, op1=ALU.mult)), reads=[rb], writes=[rb])
                S.op("dve", (lambda e=e: nc.vector.tensor_tensor(
                    out=RT[:, :, 14], in0=RT[:, :, 14], in1=RT[:, :, dst0 + e], op=ALU.subtract)),
                    reads=[rb], writes=[rb])
            S.op("dve", lambda: nc.vector.tensor_scalar(
                out=RT[:, :, dst0:dst0 + NE], in0=RT[:, :, dst0:dst0 + NE], scalar1=-1.0, scalar2=None, op0=ALU.mult),
                reads=[rb], writes=[rb])
        first_max_mask(0, 9, 16)
        S.op("dve", lambda: nc.vector.scalar_tensor_tensor(
            out=RT[:, :, 24:32], in0=RT[:, :, 16:24], scalar=-1.0e30, in1=RAW, op0=ALU.mult, op1=ALU.add),
            reads=[rb], writes=[rb])
        S.op("dve", lambda: nc.vector.tensor_reduce(out=RT[:, :, 10], in_=RT[:, :, 24:32], axis=mybir.AxisListType.X,
                                                    op=ALU.max), reads=[rb], writes=[rb])
        first_max_mask(24, 10, 32)
        S.op("dve", lambda: nc.vector.tensor_tensor(out=RT[:, :, 11], in0=RT[:, :, 9], in1=RT[:, :, 10], op=ALU.subtract),
             reads=[rb], writes=[rb])
        S.op("dve", lambda: nc.vector.tensor_tensor(out=RT[:, :, 11], in0=RT[:, :, 11], in1=RT[:, :, 8], op=ALU.mult),
             reads=[rb], writes=[rb])
        S.op("act", lambda: nc.scalar.activation(out=RT[:, :, 12], in_=RT[:, :, 11], func=AF.Sigmoid),
             reads=[rb], writes=[rb])
        S.op("dve", lambda: nc.vector.tensor_scalar(out=RT[:, :, 13], in0=RT[:, :, 12], scalar1=-1.0, scalar2=1.0,
                                                    op0=ALU.mult, op1=ALU.add), reads=[rb], writes=[rb])

        IDXb = Buf("idx")
        allrt = RTb
        S.op("dve", lambda: nc.vector.tensor_tensor(out=Mf[:], in0=RT[:, :, 16:24], in1=RT[:, :, 32:40], op=ALU.add),
             reads=allrt, writes=[IDXb])
        S.op("dve", lambda: nc.vector.tensor_copy(out=Mb, in_=Mf[:].rearrange("p j e -> p (j e)")),
             reads=[IDXb], writes=[IDXb])
        iw = mm_group([(trib[:], Mb)], 128, [Bc, IDXb])
        ic = mm_group([(onesb[:], Mb)], 128, [Bc, IDXb])
        S.op("dve", lambda: nc.vector.memset(OFF[:, 0, :], 0.0), reads=[IDXb], writes=[IDXb])
        for j in range(1, 16):
            S.op("dve", (lambda j=j: nc.vector.tensor_tensor(
                out=OFF[:, j, :], in0=OFF[:, j - 1, :], in1=ps[ic][:, (j - 1) * NE:j * NE], op=ALU.add)),
                reads=[IDXb, P[ic]], writes=[IDXb])
        for e in range(NE):
            S.op("dve", (lambda e=e: nc.vector.memset(EC[:, :, e:e + 1], float(e * CAP))), reads=[IDXb], writes=[IDXb])
        Offl = OFF.rearrange("p j e -> p (j e)")
        SLVl = SLV.rearrange("p j e -> p (j e)")
        ECl = EC.rearrange("p j e -> p (j e)")
        S.op("dve", lambda: nc.vector.tensor_tensor(out=Offl, in0=Offl, in1=ps[iw][:, 0:128], op=ALU.add),
             reads=[IDXb, P[iw]], writes=[IDXb])
        S.op("dve", lambda: nc.vector.tensor_tensor(out=SLVl, in0=Offl, in1=ECl, op=ALU.add), reads=[IDXb], writes=[IDXb])
        S.op("dve", lambda: nc.vector.tensor_scalar(out=Offl, in0=Offl, scalar1=float(CAP), scalar2=1.0e6,
                                                    op0=ALU.is_ge, op1=ALU.mult), reads=[IDXb], writes=[IDXb])
        S.op("dve", lambda: nc.vector.tensor_tensor(out=SLVl, in0=SLVl, in1=Offl, op=ALU.add), reads=[IDXb], writes=[IDXb])
        for (msk, SF, SU, wc) in ((RT[:, :, 16:24], S0F, S0U, 12), (RT[:, :, 32:40], S1F, S1U, 13)):
            S.op("dve", (lambda msk=msk: nc.vector.tensor_tensor(out=Mf[:], in0=msk, in1=SLV, op=ALU.mult)),
                 reads=[IDXb] + allrt, writes=[IDXb])
            S.op("dve", (lambda SF=SF: nc.vector.tensor_reduce(out=SF, in_=Mf[:], axis=mybir.AxisListType.X, op=ALU.add)),
                 reads=[IDXb], writes=[IDXb])
            S.op("dve", (lambda SF=SF, SU=SU: nc.vector.tensor_copy(out=SU, in_=SF)), reads=[IDXb], writes=[IDXb])
            S.op("dve", (lambda SF=SF: nc.vector.tensor_scalar(out=SF, in0=SF, scalar1=float(NSLOT), scalar2=None,
                                                               op0=ALU.is_lt)), reads=[IDXb], writes=[IDXb])
            S.op("dve", (lambda SF=SF, wc=wc: nc.vector.tensor_tensor(out=RT[:, :, wc], in0=RT[:, :, wc], in1=SF, op=ALU.mult)),
                 reads=[IDXb] + allrt, writes=allrt)

        all_slabs = [(e, sl) for e in range(NE) for sl in range(DFE // 512)]

        def slab_parts(e, sl, b):
            off = 512 * sl
            return [(WP1[b][:], kview(moe_wg[e][:, off:off + 512])),
                    (WP2[b][:], kview(moe_wu[e][:, off:off + 512])),
                    (WP3[b][:], moe_wd[e][off:off + 512, :].rearrange("(m p) n -> p m n", p=128))]
        sbase = ffn_state["slab"]
        wload(sbase % 2, slab_parts(0, 0, sbase % 2))

        HT = [B2[:, r * 1024:(r + 1) * 1024] for r in range(4)]
        HTb = [Buf(f"ht{r}") for r in range(4)]
        scs = [S.new_sem(f"dsc{r}") for r in range(4)]
        XSb = Buf("xs")
        XSb.w = dict(XS0b.w)
        prev_sc = []
        for j in range(16):
            c0 = HALO + 128 * j
            r = j % 4
            for half in range(2):
                i = state["bank"]
                state["bank"] = (i + 1) % 8
                for kk in range(4):
                    S.op("pe", (lambda i=i, kk=kk, half=half, c0=c0: nc.tensor.matmul(
                        ps[i][:, kk * 128:(kk + 1) * 128], lhsT=B1[:, 4 * half + kk, c0:c0 + 128], rhs=identb[:],
                        start=True, stop=True)),
                        reads=HNb + [Bc] if kk in (0, 3) else (), writes=[P[i]] if kk in (0, 3) else (), inc=(kk == 3))
                if half == 0:
                    S.op("act", (lambda i=i, r=r: nc.scalar.activation(out=HT[r][:, 0:512], in_=ps[i][:, :], func=AF.Identity)),
                         reads=[P[i]], writes=[HTb[r]], waw=False)
                else:
                    S.op("dve", (lambda i=i, r=r: nc.vector.tensor_copy(out=HT[r][:, 512:1024], in_=ps[i][:, :])),
                         reads=[P[i]], writes=[HTb[r]], waw=False)
            for SU in (S0U, S1U):
                S.op("pool", (lambda SU=SU, j=j, r=r: nc.gpsimd.indirect_dma_start(
                    out=xs_d, out_offset=bass.IndirectOffsetOnAxis(SU[:, j:j + 1], 0), in_=HT[r], in_offset=None,
                    bounds_check=bnd["v"], oob_is_err=False)),
                    reads=[HTb[r], IDXb], writes=[XSb], dma_sem=scs[r], waw=False, after=list(prev_sc))
                prev_sc[:] = [(scs[r], S.cnt[scs[r]])]

        if state["r1"] != "a":
            S.barrier()
            state["r1"] = "a"
        XT = [B1[:].rearrange("p k n -> p (k n)")[:, 12288 + r * 1024: 12288 + (r + 1) * 1024] for r in range(3)]
        XTb = [Buf(f"xt{r}") for r in range(3)]
        xts = [S.new_sem(f"dxt{r}") for r in range(3)]
        XG = [B1[:].rearrange("p k n -> p (k n)")[:, g * 6144:(g + 1) * 6144].rearrange("p (k n) -> p k n", k=KC)
              for g in range(2)]
        XGb = [Buf("xg0"), Buf("xg1")]
        YA = B2f[:, 0:NST * 1024].rearrange("p (s n) -> p s n", s=NST)
        YAb = [Buf(f"ya{st}") for st in range(NST)]
        YSDb = Buf("ysd")
        yst = [S.new_sem("dys0"), S.new_sem("dys1")]
        xti = 0
        evi = 0
        gtiles = [(0, CAP // 2), (CAP // 2, CAP // 2)]
        nst_t = (CAP // 2) // 128
        def emit_xload(e):
            nonlocal xti, evi
            g = e % 2
            for st in range(NST):
                xr = xti % 3
                xti += 1
                S.op("sp", (lambda e=e, st=st, xr=xr: nc.sync.dma_start(
                    out=XT[xr], in_=xs_d[e * CAP + st * 128: e * CAP + (st + 1) * 128, :])),
                    reads=[XSb], writes=[XTb[xr]], dma_sem=xts[xr])
                for half in range(2):
                    i = state["bank"]
                    state["bank"] = (i + 1) % 8
                    for kk in range(4):
                        S.op("pe", (lambda i=i, kk=kk, half=half, xr=xr: nc.tensor.matmul(
                            ps[i][:, kk * 128:(kk + 1) * 128],
                            lhsT=XT[xr][:, (4 * half + kk) * 128:(4 * half + kk + 1) * 128], rhs=identb[:],
                            start=True, stop=True)),
                            reads=[XTb[xr], Bc] if kk in (0, 3) else (), writes=[P[i]] if kk in (0, 3) else (),
                            inc=(kk == 3))
                    dst = XG[g][:, 4 * half:4 * half + 4, st * 128:(st + 1) * 128]
                    srcv = ps[i][:, :].rearrange("p (a b) -> p a b", a=4)
                    evi += 1
                    if evi % 2:
                        S.op("act", (lambda dst=dst, srcv=srcv: nc.scalar.activation(out=dst, in_=srcv, func=AF.Identity)),
                             reads=[P[i]], writes=[XGb[g]], waw=False)
                    else:
                        S.op("dve", (lambda dst=dst, srcv=srcv: nc.vector.tensor_copy(out=dst, in_=srcv)),
                             reads=[P[i]], writes=[XGb[g]], waw=False)
        emit_xload(0)
        for si, (e, sl) in enumerate(all_slabs):
            b = (sbase + si) % 2
            if si + 1 < len(all_slabs):
                wload(1 - b, slab_parts(all_slabs[si + 1][0], all_slabs[si + 1][1], 1 - b))
            g = e % 2
            if sl == 2 and e + 1 < NE:
                emit_xload(e + 1)
            tile_a = {}
            for ti, (o, n) in enumerate(gtiles):
                a = ffn_state["a"] % 2
                ffn_state["a"] += 1
                tile_a[ti] = a
                for mc in range(4):
                    ig = mm_group([(WP1[b][:, k, mc * 128:(mc + 1) * 128], XG[g][:, k, o:o + n]) for k in range(KC)],
                                  n, [WBb[b], XGb[g]])
                    iu = mm_group([(WP2[b][:, k, mc * 128:(mc + 1) * 128], XG[g][:, k, o:o + n]) for k in range(KC)],
                                  n, [WBb[b], XGb[g]])
                    sg = ffn_state["sg"] % 2
                    ffn_state["sg"] += 1
                    S.op("act", (lambda ig=ig, n=n, sg=sg: nc.scalar.activation(
                        out=TMP[1 + sg][:, :n], in_=ps[ig][:, :n], func=AF.Silu)),
                        reads=[P[ig]], writes=[SGb[sg]])
                    S.op("dve", (lambda iu=iu, n=n, sg=sg, a=a, mc=mc: nc.vector.tensor_tensor(
                        out=Abuf[a][:, mc, :n], in0=ps[iu][:, :n], in1=TMP[1 + sg][:, :n], op=ALU.mult)),
                        reads=[P[iu], SGb[sg]], writes=[Ab[a]])
            for ti, (o, n) in enumerate(gtiles):
                a = tile_a[ti]
                for s3 in range(nst_t):
                    st = ti * nst_t + s3
                    for half in range(2):
                        i = mm_group([(Abuf[a][:, mc, s3 * 128:(s3 + 1) * 128], WP3[b][:, mc, half * 512:(half + 1) * 512])
                                      for mc in range(4)], 512, [WBb[b], Ab[a]])
                        if sl == 0:
                            S.op("dve", (lambda i=i, st=st, half=half: nc.vector.tensor_copy(
                                out=YA[:, st, half * 512:(half + 1) * 512], in_=ps[i][:, :])),
                                reads=[P[i]], writes=[YAb[st]], waw=(half == 0))
                        else:
                            S.op("dve", (lambda i=i, st=st, half=half: nc.vector.tensor_tensor(
                                out=YA[:, st, half * 512:(half + 1) * 512], in0=ps[i][:, :],
                                in1=YA[:, st, half * 512:(half + 1) * 512], op=ALU.add)),
                                reads=[P[i], YAb[st]], writes=[YAb[st]])
                if sl == DFE // 512 - 1:
                    for s3 in range(nst_t):
                        st = ti * nst_t + s3
                        S.op("sp", (lambda e=e, st=st: nc.sync.dma_start(
                            out=ys_d[e * CAP + st * 128: e * CAP + (st + 1) * 128, :], in_=YA[:, st, :])),
                            reads=[YAb[st]], writes=[YSDb], dma_sem=yst[ti], waw=False)
                    for s3 in range(nst_t):
                        YAb[ti * nst_t + s3].r[yst[ti]] = S.cnt[yst[ti]]
        ffn_state["slab"] += len(all_slabs)

        B1l = B1[:].rearrange("p k n -> p (k n)").bitcast(F32)
        G0 = [B1l[:, r * 1024:(r + 1) * 1024] for r in range(2)]
        G1 = [B1l[:, 2048 + r * 1024: 2048 + (r + 1) * 1024] for r in range(2)]
        RR = [B1l[:, 4096 + r * 1024: 4096 + (r + 1) * 1024] for r in range(2)]
        GBC = B2f[:, 0:1024]
        OUTt = [B2f[:, 1024 + r * 1024: 1024 + (r + 1) * 1024] for r in range(2)]
        OUTtb = [Buf("outt0"), Buf("outt1")]
        G0b = [Buf("g00"), Buf("g01")]
        G1b = [Buf("g10"), Buf("g11")]
        RRb = [Buf("rr0"), Buf("rr1")]
        GBCb = Buf("gbc")
        gsm = [scs[0], scs[1]]
        osem = [S.new_sem("do0"), S.new_sem("do1")]
        sgb = sc
        SSQ = SCR[:, 0:16]
        RS2 = SCR[:, 16:32]
        SSb = Buf("ssq")
        S.barrier()
        S.op("sp", lambda: nc.sync.dma_start(out=GBC, in_=gbc_d), writes=[GBCb], dma_sem=sgb)
        for r in range(2):
            S.op("pool", (lambda r=r: nc.gpsimd.memset(G0[r], 0.0)), writes=[G0b[r]])
            S.op("pool", (lambda r=r: nc.gpsimd.memset(G1[r], 0.0)), writes=[G1b[r]])
        last = []
        for j in range(16):
            c0 = HALO + 128 * j
            r = j % 2
            S.op("pool", (lambda j=j, r=r: nc.gpsimd.indirect_dma_start(
                out=G0[r], out_offset=None, in_=ys_d, in_offset=bass.IndirectOffsetOnAxis(S0U[:, j:j + 1], 0),
                bounds_check=bnd["v"], oob_is_err=False)),
                reads=[YSDb, IDXb], writes=[G0b[r]], dma_sem=gsm[r])
            S.op("pool", (lambda j=j, r=r: nc.gpsimd.indirect_dma_start(
                out=G1[r], out_offset=None, in_=ys_d, in_offset=bass.IndirectOffsetOnAxis(S1U[:, j:j + 1], 0),
                bounds_check=bnd["v"], oob_is_err=False)),
                reads=[YSDb, IDXb], writes=[G1b[r]], dma_sem=gsm[r])
            G0b[r].w = dict(G1b[r].w)
            for half in range(2):
                i = state["bank"]
                state["bank"] = (i + 1) % 8
                for kk in range(4):
                    S.op("pe", (lambda i=i, kk=kk, half=half, c0=c0: nc.tensor.matmul(
                        ps[i][:, kk * 128:(kk + 1) * 128], lhsT=H[:, 4 * half + kk, c0:c0 + 128], rhs=identf[:],
                        start=True, stop=True)),
                        reads=Hb + [Bc] if kk in (0, 3) else (), writes=[P[i]] if kk in (0, 3) else (), inc=(kk == 3))
                S.op("dve", (lambda i=i, j=j, r=r, half=half: nc.vector.scalar_tensor_tensor(
                    out=RR[r][:, half * 512:(half + 1) * 512], in0=G0[r][:, half * 512:(half + 1) * 512],
                    scalar=RT[:, j, 12:13], in1=ps[i][:, :], op0=ALU.mult, op1=ALU.add)),
                    reads=[P[i], G0b[r]] + allrt, writes=[RRb[r]], waw=(half == 0))
            S.op("dve", (lambda j=j, r=r: nc.vector.scalar_tensor_tensor(
                out=RR[r], in0=G1[r], scalar=RT[:, j, 13:14], in1=RR[r], op0=ALU.mult, op1=ALU.add)),
                reads=[G1b[r], RRb[r]] + allrt, writes=[RRb[r]])
            S.op("act", (lambda j=j, r=r: nc.scalar.activation(
                out=OUTt[r], in_=RR[r], func=AF.Square, accum_out=SSQ[:, j:j + 1])),
                reads=[RRb[r]], writes=[OUTtb[r], SSb])
            S.op("act", (lambda j=j: nc.scalar.activation(
                out=RS2[:, j:j + 1], in_=SSQ[:, j:j + 1], func=AF.Sqrt, bias=eps_ap(RMS_EPS), scale=1.0 / D)),
                reads=[SSb, Bc], writes=[SSb])
            S.op("dve", (lambda j=j: nc.vector.reciprocal(out=RS2[:, j:j + 1], in_=RS2[:, j:j + 1])),
                 reads=[SSb], writes=[SSb])
            S.op("dve", (lambda j=j, r=r: nc.vector.scalar_tensor_tensor(
                out=OUTt[r], in0=RR[r], scalar=RS2[:, j:j + 1], in1=GBC, op0=ALU.mult, op1=ALU.mult)),
                reads=[RRb[r], SSb, GBCb], writes=[OUTtb[r]])
            tok = S.op("sp", (lambda j=j, r=r: nc.sync.dma_start(out=out_d[128 * j:128 * (j + 1), :], in_=OUTt[r])),
                       reads=[OUTtb[r]], dma_sem=osem[r])
            last.append(tok)
        S.op("sp", None, after=last, inc=False)
        print("sbuf bytes remaining:", nc.sbuf_bytes_remaining)
        S.run_block()
    return nc


_CACHE = {}


def _prep_inputs(x, meta_tokens, conv_w_pw1, conv_b_pw1, conv_w_dw, conv_b_dw, conv_ln_g, conv_ln_b,
                 conv_w_pw2, conv_b_pw2, pool_w_group, pool_scale, ffn_w_gate, ffn_w_up, ffn_w_down,
                 moe_w_router, moe_w_gate, moe_w_up, moe_w_down, mix_norm_g, ffn_norm_g, final_norm_g):
    f = lambda a: np.ascontiguousarray(np.asarray(a, dtype=np.float32))

    def pk(v):
        v = np.asarray(v, dtype=np.float32).reshape(-1, 128)
        return v.T

    cols = [pk(mix_norm_g[0]), pk(mix_norm_g[1]), pk(ffn_norm_g[0]), pk(ffn_norm_g[1]), pk(final_norm_g),
            pk(conv_b_pw1[0]), pk(conv_b_dw[0]), pk(conv_ln_g[0]), pk(conv_ln_b[0]), pk(conv_b_pw2[0]),
            pk(pool_scale[0])]
    wdw = np.asarray(conv_w_dw[0], dtype=np.float32)
    wdw = wdw.reshape(CONVW, KC, 128).transpose(2, 1, 0).reshape(128, KC * CONVW)
    cvec = f(np.concatenate(cols + [wdw], axis=1))
    assert cvec.shape == (128, NCV), cvec.shape
    wr = np.asarray(moe_w_router[0], dtype=np.float32).reshape(KC, 128, NE).transpose(1, 0, 2).reshape(128, KC * NE)
    xe = np.concatenate([np.zeros((32, D), np.float32), np.asarray(meta_tokens, np.float32),
                         np.asarray(x[0], np.float32)], axis=0)
    shared = {
        "cvec": cvec, "wr": f(wr), "ident": np.eye(128, dtype=np.float32),
        "tri": np.triu(np.ones((128, 128), np.float32), 1),
        "gbc": f(np.broadcast_to(np.asarray(final_norm_g, np.float32)[None, :], (128, D))),
        "w_pw1": f(conv_w_pw1[0]), "w_pw2": f(conv_w_pw2[0]), "pool_w": f(pool_w_group[0]),
        "ffn_wg": f(ffn_w_gate[0]), "ffn_wu": f(ffn_w_up[0]), "ffn_wd": f(ffn_w_down[0]),
        "moe_wg": f(moe_w_gate[0]), "moe_wu": f(moe_w_up[0]), "moe_wd": f(moe_w_down[0]),
    }
    in_maps = []
    for c in range(NCORES):
        m = dict(shared)
        m["xT"] = f(xe[TOK * c: TOK * c + TW].T)
        um = np.ones((128, HALO), np.float32)
        if c == 0:
            um[:, :32] = 0.0
        m["umask"] = um
        in_maps.append(m)
    return in_maps


def kernel(**inputs):
    if "nc" not in _CACHE:
        _CACHE["nc"] = build_program()
    nc = _CACHE["nc"]
    in_maps = _prep_inputs(**inputs)
    res = run_bass_kernel_spmd(nc, in_maps, core_ids=list(range(NCORES)))
    outs = [np.asarray(r["out"]) for r in res.results]
    out = np.concatenate(outs, axis=0).reshape(1, SEQ, D).astype(np.float32)
    return out
```
